# Optimizing a Trainium2 kernel written in Bass

```python
import math
import jax, jax.numpy as jnp
from jax import lax
import numpy as np

D_MODEL = 1024
BATCH = 32
SEQ = 2048
DEPTH = 1
DEC_BATCH = 16
DEC_SEQ = 4096
PAST_LEN = 128

PLE_DIM = 256
N_ATTN_HEADS = 8
ATTN_HEAD_DIM = 64
ATTN_WIDTH = N_ATTN_HEADS * ATTN_HEAD_DIM
DILATED_PATTERNS = ((128, 1), (512, 4), (2048, 16))
REL_BUCKETS = 32
REL_MAX_DISTANCE = 1024
SSD_HEADS = 8
SSD_HEAD_DIM = 64
SSD_INNER = SSD_HEADS * SSD_HEAD_DIM
SSD_GROUPS = 2
SSD_STATE = 128
SSD_CONV = 5
SSD_CHUNK = 128
CONV_DIM = SSD_INNER + 2 * SSD_GROUPS * SSD_STATE
MIX_WIDTH = ATTN_WIDTH + SSD_INNER
IN_PROJ = 3 * ATTN_WIDTH + SSD_INNER + CONV_DIM + 2 * SSD_HEADS
PROJ_SPLITS = (ATTN_WIDTH, 2 * ATTN_WIDTH, 3 * ATTN_WIDTH, 3 * ATTN_WIDTH + SSD_INNER, 3 * ATTN_WIDTH + SSD_INNER + CONV_DIM)
N_EXPERTS = 16
EXPERT_FF = 2816
CAPACITY_FACTOR = 2
EPS = 1e-6

kernel_name = 'hybrid_dilated_ssd_expert_choice_encoder'


def _rmsnorm(x, g):
    xf = x.astype(jnp.float32)
    y = xf * lax.rsqrt(jnp.mean(xf * xf, axis=-1, keepdims=True) + EPS)
    return (y * g.astype(jnp.float32)).astype(x.dtype)


def _t5_bucket(rel):
    half = REL_BUCKETS // 2
    max_exact = half // 2
    n = np.abs(rel)
    large = max_exact + (np.log(np.maximum(n, 1) / max_exact) / math.log(REL_MAX_DISTANCE / max_exact) * (half - max_exact)).astype(np.int32)
    large = np.minimum(large, half - 1)
    return (np.where(rel > 0, half, 0) + np.where(n < max_exact, n, large)).astype(np.int32)


def _dilated_band_attention(q, k, v, rel_bias, window, dilation):
    b, s, h, e = q.shape
    half = window // (2 * dilation)
    blk = half
    n = s // dilation
    nb = -(-n // blk)
    npad = nb * blk

    def to_sub(a):
        a = a.reshape(b, n, dilation, h, e).transpose(0, 2, 1, 3, 4)
        return jnp.pad(a, ((0, 0), (0, 0), (0, npad - n), (0, 0), (0, 0)))

    def band(a):
        a = jnp.pad(to_sub(a), ((0, 0), (0, 0), (blk, blk), (0, 0), (0, 0))).reshape(b, dilation, nb + 2, blk, h, e)
        return jnp.concatenate([a[:, :, :-2], a[:, :, 1:-1], a[:, :, 2:]], axis=3)

    qb = to_sub(q).reshape(b, dilation, nb, blk, h, e)
    kb, vb = band(k), band(v)
    qi = np.arange(blk)[:, None]
    ki = np.arange(3 * blk)[None, :]
    off = ki - blk - qi
    u = np.arange(nb)[:, None, None] * blk - blk + ki[None]
    mask = (np.abs(off) <= half)[None] & (u >= 0) & (u < n)
    bias = rel_bias[_t5_bucket(off * dilation)].astype(jnp.float32).transpose(2, 0, 1)
    logits = jnp.einsum('bdnqhe,bdnkhe->bdnhqk', qb, kb) * (ATTN_HEAD_DIM ** -0.5) + bias
    logits = jnp.where(mask[:, None], logits, -jnp.inf)
    m = jnp.max(logits, axis=-1, keepdims=True)
    pexp = jnp.exp(logits - m)
    ssum = jnp.sum(pexp, axis=-1, keepdims=True)
    out = jnp.einsum('bdnhqk,bdnkhe->bdnqhe', pexp, vb) / ssum[..., 0].transpose(0, 1, 2, 4, 3)[..., None]
    lse = (m + jnp.log(ssum))[..., 0].transpose(0, 1, 2, 4, 3)
    out = out.reshape(b, dilation, npad, h, e)[:, :, :n].transpose(0, 2, 1, 3, 4).reshape(b, s, h, e)
    lse = lse.reshape(b, dilation, npad, h)[:, :, :n].transpose(0, 2, 1, 3).reshape(b, s, h)
    return out, lse


def _dilated_mixture(q, k, v, rel_bias):
    q, k, v = (a.astype(jnp.float32) for a in (q, k, v))
    outs, lses = [], []
    for window, dilation in DILATED_PATTERNS:
        o, l = _dilated_band_attention(q, k, v, rel_bias, window, dilation)
        outs.append(o)
        lses.append(l)
    w = jax.nn.softmax(jnp.stack(lses), axis=0)
    return jnp.sum(w[..., None] * jnp.stack(outs), axis=0)


def _ssd_scan(x, dt, A, bm, cm):
    b, s, h, p = x.shape
    g, n = bm.shape[2], bm.shape[3]
    r = h // g
    l = SSD_CHUNK
    c = s // l
    xdt = (x * dt[..., None]).reshape(b, c, l, g, r, p)
    a = (dt * A).reshape(b, c, l, g, r).transpose(0, 3, 4, 1, 2)
    a_cum = jnp.cumsum(a, axis=-1)
    bc = bm.reshape(b, c, l, g, n)
    cc = cm.reshape(b, c, l, g, n)
    seg = a_cum[..., :, None] - a_cum[..., None, :]
    tril = np.tril(np.ones((l, l), dtype=bool))
    decay = jnp.exp(jnp.where(tril, seg, -jnp.inf))
    cb = jnp.einsum('bclgn,bcsgn->bgcls', cc, bc)
    y_diag = jnp.einsum('bgcls,bgrcls,bcsgrp->bclgrp', cb, decay, xdt)
    decay_states = jnp.exp(a_cum[..., -1:] - a_cum)
    states = jnp.einsum('bclgn,bgrcl,bclgrp->bcgrpn', bc, decay_states, xdt)
    chunk_decay = jnp.exp(a_cum[..., -1])

    def step(hstate, inp):
        st, dec = inp
        return hstate * dec[..., None, None] + st, hstate

    h0 = jnp.zeros((b, g, r, p, n), jnp.float32)
    _, h_in = lax.scan(step, h0, (states.transpose(1, 0, 2, 3, 4, 5), chunk_decay.transpose(3, 0, 1, 2)))
    y_off = jnp.einsum('bclgn,cbgrpn,bgrcl->bclgrp', cc, h_in, jnp.exp(a_cum))
    return (y_diag + y_off).reshape(b, s, h, p)


def _ssd_mixer(z, xbc, dt_raw, conv_w, conv_b, dt_bias, a_log, d_skip, g_norm):
    b, s, _ = z.shape
    xbc = lax.conv_general_dilated(xbc, conv_w[:, None, :].astype(xbc.dtype), (1,), [(SSD_CONV // 2, SSD_CONV // 2)],
                                   dimension_numbers=('NWC', 'WIO', 'NWC'), feature_group_count=CONV_DIM)
    xbc = jax.nn.silu(xbc + conv_b.astype(xbc.dtype)).astype(jnp.float32)
    xs, bm, cm = jnp.split(xbc, [SSD_INNER, SSD_INNER + SSD_GROUPS * SSD_STATE], axis=-1)
    xs = xs.reshape(b, s, SSD_HEADS, SSD_HEAD_DIM)
    bm = bm.reshape(b, s, SSD_GROUPS, SSD_STATE)
    cm = cm.reshape(b, s, SSD_GROUPS, SSD_STATE)
    dt = jax.nn.softplus(dt_raw.astype(jnp.float32).reshape(b, s, 2, SSD_HEADS) + dt_bias.astype(jnp.float32))
    A = -jnp.exp(a_log.astype(jnp.float32))
    y_fwd = _ssd_scan(xs, dt[:, :, 0], A[0], bm, cm)
    flip = lambda a: a[:, ::-1]
    y_bwd = flip(_ssd_scan(flip(xs), flip(dt[:, :, 1]), A[1], flip(bm), flip(cm)))
    y = y_fwd + y_bwd + d_skip.astype(jnp.float32)[:, None] * xs
    y = y.reshape(b, s, SSD_INNER) * jax.nn.silu(z.astype(jnp.float32))
    yg = y.reshape(b, s, SSD_GROUPS, SSD_INNER // SSD_GROUPS)
    yg = yg * lax.rsqrt(jnp.mean(yg * yg, axis=-1, keepdims=True) + EPS)
    return (yg.reshape(b, s, SSD_INNER) * g_norm.astype(jnp.float32)).astype(z.dtype)


def _expert_choice_ffn(h, w_router, w_gate, w_up, w_down):
    t = h.shape[0]
    cap = max(1, CAPACITY_FACTOR * t // N_EXPERTS)
    aff = jax.nn.softmax((h @ w_router).astype(jnp.float32), axis=-1)
    gate, idx = lax.top_k(aff.T, cap)
    xs = h[idx]

    def expert(args):
        xe, wg, wu, wd = args
        return (jax.nn.silu(xe @ wg) * (xe @ wu)) @ wd

    ye = lax.map(expert, (xs, w_gate, w_up, w_down)) * gate[..., None].astype(h.dtype)
    return jnp.zeros_like(h).at[idx.reshape(-1)].add(ye.reshape(-1, h.shape[1]))


def _layer(x, pe, rel_bias, g_mix, w_in, conv_w, conv_b, dt_bias, a_log, d_skip, g_ssd, w_out,
           g_ffn, w_router, w_gate, w_up, w_down, g_pg, w_pg, w_ple, g_ple):
    b, s, _ = x.shape
    h = _rmsnorm(x, g_mix)
    proj = h @ w_in
    q, k, v, z, xbc, dt_raw = jnp.split(proj, PROJ_SPLITS, axis=-1)
    heads = lambda a: a.reshape(b, s, N_ATTN_HEADS, ATTN_HEAD_DIM)
    attn = _dilated_mixture(heads(q), heads(k), heads(v), rel_bias).reshape(b, s, ATTN_WIDTH).astype(x.dtype)
    ssd = _ssd_mixer(z, xbc, dt_raw, conv_w, conv_b, dt_bias, a_log, d_skip, g_ssd)
    x = x + jnp.concatenate([attn, ssd], axis=-1) @ w_out
    hn = _rmsnorm(x, g_ffn)
    x = x + _expert_choice_ffn(hn.reshape(b * s, D_MODEL), w_router, w_gate, w_up, w_down).reshape(b, s, D_MODEL)
    e = _rmsnorm(pe @ w_ple, g_ple)
    gate = jax.nn.sigmoid(_rmsnorm(x, g_pg) @ w_pg)
    return x + gate * e


def _trunk(x, p, rel_bias, g_mix, w_in, conv_w, conv_b, dt_bias, a_log, d_skip, g_ssd, w_out,
           g_ffn, w_router, w_gate, w_up, w_down, g_pg, w_pg, w_ple, g_ple, g_final):
    for i in range(DEPTH):
        x = _layer(x, p[i], rel_bias, g_mix[i], w_in[i], conv_w[i], conv_b[i], dt_bias[i], a_log[i], d_skip[i],
                   g_ssd[i], w_out[i], g_ffn[i], w_router[i], w_gate[i], w_up[i], w_down[i], g_pg[i], w_pg[i],
                   w_ple[i], g_ple[i])
    return _rmsnorm(x, g_final)


def setup_inputs(seed: int = 0) -> dict:
    key = jax.random.key(seed)
    ks = jax.random.split(key, 24)
    f32 = jnp.float32
    nrm = lambda k, shape, scale: jax.random.normal(k, shape, f32) * scale
    gain = lambda k, shape: 1.0 + 0.05 * jax.random.normal(k, shape, f32)
    dt0 = jnp.exp(jax.random.uniform(ks[10], (DEPTH, 2, SSD_HEADS), f32, math.log(1e-3), math.log(1e-1)))
    return {
        'x_prompt': nrm(ks[0], (BATCH, SEQ, D_MODEL), 1.0),
        'x_sample': nrm(ks[1], (DEC_BATCH, DEC_SEQ, D_MODEL), 1.0),
        'p_prompt': nrm(ks[2], (DEPTH, BATCH, SEQ, PLE_DIM), 1.0),
        'p_sample': nrm(ks[3], (DEPTH, DEC_BATCH, DEC_SEQ, PLE_DIM), 1.0),
        'rel_bias': nrm(ks[4], (REL_BUCKETS, N_ATTN_HEADS), 0.2),
        'g_mix': gain(ks[5], (DEPTH, D_MODEL)),
        'w_in': nrm(ks[6], (DEPTH, D_MODEL, IN_PROJ), D_MODEL ** -0.5),
        'conv_w': nrm(ks[7], (DEPTH, SSD_CONV, CONV_DIM), SSD_CONV ** -0.5),
        'conv_b': nrm(ks[8], (DEPTH, CONV_DIM), 0.02),
        'dt_bias': dt0 + jnp.log(-jnp.expm1(-dt0)),
        'a_log': jnp.log(jax.random.uniform(ks[9], (DEPTH, 2, SSD_HEADS), f32, 1.0, 16.0)),
        'd_skip': gain(ks[11], (DEPTH, SSD_HEADS)),
        'g_ssd': gain(ks[12], (DEPTH, SSD_INNER)),
        'w_out': nrm(ks[13], (DEPTH, MIX_WIDTH, D_MODEL), MIX_WIDTH ** -0.5),
        'g_ffn': gain(ks[14], (DEPTH, D_MODEL)),
        'w_router': nrm(ks[15], (DEPTH, D_MODEL, N_EXPERTS), D_MODEL ** -0.5),
        'w_gate': nrm(ks[16], (DEPTH, N_EXPERTS, D_MODEL, EXPERT_FF), D_MODEL ** -0.5),
        'w_up': nrm(ks[17], (DEPTH, N_EXPERTS, D_MODEL, EXPERT_FF), D_MODEL ** -0.5),
        'w_down': nrm(ks[18], (DEPTH, N_EXPERTS, EXPERT_FF, D_MODEL), EXPERT_FF ** -0.5),
        'g_pg': gain(ks[19], (DEPTH, D_MODEL)),
        'w_pg': nrm(ks[20], (DEPTH, D_MODEL, D_MODEL), D_MODEL ** -0.5),
        'w_ple': nrm(ks[21], (DEPTH, PLE_DIM, D_MODEL), PLE_DIM ** -0.5),
        'g_ple': gain(ks[22], (DEPTH, D_MODEL)),
        'g_final': gain(ks[23], (D_MODEL,)),
    }


def reference(x_prompt, x_sample, p_prompt, p_sample, rel_bias, g_mix, w_in, conv_w, conv_b, dt_bias, a_log,
              d_skip, g_ssd, w_out, g_ffn, w_router, w_gate, w_up, w_down, g_pg, w_pg, w_ple, g_ple, g_final):
    y_prompt = _trunk(x_prompt, p_prompt, rel_bias, g_mix, w_in, conv_w, conv_b, dt_bias, a_log, d_skip, g_ssd,
                      w_out, g_ffn, w_router, w_gate, w_up, w_down, g_pg, w_pg, w_ple, g_ple, g_final)
    y_sample = _trunk(x_sample, p_sample, rel_bias, g_mix, w_in, conv_w, conv_b, dt_bias, a_log, d_skip, g_ssd,
                      w_out, g_ffn, w_router, w_gate, w_up, w_down, g_pg, w_pg, w_ple, g_ple, g_final)
    return (y_prompt, y_sample)
```

```python
import math
from contextlib import ExitStack
import numpy as np
import concourse.bass as bass
import concourse.mybir as mybir
from concourse.bass_utils import run_bass_kernel_spmd

F32 = mybir.dt.float32
BF16 = mybir.dt.bfloat16
ALU = mybir.AluOpType
AF = mybir.ActivationFunctionType
AX = mybir.AxisListType

NCORES = 8
D = 1024
PAD = 1024
NEG = -30000.0
EPS = 1e-6
DILS = (1, 4, 16)


class Buf:
    __slots__ = ("name", "w", "r", "excl")

    def __init__(self, name, excl=False):
        self.name = name
        self.excl = excl
        self.w = {}
        self.r = {}


class Sem:
    __slots__ = ("h", "total")

    def __init__(self, h):
        self.h = h
        self.total = 0


class Eng:
    def __init__(self, name, handle, sem):
        self.name = name
        self.h = handle
        self.sem = sem
        self.waited = {}
        self.ring = []
        self.rpos = 0


class K:
    def __init__(self, nc, sp_ring=44, pool_ring=20, act_ring=12):
        self.nc = nc
        self.es = ExitStack()
        self.es_cur = self.es
        self.pe = self._eng("pe", nc.tensor)
        self.act = self._eng("act", nc.scalar)
        self.dve = self._eng("dve", nc.vector)
        self.pool = self._eng("pool", nc.gpsimd)
        self.sp = self._eng("sp", nc.sync)
        for e, n in ((self.sp, sp_ring), (self.pool, pool_ring), (self.act, act_ring)):
            e.ring = [self.new_sem(f"r_{e.name}{i}") for i in range(n)]
        self.n_ins = 0

    def new_sem(self, name):
        return Sem(self.es.enter_context(self.nc.semaphore(name)))

    def _eng(self, name, handle):
        return Eng(name, handle, self.new_sem("e_" + name))

    def sb(self, name, shape, dtype):
        self.uid = getattr(self, "uid", 0) + 1
        return self.es_cur.enter_context(self.nc.sbuf_tensor(f"{name}_{self.uid}", list(shape), dtype))

    def barrier(self):
        if getattr(self, "dead", False):
            return
        engs = [self.pe, self.act, self.dve, self.pool, self.sp]
        for e in engs:
            for o in engs:
                if o is not e and o.sem.total > 0:
                    self._wait(e, o.sem, o.sem.total)
                for s in o.ring:
                    if s.total > 0:
                        self._wait(e, s, s.total)

    def scope(self):
        return _Scope(self)

    def ps(self, name, shape, dtype):
        return self.es.enter_context(self.nc.psum_tensor(name, list(shape), dtype))

    def _wait(self, eng, s, v):
        if eng.waited.get(id(s), 0) < v:
            eng.h.wait_ge(s.h, v)
            eng.waited[id(s)] = v
            self.n_ins += 1

    def _deps(self, eng, reads, writes):
        for b in reads:
            for s, v in b.w.values():
                self._wait(eng, s, v)
        for b in writes:
            for s, v in b.w.values():
                self._wait(eng, s, v)
            for s, v in b.r.values():
                self._wait(eng, s, v)

    def _record(self, ev, reads, writes):
        s, v = ev
        for b in writes:
            b.w[id(s)] = ev
            b.r = {}
        for b in reads:
            b.r[id(s)] = ev

    def op(self, eng, fn, R=(), W=()):
        if getattr(self, "dead", False):
            return None
        ex = [b for b in R if b.excl]
        if ex:
            W = list(W) + ex
        self._deps(eng, R, W)
        ins = fn(eng.h)
        eng.sem.total += 1
        ins.then_inc(eng.sem.h, 1)
        self.n_ins += 1
        self._record((eng.sem, eng.sem.total), R, W)
        return ins

    def dma(self, eng, out, in_, R=(), W=(), **kw):
        if getattr(self, "dead", False):
            return None
        self._deps(eng, R, W)
        s = eng.ring[eng.rpos]
        eng.rpos = (eng.rpos + 1) % len(eng.ring)
        self._wait(eng, s, s.total)
        ins = eng.h.dma_start(out=out, in_=in_, **kw)
        s.total += 16
        ins.then_inc(s.h, 16)
        self.n_ins += 1
        self._record((s, s.total), R, W)
        return ins

    def finish(self, bufs):
        self._deps(self.sp, bufs, [])

    def close(self):
        self.es.close()

    def __del__(self):
        pass


class _Scope:
    def __init__(self, k):
        self.k = k

    def __enter__(self):
        self.k.barrier()
        self.old = self.k.es_cur
        self.es = ExitStack()
        self.es.__enter__()
        self.k.es_cur = self.es
        return self

    def __exit__(self, *a):
        self.k.barrier()
        self.k.es_cur = self.old
        return self.es.__exit__(*a)


def _t5_bucket(rel):
    half = 16
    max_exact = 8
    n = np.abs(rel)
    large = max_exact + (np.log(np.maximum(n, 1) / max_exact) / math.log(1024 / max_exact) * (half - max_exact)).astype(np.int32)
    large = np.minimum(large, half - 1)
    return (np.where(rel > 0, half, 0) + np.where(n < max_exact, n, large)).astype(np.int32)


def _onehot_tables():
    oh = np.zeros((3, 33, 384), np.float32)
    for p, d in enumerate(DILS):
        for i in range(384):
            ds = i - 191
            if abs(ds) <= 64:
                oh[p, int(_t5_bucket(np.array(ds * d))), i] = 1.0
            else:
                oh[p, 32, i] = 1.0
    return oh


def build_a(seqs, dbg=False, upto=9):
    nc, k = _build_a(seqs, dbg, upto)
    k.close()
    return nc, k.n_ins


def _build_a(seqs, dbg=False, upto=9):
    NT = sum(s for _, s in seqs)
    SMAX = max(s for _, s in seqs)
    nc = bass.Bass("TRN2", target_bir_lowering=False)
    k = K(nc)

    def din(name, shape):
        return nc.dram_tensor(name, list(shape), F32, kind="ExternalInput").ap()

    x_in = din("x", [NT, D])
    w_in = din("w_in", [D, 3088])
    w_out = din("w_out", [D, D])
    w_router = din("w_router", [D, 16])
    g_mix = din("g_mix", [1, D])
    g_ffn = din("g_ffn", [1, D])
    conv_w = din("conv_w", [5, 1024])
    conv_b = din("conv_b", [1, 1024])
    dt_bias = din("dt_bias", [1, 16])
    a_log = din("a_log", [1, 16])
    d_skip = din("d_skip", [1, 8])
    g_ssd = din("g_ssd", [1, 512])
    rel_bias = din("rel_bias", [32, 8])
    oh_in = din("oh", [3, 33, 384])
    x1_out = nc.dram_tensor("x1", [NT, D], F32, kind="ExternalOutput").ap()
    aff_out = nc.dram_tensor("aff", [NT, 16], F32, kind="ExternalOutput").ap()
    b_x1 = Buf("x1o")
    b_aff = Buf("affo")

    def dscr(name, shape, dt):
        return nc.dram_tensor(name, list(shape), dt).ap(), Buf(name)

    qT_d, b_qT = dscr("qT_d", [512, SMAX], BF16)
    kT_d, b_kT = dscr("kT_d", [512, SMAX], BF16)
    vT_d, b_vT = dscr("vT_d", [512, SMAX], BF16)
    xbc_d, b_xbc = dscr("xbc_d", [1024, SMAX + 4], F32)
    z_d, b_z = dscr("z_d", [SMAX, 512], F32)
    dt_d, b_dt = dscr("dt_d", [SMAX, 16], F32)
    yf_d, b_yf = dscr("yf_d", [SMAX, 512], F32)
    at_d, b_at = dscr("at_d", [8, 64, SMAX], BF16)
    tab_d_t = nc.dram_tensor("tab_d", [3, 8, 384], F32)
    tab_d = tab_d_t.ap()
    b_tab = Buf("tab_d")

    def T(name, shape, dt):
        return k.sb(name, shape, dt), Buf(name)

    w_in_bf, b_win = T("w_in_bf", [128, 8, 3088], BF16)
    wo_att, b_woa = T("wo_att", [64, 8, 1024], BF16)
    wo_ssd, b_wos = T("wo_ssd", [128, 4, 1024], BF16)
    wr_bf, b_wr = T("wr_bf", [128, 8, 16], BF16)
    gmix, b_gmix = T("gmix", [128, D], F32)
    gffn, b_gffn = T("gffn", [128, D], F32)
    gssd, b_gssd = T("gssd", [128, 512], F32)
    cw, b_cw = T("cw", [128, 5, 8], F32)
    cb, b_cb = T("cb", [128, 8], F32)
    dtb, b_dtb = T("dtb", [128, 16], F32)
    Abc, b_A = T("Abc", [128, 16], F32)
    dsk, b_dsk = T("dsk", [128, 8], F32)
    identf, b_idf = T("identf", [128, 128], F32)
    ident, b_id = T("ident", [128, 128], BF16)
    triU, b_triU = T("triU", [128, 128], F32)
    triL, b_triL = T("triL", [128, 128], F32)
    nmU, b_nmU = T("nmU", [128, 128], F32)
    nmL, b_nmL = T("nmL", [128, 128], F32)
    onesf, b_ones = T("onesf", [128, 128], F32)
    biasT, b_bias = T("biasT", [128, 12, 512], BF16)

    PB = []
    for i in range(6):
        PB.append((k.ps(f"pb{i}", [128, 512], F32), Buf(f"pb{i}", excl=True)))
    PH = []
    for i in range(2):
        PH.append((k.ps(f"ph{i}", [128, 1024], BF16), Buf(f"ph{i}", excl=True)))

    sp, act, dve, pool, pe = k.sp, k.act, k.dve, k.pool, k.pe

    def bc_load(dst, bdst, src_row, n):
        k.dma(sp, dst[:], src_row.to_broadcast([128, n]), W=[bdst])

    bc_load(gmix, b_gmix, g_mix[0:1, :], D)
    bc_load(gffn, b_gffn, g_ffn[0:1, :], D)
    bc_load(gssd, b_gssd, g_ssd[0:1, :], 512)
    bc_load(dtb, b_dtb, dt_bias[0:1, :], 16)
    bc_load(Abc, b_A, a_log[0:1, :], 16)
    bc_load(dsk, b_dsk, d_skip[0:1, :], 8)
    for kk in range(5):
        k.dma(sp, cw[:, kk, :], conv_w[kk:kk + 1, :].rearrange("o (c p) -> p (o c)", p=128), W=[b_cw], allow_slow_non_contiguous=True)
    k.dma(sp, cb[:], conv_b.rearrange("o (c p) -> p (o c)", p=128), W=[b_cb], allow_slow_non_contiguous=True)
    k.op(act, lambda e: e.activation(out=Abc[:], in_=Abc[:], func=AF.Exp), R=[b_A], W=[b_A])
    k.op(dve, lambda e: e.tensor_scalar(out=Abc[:], in0=Abc[:], scalar1=-1.0, scalar2=None, op0=ALU.mult), R=[b_A], W=[b_A])

    k.op(pool, lambda e: e.memset(onesf[:], 1.0), W=[b_ones])
    k.op(pool, lambda e: e.memset(identf[:], 0.0), W=[b_idf])
    k.op(pool, lambda e: e.affine_select(out=identf[:], in_=identf[:], pattern=[[-1, 128]], compare_op=ALU.not_equal, fill=1.0, base=0, channel_multiplier=1), R=[b_idf], W=[b_idf])
    k.op(dve, lambda e: e.tensor_copy(out=ident[:], in_=identf[:]), R=[b_idf], W=[b_id])
    k.op(pool, lambda e: e.affine_select(out=triU[:], in_=onesf[:], pattern=[[1, 128]], compare_op=ALU.is_ge, fill=0.0, base=0, channel_multiplier=-1), R=[b_ones], W=[b_triU])
    k.op(pool, lambda e: e.affine_select(out=triL[:], in_=onesf[:], pattern=[[-1, 128]], compare_op=ALU.is_ge, fill=0.0, base=0, channel_multiplier=1), R=[b_ones], W=[b_triL])
    k.op(dve, lambda e: e.tensor_scalar(out=nmU[:], in0=triU[:], scalar1=-1.0, scalar2=-NEG, op0=ALU.add, op1=ALU.mult), R=[b_triU], W=[b_nmU])
    k.op(dve, lambda e: e.tensor_scalar(out=nmL[:], in0=triL[:], scalar1=-1.0, scalar2=-NEG, op0=ALU.add, op1=ALU.mult), R=[b_triL], W=[b_nmL])

    with k.scope():
        stg, b_stg = T("stg", [128, 2048], F32)
        relx, b_relx = T("relx", [33, 8], F32)
        oh_sb, b_oh = T("oh_sb", [33, 3, 384], F32)
        tab_sb, b_tabsb = T("tab_sb", [8, 3, 384], F32)
        for c0 in range(0, 3088, 256):
            cn = min(256, 3088 - c0)
            sv = stg[:, 0:8 * cn].rearrange("p (c n) -> p c n", c=8)
            k.dma(sp, sv, w_in[:, c0:c0 + cn].rearrange("(c p) n -> p c n", p=128), W=[b_stg])
            k.op(dve, lambda e: e.tensor_copy(out=w_in_bf[:, :, c0:c0 + cn], in_=sv), R=[b_stg], W=[b_win])
        for hh in range(4):
            sv = stg[0:64, :].rearrange("p (h n) -> p h n", h=2)
            k.dma(sp, sv, w_out[hh * 128:(hh + 1) * 128, :].rearrange("(h p) n -> p h n", p=64), W=[b_stg])
            k.op(dve, lambda e: e.tensor_copy(out=wo_att[:, hh * 2:(hh + 1) * 2, :], in_=sv), R=[b_stg], W=[b_woa])
        for hh in range(2):
            sv = stg[:, :].rearrange("p (c n) -> p c n", c=2)
            k.dma(sp, sv, w_out[512 + hh * 256:512 + (hh + 1) * 256, :].rearrange("(c p) n -> p c n", p=128), W=[b_stg])
            k.op(dve, lambda e: e.tensor_copy(out=wo_ssd[:, hh * 2:(hh + 1) * 2, :], in_=sv), R=[b_stg], W=[b_wos])
        sv = stg[:, 0:128].rearrange("p (c n) -> p c n", c=8)
        k.dma(sp, sv, w_router.rearrange("(c p) n -> p c n", p=128), W=[b_stg])
        k.op(dve, lambda e: e.tensor_copy(out=wr_bf[:], in_=sv), R=[b_stg], W=[b_wr])

        k.op(pool, lambda e: e.memset(relx[:], NEG), W=[b_relx])
        k.dma(sp, relx[0:32, :], rel_bias[:, :], W=[b_relx])
        k.dma(sp, oh_sb[:], oh_in.rearrange("p b i -> b p i"), W=[b_oh])
        for p in range(3):
            pt, bpt = PB[p % 2]
            k.op(pe, lambda e: e.matmul(pt[0:8, 0:384], lhsT=relx[:, :], rhs=oh_sb[:, p, :], start=True, stop=True), R=[b_relx, b_oh], W=[bpt])
            k.op(act, lambda e: e.copy(out=tab_sb[:, p, :], in_=pt[0:8, 0:384]), R=[bpt], W=[b_tabsb])
        k.dma(sp, tab_d.rearrange("p h i -> h p i"), tab_sb[:], R=[b_tabsb], W=[b_tab])
        for hp in range(4):
            for p in range(3):
                for h in range(2):
                    for jj in range(2):
                        src = bass.AP(tensor=tab_d_t, offset=(p * 8 + hp * 2 + h) * 384 + 127 + 128 * jj, ap=[[1, 128], [-1, 128]])
                        c0 = (h * 2 + jj) * 128
                        k.dma(sp, stg[:, c0:c0 + 128], src, R=[b_tab], W=[b_stg], allow_slow_non_contiguous=True)
                k.op(dve, lambda e: e.tensor_copy(out=biasT[:, hp * 3 + p, :], in_=stg[:, 0:512]), R=[b_stg], W=[b_bias])

    xt = [T(f"xt{i}", [128, D], F32) for i in range(2)]
    junk, b_junk = T("junk", [128, D], F32)
    st_s, b_sts = T("st_s", [128, 8], F32)
    hb, b_hb = T("hb", [128, D], BF16)
    hT, b_hT = T("hT", [128, 8, 512], BF16)
    evb = [T(f"evb{i}", [128, 512], BF16) for i in range(3)]
    evf = [T(f"evf{i}", [128, 512], F32) for i in range(3)]
    zpad, b_zpad = T("zpad", [128, 8, 2], F32)
    k.op(pool, lambda e: e.memset(zpad[:], 0.0), W=[b_zpad])

    def rmsnorm_to_bf(xtile, bx, gtile, bg, out_bf, bout, n=D):
        k.op(act, lambda e: e.activation(out=junk[:, 0:n], in_=xtile, func=AF.Square, accum_out=st_s[:, 0:1]), R=[bx], W=[b_junk, b_sts])
        k.op(dve, lambda e: e.tensor_scalar(out=st_s[:, 1:2], in0=st_s[:, 0:1], scalar1=1.0 / n, scalar2=EPS, op0=ALU.mult, op1=ALU.add), R=[b_sts], W=[b_sts])
        k.op(act, lambda e: e.activation(out=st_s[:, 2:3], in_=st_s[:, 1:2], func=AF.Sqrt), R=[b_sts], W=[b_sts])
        k.op(dve, lambda e: e.reciprocal(out=st_s[:, 3:4], in_=st_s[:, 2:3]), R=[b_sts], W=[b_sts])
        k.op(dve, lambda e: e.scalar_tensor_tensor(out=out_bf, in0=xtile, scalar=st_s[:, 3:4], in1=gtile, op0=ALU.mult, op1=ALU.mult), R=[bx, b_sts, bg], W=[bout])

    cnt = {"ev": 0, "ph": 0, "pb": 0}

    def done():
        k.barrier()
        k.dead = True
        return nc, k

    if upto == 0:
        return done()

    for (row0, S) in seqs:
        k.dma(sp, xbc_d[:, 0:2].rearrange("(c p) t -> p c t", p=128), zpad[:], R=[b_zpad], W=[b_xbc])
        k.dma(sp, xbc_d[:, S + 2:S + 4].rearrange("(c p) t -> p c t", p=128), zpad[:], R=[b_zpad], W=[b_xbc])
        for blk in range(S // 512):
            for t in range(4):
                xtile, bx = xt[t % 2]
                r = row0 + blk * 512 + t * 128
                k.dma(sp, xtile[:], x_in[r:r + 128, :], W=[bx])
                rmsnorm_to_bf(xtile[:], bx, gmix[:], b_gmix, hb[:], b_hb)
                ph, bph = PH[cnt["ph"] % 2]
                cnt["ph"] += 1
                for c in range(8):
                    k.op(pe, lambda e: e.transpose(out=ph[:, c * 128:(c + 1) * 128], in_=hb[:, c * 128:(c + 1) * 128], identity=ident[:]), R=[b_hb, b_id], W=[bph])
                k.op(act, lambda e: e.copy(out=hT[:, :, t * 128:(t + 1) * 128], in_=ph[:, :].rearrange("p (c t) -> p c t", c=8)), R=[bph], W=[b_hT])
            fcs = [(i, i * 128) for i in range(12)] + [(12 + i, 2048 + i * 128) for i in range(8)]
            for (fi, col0) in fcs:
                pt, bpt = PB[cnt["pb"] % 2]
                cnt["pb"] += 1
                for kc in range(8):
                    k.op(pe, lambda e: e.matmul(pt[:, :], lhsT=w_in_bf[:, kc, col0:col0 + 128], rhs=hT[:, kc, :], start=(kc == 0), stop=(kc == 7)), R=[b_win, b_hT], W=[bpt])
                i = cnt["ev"] % 3
                cnt["ev"] += 1
                if fi < 12:
                    et, bet = evb[i]
                    dst, bd = [(qT_d, b_qT), (kT_d, b_kT), (vT_d, b_vT)][fi // 4]
                    drows = dst[(fi % 4) * 128:(fi % 4 + 1) * 128, blk * 512:(blk + 1) * 512]
                else:
                    et, bet = evf[i]
                    dst, bd = xbc_d, b_xbc
                    drows = dst[(fi - 12) * 128:(fi - 11) * 128, 2 + blk * 512:2 + (blk + 1) * 512]
                if fi % 2 == 0:
                    k.op(act, lambda e: e.copy(out=et[:], in_=pt[:, :]), R=[bpt], W=[bet])
                else:
                    k.op(dve, lambda e: e.tensor_copy(out=et[:], in_=pt[:, :]), R=[bpt], W=[bet])
                k.dma(sp, drows, et[:], R=[bet], W=[bd])
            for t in range(4):
                pt, bpt = PB[2 + t % 2]
                for kc in range(8):
                    k.op(pe, lambda e: e.matmul(pt[:, :], lhsT=hT[:, kc, t * 128:(t + 1) * 128], rhs=w_in_bf[:, kc, 1536:2048], start=(kc == 0), stop=(kc == 7)), R=[b_win, b_hT], W=[bpt])
                i = cnt["ev"] % 3
                cnt["ev"] += 1
                et, bet = evf[i]
                k.op(act, lambda e: e.copy(out=et[:], in_=pt[:, :]), R=[bpt], W=[bet])
                r = blk * 512 + t * 128
                k.dma(sp, z_d[r:r + 128, :], et[:], R=[bet], W=[b_z])
                pt2, bpt2 = PB[4 + t % 2]
                for kc in range(8):
                    k.op(pe, lambda e: e.matmul(pt2[:, 0:16], lhsT=hT[:, kc, t * 128:(t + 1) * 128], rhs=w_in_bf[:, kc, 3072:3088], start=(kc == 0), stop=(kc == 7)), R=[b_win, b_hT], W=[bpt2])
                i = cnt["ev"] % 3
                cnt["ev"] += 1
                et, bet = evf[i]
                k.op(dve, lambda e: e.tensor_copy(out=et[:, 0:16], in_=pt2[:, 0:16]), R=[bpt2], W=[bet])
                k.dma(sp, dt_d[r:r + 128, :], et[:, 0:16], R=[bet], W=[b_dt])

        if upto == 1:
            return done()
        with k.scope():
            qp, b_qp = T("qp", [128, SMAX], BF16)
            kp, b_kp = T("kp", [128, SMAX + 2 * PAD], BF16)
            vp, b_vp = T("vp", [128, SMAX + 2 * PAD], BF16)
            k.op(pool, lambda e: e.memset(kp[:], 0.0), W=[b_kp])
            k.op(pool, lambda e: e.memset(vp[:], 0.0), W=[b_vp])
            NVT = 17
            vts = [T(f"vt{i}", [128, 2, 65], BF16) for i in range(NVT + 2)]
            for i, (vt, bvt) in enumerate(vts):
                k.op(pool, lambda e: e.memset(vt[:], 1.0), W=[bvt])
            k.op(pool, lambda e: e.memset(vts[NVT][0][0:64, :, :], 0.0), W=[vts[NVT][1]])
            k.op(pool, lambda e: e.memset(vts[NVT + 1][0][64:128, :, :], 0.0), W=[vts[NVT + 1][1]])
            ef = [T(f"ef{i}", [128, 512], F32) for i in range(2)]
            ex = [T(f"ex{i}", [128, 512], BF16) for i in range(2)]
            acc, b_acc = T("acc", [65, 2, 2048], F32)
            ao, b_ao = T("ao", [64, 2, 2048], BF16)

            NSB = S // 2048
            for hp in range(4):
                k.dma(sp, qp[:, 0:S], qT_d[hp * 128:(hp + 1) * 128, 0:S], R=[b_qT], W=[b_qp])
                k.op(pool, lambda e: e.memset(kp[:, PAD + S:PAD + S + PAD], 0.0), W=[b_kp])
                k.op(pool, lambda e: e.memset(vp[:, PAD + S:PAD + S + PAD], 0.0), W=[b_vp])
                k.dma(sp, kp[:, PAD:PAD + S], kT_d[hp * 128:(hp + 1) * 128, 0:S], R=[b_kT], W=[b_kp])
                k.dma(sp, vp[:, PAD:PAD + S], vT_d[hp * 128:(hp + 1) * 128, 0:S], R=[b_vT], W=[b_vp])
                for sbk in range(NSB):
                    q0 = sbk * 2048
                    for p, d in enumerate(DILS):
                        nq = 2048 // d // 128
                        for r in range(d):
                            def kslice(j):
                                ks = PAD + q0 + r + d * (128 * j - 64)
                                return slice(ks, ks + 127 * d + 1, d)

                            def vtile(j):
                                if sbk == 0 and j == 0:
                                    return vts[NVT]
                                if sbk == NSB - 1 and j == nq:
                                    return vts[NVT + 1]
                                return vts[j]

                            for j in range(nq + 1):
                                ph, bph = PH[cnt["ph"] % 2]
                                cnt["ph"] += 1
                                k.op(pe, lambda e: e.transpose(out=ph[:, 0:128], in_=vp[:, kslice(j)], identity=ident[:]), R=[b_vp, b_id], W=[bph])
                                vt, bvt = vtile(j)
                                if j % 2 == 0:
                                    k.op(act, lambda e: e.copy(out=vt[:, :, 0:64], in_=ph[:, 0:128].rearrange("p (h e) -> p h e", h=2)), R=[bph], W=[bvt])
                                else:
                                    k.op(dve, lambda e: e.tensor_copy(out=vt[:, :, 0:64], in_=ph[:, 0:128].rearrange("p (h e) -> p h e", h=2)), R=[bph], W=[bvt])
                            for qi in range(nq):
                                qs = q0 + r + d * 128 * qi
                                qsl = slice(qs, qs + 127 * d + 1, d)
                                pS, bpS = PB[cnt["pb"] % 2]
                                cnt["pb"] += 1
                                for h in range(2):
                                    for jj in range(2):
                                        c0 = (h * 2 + jj) * 128
                                        k.op(pe, lambda e: e.matmul(pS[:, c0:c0 + 128], lhsT=kp[h * 64:(h + 1) * 64, kslice(qi + jj)], rhs=qp[h * 64:(h + 1) * 64, qsl], start=True, stop=True), R=[b_kp, b_qp], W=[bpS])
                                i = cnt["ev"] % 2
                                cnt["ev"] += 1
                                eft, beft = ef[i]
                                ext, bext = ex[i]
                                k.op(dve, lambda e: e.scalar_tensor_tensor(out=eft[:], in0=pS[:, :], scalar=0.125, in1=biasT[:, hp * 3 + p, :], op0=ALU.mult, op1=ALU.add), R=[bpS, b_bias], W=[beft])
                                k.op(act, lambda e: e.activation(out=ext[:], in_=eft[:], func=AF.Exp), R=[beft], W=[bext])
                                pO, bpO = PB[2 + i]
                                for h in range(2):
                                    for jj in range(2):
                                        c0 = (h * 2 + jj) * 128
                                        vt, bvt = vtile(qi + jj)
                                        k.op(pe, lambda e: e.matmul(pO[0:65, h * 128:(h + 1) * 128], lhsT=vt[:, h, :], rhs=ext[:, c0:c0 + 128], start=(jj == 0), stop=(jj == 1)), R=[bvt, bext], W=[bpO])
                                asl = slice(qs - q0, qs - q0 + 127 * d + 1, d)
                                pOv = pO[0:65, 0:256].rearrange("p (h q) -> p h q", h=2)
                                if p == 0:
                                    k.op(act, lambda e: e.copy(out=acc[:, :, asl], in_=pOv), R=[bpO], W=[b_acc])
                                else:
                                    k.op(dve, lambda e: e.tensor_tensor(out=acc[:, :, asl], in0=acc[:, :, asl], in1=pOv, op=ALU.add), R=[bpO, b_acc], W=[b_acc])
                    k.op(dve, lambda e: e.reciprocal(out=acc[64:65, :, :], in_=acc[64:65, :, :]), R=[b_acc], W=[b_acc])
                    for h in range(2):
                        for c in range(4):
                            pBt, bpB = PB[4 + c % 2]
                            k.op(pe, lambda e: e.matmul(pBt[0:64, :], lhsT=onesf[64:65, 0:64], rhs=acc[64:65, h, c * 512:(c + 1) * 512], start=True, stop=True), R=[b_ones, b_acc], W=[bpB])
                            k.op(dve, lambda e: e.tensor_tensor(out=ao[:, h, c * 512:(c + 1) * 512], in0=acc[0:64, h, c * 512:(c + 1) * 512], in1=pBt[0:64, :], op=ALU.mult), R=[b_acc, bpB], W=[b_ao])
                    k.dma(sp, at_d[hp * 2:hp * 2 + 2, :, q0:q0 + 2048].rearrange("h e t -> e h t"), ao[:], R=[b_ao], W=[b_at])

        if upto == 2:
            return done()
        with k.scope():
            xin = [T(f"xin{i}", [128, 8, 132], F32) for i in range(2)]
            cv, b_cv = T("cv", [128, 8, 128], F32)
            xbf, b_xbf = T("xbf", [128, 8, 128], BF16)
            xs_tok, b_xst = T("xs_tok", [128, 512], BF16)
            b_tok, b_btok = T("b_tok", [128, 2, 128], BF16)
            dtr, b_dtr = T("dtr", [128, 16], F32)
            dtv, b_dtv = T("dtv", [128, 16], F32)
            av, b_av = T("av", [128, 16], F32)
            cum, b_cum = T("cum", [128, 8], F32)
            ncum, b_ncum = T("ncum", [128, 8], F32)
            dsv, b_dsv = T("dsv", [128, 8], F32)
            Ev, b_Ev = T("Ev", [128, 8], F32)
            cdv, b_cdv = T("cdv", [128, 8], F32)
            xdt, b_xdt = T("xdt", [128, 8, 64], BF16)
            xdd, b_xdd = T("xdd", [128, 8, 64], BF16)
            cbs, b_cbs = T("cbs", [128, 2, 128], F32)
            abc = [T(f"abc{i}", [128, 128], F32) for i in range(2)]
            dec = [T(f"dec{i}", [128, 128], F32) for i in range(2)]
            Gb = [T(f"Gb{i}", [128, 128], BF16) for i in range(2)]
            Hf, b_Hf = T("Hf", [128, 8, 64], F32)
            Hb, b_Hb = T("Hb", [128, 8, 64], BF16)
            ytmp, b_ytmp = T("ytmp", [128, 512], F32)
            ydir, b_ydir = T("ydir", [128, 512], F32)
            yfw, b_yfw = T("yfw", [128, 512], F32)
            zt, b_zt = T("zt", [128, 512], F32)
            ssd_bf, b_ssdbf = T("ssd_bf", [128, 512], BF16)
            ssdT, b_ssdT = T("ssdT", [128, 4, 128], BF16)
            at_sb, b_atsb = T("at_sb", [64, 8, 128], BF16)
            x1t, b_x1t = T("x1t", [128, D], F32)
            hnT, b_hnT = T("hnT", [128, 8, 128], BF16)
            lg, b_lg = T("lg", [128, 16], F32)
            afft, b_afft = T("afft", [128, 16], F32)
            sm, b_sm = T("sm", [128, 8], F32)

            NCH = S // 128
            for direction in range(2):
                k.op(pool, lambda e: e.memset(Hf[:], 0.0), W=[b_Hf])
                k.op(pool, lambda e: e.memset(Hb[:], 0.0), W=[b_Hb])
                tri, b_tri = (triU, b_triU) if direction == 0 else (triL, b_triL)
                nm, b_nm = (nmU, b_nmU) if direction == 0 else (nmL, b_nmL)
                order = range(NCH) if direction == 0 else range(NCH - 1, -1, -1)
                dc = direction * 8
                for ci, c in enumerate(order):
                    t0 = c * 128
                    xi, bxi = xin[ci % 2]
                    k.dma(sp, xi[:], xbc_d[:, t0:t0 + 132].rearrange("(c p) t -> p c t", p=128), R=[b_xbc], W=[bxi])
                    k.dma(sp, dtr[:], dt_d[t0:t0 + 128, :], R=[b_dt], W=[b_dtr])
                    if upto == 30:
                        return done()
                    for cc in range(8):
                        k.op(dve, lambda e: e.tensor_scalar(out=cv[:, cc, :], in0=xi[:, cc, 0:128], scalar1=cw[:, 0, cc:cc + 1], scalar2=None, op0=ALU.mult), R=[bxi, b_cw], W=[b_cv])
                        for kk in range(1, 5):
                            k.op(dve, lambda e: e.scalar_tensor_tensor(out=cv[:, cc, :], in0=xi[:, cc, kk:kk + 128], scalar=cw[:, kk, cc:cc + 1], in1=cv[:, cc, :], op0=ALU.mult, op1=ALU.add), R=[bxi, b_cw, b_cv], W=[b_cv])
                        k.op(act, lambda e: e.activation(out=xbf[:, cc, :], in_=cv[:, cc, :], func=AF.Silu, bias=cb[:, cc:cc + 1]), R=[b_cv, b_cb], W=[b_xbf])
                    if upto == 31:
                        return done()
                    ph, bph = PH[cnt["ph"] % 2]
                    cnt["ph"] += 1
                    for cc in range(6):
                        k.op(pe, lambda e: e.transpose(out=ph[:, cc * 128:(cc + 1) * 128], in_=xbf[:, cc, :], identity=ident[:]), R=[b_xbf, b_id], W=[bph])
                    k.op(act, lambda e: e.copy(out=xs_tok[:], in_=ph[:, 0:512]), R=[bph], W=[b_xst])
                    k.op(dve, lambda e: e.tensor_copy(out=b_tok[:], in_=ph[:, 512:768].rearrange("p (g n) -> p g n", g=2)), R=[bph], W=[b_btok])
                    if upto == 32:
                        return done()
                    k.op(dve, lambda e: e.tensor_tensor(out=dtv[:], in0=dtr[:], in1=dtb[:], op=ALU.add), R=[b_dtr, b_dtb], W=[b_dtv])
                    k.op(act, lambda e: e.activation(out=dtv[:], in_=dtv[:], func=AF.Exp), R=[b_dtv], W=[b_dtv])
                    k.op(act, lambda e: e.activation(out=dtv[:], in_=dtv[:], func=AF.Ln, bias=1.0), R=[b_dtv], W=[b_dtv])
                    k.op(dve, lambda e: e.tensor_tensor(out=av[:], in0=dtv[:], in1=Abc[:], op=ALU.mult), R=[b_dtv, b_A], W=[b_av])
                    if upto == 3:
                        return done()
                    pC, bpC = PB[0]
                    k.op(pe, lambda e: e.matmul(pC[:, 0:8], lhsT=tri[:, :], rhs=av[:, dc:dc + 8], start=True, stop=True), R=[b_tri, b_av], W=[bpC])
                    k.op(pe, lambda e: e.matmul(pC[:, 8:16], lhsT=onesf[:, :], rhs=av[:, dc:dc + 8], start=True, stop=True), R=[b_ones, b_av], W=[bpC])
                    k.op(dve, lambda e: e.tensor_copy(out=cum[:], in_=pC[:, 0:8]), R=[bpC], W=[b_cum])
                    k.op(dve, lambda e: e.tensor_scalar(out=ncum[:], in0=pC[:, 0:8], scalar1=-1.0, scalar2=None, op0=ALU.mult), R=[bpC], W=[b_ncum])
                    k.op(dve, lambda e: e.tensor_tensor(out=dsv[:], in0=pC[:, 8:16], in1=cum[:], op=ALU.subtract), R=[bpC, b_cum], W=[b_dsv])
                    k.op(act, lambda e: e.activation(out=dsv[:], in_=dsv[:], func=AF.Exp), R=[b_dsv], W=[b_dsv])
                    k.op(act, lambda e: e.activation(out=Ev[:], in_=cum[:], func=AF.Exp), R=[b_cum], W=[b_Ev])
                    k.op(act, lambda e: e.activation(out=cdv[:], in_=pC[:, 8:16], func=AF.Exp), R=[bpC], W=[b_cdv])
                    xs3 = xs_tok[:, :].rearrange("p (h e) -> p h e", h=8)
                    k.op(dve, lambda e: e.tensor_tensor(out=xdt[:], in0=xs3, in1=dtv[:, dc:dc + 8].unsqueeze(2).to_broadcast([128, 8, 64]), op=ALU.mult), R=[b_xst, b_dtv], W=[b_xdt])
                    k.op(dve, lambda e: e.tensor_tensor(out=xdd[:], in0=xdt[:], in1=dsv[:, :].unsqueeze(2).to_broadcast([128, 8, 64]), op=ALU.mult), R=[b_xdt, b_dsv], W=[b_xdd])
                    if upto == 4:
                        return done()
                    pCB, bpCB = PB[1]
                    for g in range(2):
                        k.op(pe, lambda e: e.matmul(pCB[:, g * 128:(g + 1) * 128], lhsT=xbf[:, 4 + g, :], rhs=xbf[:, 6 + g, :], start=True, stop=True), R=[b_xbf], W=[bpCB])
                    k.op(act, lambda e: e.copy(out=cbs[:], in_=pCB[:, 0:256].rearrange("p (g l) -> p g l", g=2)), R=[bpCB], W=[b_cbs])
                    pY, bpY = PB[2]
                    pYo, bpYo = PB[3]
                    for h in range(8):
                        ab, bab = abc[h % 2]
                        de, bde = dec[h % 2]
                        G, bG = Gb[h % 2]
                        pD, bpD = PB[4 + h % 2]
                        k.op(pool, lambda e: e.tensor_copy(out=ab[:], in_=av[:, dc + h:dc + h + 1].to_broadcast([128, 128])), R=[b_av], W=[bab])
                        k.op(pe, lambda e: e.matmul(pD[:, 0:128], lhsT=ab[:, :], rhs=tri[:, :], start=True, stop=False), R=[bab, b_tri], W=[bpD])
                        k.op(pe, lambda e: e.matmul(pD[:, 0:128], lhsT=identf[:, :], rhs=nm[:, :], start=False, stop=True), R=[b_idf, b_nm], W=[bpD])
                        k.op(act, lambda e: e.activation(out=de[:], in_=pD[:, 0:128], func=AF.Exp, bias=ncum[:, h:h + 1]), R=[bpD, b_ncum], W=[bde])
                        k.op(dve, lambda e: e.tensor_tensor(out=G[:], in0=de[:], in1=cbs[:, h // 4, :], op=ALU.mult), R=[bde, b_cbs], W=[bG])
                        k.op(pe, lambda e: e.matmul(pY[:, h * 64:(h + 1) * 64], lhsT=G[:, :], rhs=xdt[:, h, :], start=True, stop=True), R=[bG, b_xdt], W=[bpY])
                    for g in range(2):
                        k.op(pe, lambda e: e.matmul(pYo[:, g * 256:(g + 1) * 256], lhsT=xbf[:, 6 + g, :], rhs=Hb[:, g * 4:(g + 1) * 4, :].rearrange("p h e -> p (h e)"), start=True, stop=True), R=[b_xbf, b_Hb], W=[bpYo])
                    k.op(dve, lambda e: e.tensor_tensor(out=ytmp[:, :].rearrange("p (h e) -> p h e", h=8), in0=pYo[:, :].rearrange("p (h e) -> p h e", h=8), in1=Ev[:, :].unsqueeze(2).to_broadcast([128, 8, 64]), op=ALU.mult), R=[bpYo, b_Ev], W=[b_ytmp])
                    k.op(dve, lambda e: e.tensor_tensor(out=ydir[:], in0=ytmp[:], in1=pY[:, :], op=ALU.add), R=[b_ytmp, bpY], W=[b_ydir])
                    if upto == 5:
                        return done()
                    pSt, bpSt = PB[0]
                    for g in range(2):
                        k.op(pe, lambda e: e.matmul(pSt[:, g * 256:(g + 1) * 256], lhsT=b_tok[:, g, :], rhs=xdd[:, g * 4:(g + 1) * 4, :].rearrange("p h e -> p (h e)"), start=True, stop=True), R=[b_btok, b_xdd], W=[bpSt])
                    k.op(dve, lambda e: e.tensor_tensor(out=Hf[:], in0=Hf[:], in1=cdv[:, :].unsqueeze(2).to_broadcast([128, 8, 64]), op=ALU.mult), R=[b_Hf, b_cdv], W=[b_Hf])
                    k.op(dve, lambda e: e.tensor_tensor(out=Hf[:], in0=Hf[:], in1=pSt[:, :].rearrange("p (h e) -> p h e", h=8), op=ALU.add), R=[b_Hf, bpSt], W=[b_Hf])
                    k.op(pool, lambda e: e.tensor_copy(out=Hb[:], in_=Hf[:]), R=[b_Hf], W=[b_Hb])
                    if direction == 0:
                        k.dma(sp, yf_d[t0:t0 + 128, :], ydir[:], R=[b_ydir], W=[b_yf])
                        continue
                    if upto == 6:
                        return done()
                    k.dma(sp, yfw[:], yf_d[t0:t0 + 128, :], R=[b_yf], W=[b_yfw])
                    k.dma(sp, zt[:], z_d[t0:t0 + 128, :], R=[b_z], W=[b_zt])
                    k.dma(sp, at_sb[:], at_d[:, :, t0:t0 + 128].rearrange("h e t -> e h t"), R=[b_at], W=[b_atsb])
                    xtile, bx = xt[ci % 2]
                    k.dma(sp, xtile[:], x_in[row0 + t0:row0 + t0 + 128, :], W=[bx])
                    k.op(dve, lambda e: e.tensor_tensor(out=ydir[:], in0=ydir[:], in1=yfw[:], op=ALU.add), R=[b_ydir, b_yfw], W=[b_ydir])
                    k.op(pool, lambda e: e.tensor_tensor(out=ytmp[:, :].rearrange("p (h e) -> p h e", h=8), in0=xs3, in1=dsk[:, :].unsqueeze(2).to_broadcast([128, 8, 64]), op=ALU.mult), R=[b_xst, b_dsk], W=[b_ytmp])
                    k.op(dve, lambda e: e.tensor_tensor(out=ydir[:], in0=ydir[:], in1=ytmp[:], op=ALU.add), R=[b_ydir, b_ytmp], W=[b_ydir])
                    k.op(act, lambda e: e.activation(out=zt[:], in_=zt[:], func=AF.Silu), R=[b_zt], W=[b_zt])
                    k.op(dve, lambda e: e.tensor_tensor(out=ydir[:], in0=ydir[:], in1=zt[:], op=ALU.mult), R=[b_ydir, b_zt], W=[b_ydir])
                    for g in range(2):
                        k.op(act, lambda e: e.activation(out=junk[:, g * 256:(g + 1) * 256], in_=ydir[:, g * 256:(g + 1) * 256], func=AF.Square, accum_out=sm[:, g:g + 1]), R=[b_ydir], W=[b_junk, b_sm])
                    k.op(dve, lambda e: e.tensor_scalar(out=sm[:, 2:4], in0=sm[:, 0:2], scalar1=1.0 / 256, scalar2=EPS, op0=ALU.mult, op1=ALU.add), R=[b_sm], W=[b_sm])
                    k.op(act, lambda e: e.activation(out=sm[:, 4:6], in_=sm[:, 2:4], func=AF.Sqrt), R=[b_sm], W=[b_sm])
                    k.op(dve, lambda e: e.reciprocal(out=sm[:, 6:8], in_=sm[:, 4:6]), R=[b_sm], W=[b_sm])
                    for g in range(2):
                        k.op(dve, lambda e: e.scalar_tensor_tensor(out=ssd_bf[:, g * 256:(g + 1) * 256], in0=ydir[:, g * 256:(g + 1) * 256], scalar=sm[:, 6 + g:7 + g], in1=gssd[:, g * 256:(g + 1) * 256], op0=ALU.mult, op1=ALU.mult), R=[b_ydir, b_sm, b_gssd], W=[b_ssdbf])
                    ph, bph = PH[cnt["ph"] % 2]
                    cnt["ph"] += 1
                    for cc in range(4):
                        k.op(pe, lambda e: e.transpose(out=ph[:, cc * 128:(cc + 1) * 128], in_=ssd_bf[:, cc * 128:(cc + 1) * 128], identity=ident[:]), R=[b_ssdbf, b_id], W=[bph])
                    k.op(act, lambda e: e.copy(out=ssdT[:], in_=ph[:, 0:512].rearrange("p (c t) -> p c t", c=4)), R=[bph], W=[b_ssdT])
                    for half in range(2):
                        pX, bpX = PB[4 + half]
                        hs = slice(half * 512, (half + 1) * 512)
                        for h in range(8):
                            k.op(pe, lambda e: e.matmul(pX[:, :], lhsT=at_sb[:, h, :], rhs=wo_att[:, h, hs], start=(h == 0), stop=False), R=[b_atsb, b_woa], W=[bpX])
                        for cc in range(4):
                            k.op(pe, lambda e: e.matmul(pX[:, :], lhsT=ssdT[:, cc, :], rhs=wo_ssd[:, cc, hs], start=False, stop=(cc == 3)), R=[b_ssdT, b_wos], W=[bpX])
                        k.op(dve, lambda e: e.tensor_tensor(out=x1t[:, hs], in0=xtile[:, hs], in1=pX[:, :], op=ALU.add), R=[bx, bpX], W=[b_x1t])
                    k.dma(sp, x1_out[row0 + t0:row0 + t0 + 128, :], x1t[:], R=[b_x1t], W=[b_x1])
                    rmsnorm_to_bf(x1t[:], b_x1t, gffn[:], b_gffn, hb[:], b_hb)
                    ph, bph = PH[cnt["ph"] % 2]
                    cnt["ph"] += 1
                    for cc in range(8):
                        k.op(pe, lambda e: e.transpose(out=ph[:, cc * 128:(cc + 1) * 128], in_=hb[:, cc * 128:(cc + 1) * 128], identity=ident[:]), R=[b_hb, b_id], W=[bph])
                    k.op(act, lambda e: e.copy(out=hnT[:], in_=ph[:, :].rearrange("p (c t) -> p c t", c=8)), R=[bph], W=[b_hnT])
                    pR, bpR = PB[1]
                    for kc in range(8):
                        k.op(pe, lambda e: e.matmul(pR[:, 0:16], lhsT=hnT[:, kc, :], rhs=wr_bf[:, kc, :], start=(kc == 0), stop=(kc == 7)), R=[b_hnT, b_wr], W=[bpR])
                    k.op(dve, lambda e: e.tensor_reduce(out=sm[:, 0:1], in_=pR[:, 0:16], axis=AX.X, op=ALU.max, negate=True), R=[bpR], W=[b_sm])
                    k.op(act, lambda e: e.activation(out=lg[:], in_=pR[:, 0:16], func=AF.Exp, bias=sm[:, 0:1], accum_out=sm[:, 1:2]), R=[bpR, b_sm], W=[b_lg, b_sm])
                    k.op(dve, lambda e: e.reciprocal(out=sm[:, 2:3], in_=sm[:, 1:2]), R=[b_sm], W=[b_sm])
                    k.op(dve, lambda e: e.tensor_scalar(out=afft[:], in0=lg[:], scalar1=sm[:, 2:3], scalar2=None, op0=ALU.mult), R=[b_lg, b_sm], W=[b_afft])
                    k.dma(sp, aff_out[row0 + t0:row0 + t0 + 128, :], afft[:], R=[b_afft], W=[b_aff])

    k.finish([b_x1, b_aff])
    return nc, k


def build_b(NT, TG, n_exp=16, FF=2816):
    nc, k = _build_b(NT, TG, n_exp, FF)
    k.close()
    return nc, k.n_ins


def _build_b(NT, TG, n_exp, FF):
    nc = bass.Bass("TRN2", target_bir_lowering=False)
    k = K(nc)
    sp, act, dve, pool, pe = k.sp, k.act, k.dve, k.pool, k.pe
    NTL = NT // 128
    NFC = FF // 128
    CAP = TG // 8
    TPP = TG // 128

    def din(name, shape):
        return nc.dram_tensor(name, list(shape), F32, kind="ExternalInput").ap()

    x1_in = din("x1", [NT, D])
    aff_all = din("aff_all", [2, TG, 16])
    aff_loc = din("aff_loc", [NT, 16])
    p_in = din("p", [NT, 256])
    w_gate = din("w_gate", [n_exp, D, FF])
    w_up = din("w_up", [n_exp, D, FF])
    w_down = din("w_down", [n_exp, FF, D])
    g_ffn = din("g_ffn", [1, D])
    g_pg = din("g_pg", [1, D])
    g_ple = din("g_ple", [1, D])
    g_fin = din("g_final", [1, D])
    w_pg = din("w_pg", [D, D])
    w_ple = din("w_ple", [256, D])
    y_out = nc.dram_tensor("y", [NT, D], F32, kind="ExternalOutput").ap()
    b_y = Buf("y")
    hnT_d = nc.dram_tensor("hnT_d", [D, NT], BF16).ap()
    b_hnT_d = Buf("hnT_d")
    yacc_d = nc.dram_tensor("yacc_d", [NT, D], F32).ap()
    NTB = NT // 512
    b_yacc = [Buf(f"yacc{i}") for i in range(NTB)]

    def T(name, shape, dt):
        return k.sb(name, shape, dt), Buf(name)

    PB = [(k.ps(f"pb{i}", [128, 512], F32), Buf(f"pb{i}", excl=True)) for i in range(6)]
    PH = [(k.ps(f"ph{i}", [128, 1024], BF16), Buf(f"ph{i}", excl=True)) for i in range(2)]

    onesf, b_ones = T("onesf", [128, 128], F32)
    identf, b_idf = T("identf", [128, 128], F32)
    ident, b_id = T("ident", [128, 128], BF16)
    k.op(pool, lambda e: e.memset(onesf[:], 1.0), W=[b_ones])
    k.op(pool, lambda e: e.memset(identf[:], 0.0), W=[b_idf])
    k.op(pool, lambda e: e.affine_select(out=identf[:], in_=identf[:], pattern=[[-1, 128]], compare_op=ALU.not_equal, fill=1.0, base=0, channel_multiplier=1), R=[b_idf], W=[b_idf])
    k.op(dve, lambda e: e.tensor_copy(out=ident[:], in_=identf[:]), R=[b_idf], W=[b_id])
    gffn, b_gffn = T("gffn", [128, D], F32)
    k.dma(sp, gffn[:], g_ffn[0:1, :].to_broadcast([128, D]), W=[b_gffn])
    thr, b_thr = T("thr", [128, 32], F32)
    gm, b_gm = T("gm", [128, NTL, 16], F32)
    xt = [T(f"xt{i}", [128, D], F32) for i in range(1)]
    junk, b_junk = T("junk", [128, D], F32)
    st_s, b_sts = T("st_s", [128, 8], F32)
    hb, b_hb = T("hb", [128, D], BF16)
    hnT, b_hnT = T("hnT", [128, 8, 128], BF16)
    stg = [T(f"stg{i}", [128, 1024], F32) for i in range(2)]

    def rmsnorm(xtile, bx, gtile, bg, out, bout, n=D):
        k.op(act, lambda e: e.activation(out=junk[:, 0:n], in_=xtile, func=AF.Square, accum_out=st_s[:, 0:1]), R=[bx], W=[b_junk, b_sts])
        k.op(dve, lambda e: e.tensor_scalar(out=st_s[:, 1:2], in0=st_s[:, 0:1], scalar1=1.0 / n, scalar2=EPS, op0=ALU.mult, op1=ALU.add), R=[b_sts], W=[b_sts])
        k.op(act, lambda e: e.activation(out=st_s[:, 2:3], in_=st_s[:, 1:2], func=AF.Sqrt), R=[b_sts], W=[b_sts])
        k.op(dve, lambda e: e.reciprocal(out=st_s[:, 3:4], in_=st_s[:, 2:3]), R=[b_sts], W=[b_sts])
        k.op(dve, lambda e: e.scalar_tensor_tensor(out=out, in0=xtile, scalar=st_s[:, 3:4], in1=gtile, op0=ALU.mult, op1=ALU.mult), R=[bx, b_sts, bg], W=[bout])

    with k.scope():
        aff, b_affs = T("aff", [128, 2, TPP, 16], F32)
        cmp_, b_cmp = T("cmp", [128, 2, TPP, 16], BF16)
        cntt, b_cnt = T("cntt", [128, 32], F32)
        lo, b_lo = T("lo", [128, 32], F32)
        hi, b_hi = T("hi", [128, 32], F32)
        mid, b_mid = T("mid", [128, 32], F32)
        ge, b_ge = T("ge", [128, 32], F32)
        d1, b_d1 = T("d1", [128, 32], F32)
        for g in range(2):
            k.dma(sp, aff[:, g, :, :], aff_all[g].rearrange("(p t) e -> p t e", p=128), W=[b_affs])
        k.op(pool, lambda e: e.memset(lo[:], 0.0), W=[b_lo])
        k.op(pool, lambda e: e.memset(hi[:], 1.0), W=[b_hi])
        k.op(pool, lambda e: e.memset(mid[:], 0.5), W=[b_mid])
        for it in range(32):
            mb = mid[:, :].rearrange("p (g e) -> p g e", g=2).unsqueeze(2).to_broadcast([128, 2, TPP, 16])
            k.op(dve, lambda e: e.tensor_tensor(out=cmp_[:], in0=aff[:], in1=mb, op=ALU.is_ge), R=[b_affs, b_mid], W=[b_cmp])
            k.op(dve, lambda e: e.tensor_reduce(out=cntt[:, :].rearrange("p (g e) -> p g e", g=2), in_=cmp_[:].rearrange("p g t e -> p g e t"), axis=AX.X, op=ALU.add), R=[b_cmp], W=[b_cnt])
            pC, bpC = PB[it % 2]
            k.op(pe, lambda e: e.matmul(pC[:, 0:32], lhsT=onesf[:, :], rhs=cntt[:, :], start=True, stop=True), R=[b_ones, b_cnt], W=[bpC])
            k.op(dve, lambda e: e.tensor_scalar(out=ge[:], in0=pC[:, 0:32], scalar1=CAP - 0.5, scalar2=None, op0=ALU.is_ge), R=[bpC], W=[b_ge])
            k.op(dve, lambda e: e.tensor_tensor(out=d1[:], in0=mid[:], in1=lo[:], op=ALU.subtract), R=[b_mid, b_lo], W=[b_d1])
            k.op(dve, lambda e: e.tensor_tensor(out=d1[:], in0=d1[:], in1=ge[:], op=ALU.mult), R=[b_d1, b_ge], W=[b_d1])
            k.op(dve, lambda e: e.tensor_tensor(out=lo[:], in0=lo[:], in1=d1[:], op=ALU.add), R=[b_lo, b_d1], W=[b_lo])
            k.op(dve, lambda e: e.tensor_tensor(out=d1[:], in0=hi[:], in1=mid[:], op=ALU.subtract), R=[b_hi, b_mid], W=[b_d1])
            k.op(dve, lambda e: e.tensor_tensor(out=d1[:], in0=d1[:], in1=ge[:], op=ALU.mult), R=[b_d1, b_ge], W=[b_d1])
            k.op(dve, lambda e: e.tensor_tensor(out=hi[:], in0=mid[:], in1=d1[:], op=ALU.add), R=[b_mid, b_d1], W=[b_hi])
            k.op(dve, lambda e: e.tensor_tensor(out=mid[:], in0=lo[:], in1=hi[:], op=ALU.add), R=[b_lo, b_hi], W=[b_mid])
            k.op(dve, lambda e: e.tensor_scalar(out=mid[:], in0=mid[:], scalar1=0.5, scalar2=None, op0=ALU.mult), R=[b_mid], W=[b_mid])
        k.op(dve, lambda e: e.tensor_copy(out=thr[:], in_=lo[:]), R=[b_lo], W=[b_thr])

    afl, b_afl = T("afl", [128, 16], F32)
    msk, b_msk = T("msk", [128, 16], F32)
    cnt = {"ph": 0}
    for t in range(NTL):
        g = 0 if t < NTL // 2 else 1
        xtile, bx = xt[0]
        k.dma(sp, xtile[:], x1_in[t * 128:(t + 1) * 128, :], W=[bx])
        k.dma(sp, yacc_d[t * 128:(t + 1) * 128, :], xtile[:], R=[bx], W=[b_yacc[t // 4]])
        k.dma(sp, afl[:], aff_loc[t * 128:(t + 1) * 128, :], W=[b_afl])
        k.op(dve, lambda e: e.tensor_tensor(out=msk[:], in0=afl[:], in1=thr[:, g * 16:(g + 1) * 16], op=ALU.is_ge), R=[b_afl, b_thr], W=[b_msk])
        k.op(dve, lambda e: e.tensor_tensor(out=gm[:, t, :], in0=afl[:], in1=msk[:], op=ALU.mult), R=[b_afl, b_msk], W=[b_gm])
        rmsnorm(xtile[:], bx, gffn[:], b_gffn, hb[:], b_hb)
        ph, bph = PH[cnt["ph"] % 2]
        cnt["ph"] += 1
        for c in range(8):
            k.op(pe, lambda e: e.transpose(out=ph[:, c * 128:(c + 1) * 128], in_=hb[:, c * 128:(c + 1) * 128], identity=ident[:]), R=[b_hb, b_id], W=[bph])
        k.op(act, lambda e: e.copy(out=hnT[:], in_=ph[:, :].rearrange("p (c t) -> p c t", c=8)), R=[bph], W=[b_hnT])
        k.dma(sp, hnT_d[:, t * 128:(t + 1) * 128].rearrange("(c p) t -> p c t", p=128), hnT[:], R=[b_hnT], W=[b_hnT_d])

    with k.scope():
        wg, b_wg = T("wg", [128, 8, FF], BF16)
        wu, b_wu = T("wu", [128, 8, FF], BF16)
        wd, b_wd = T("wd", [128, NFC, D], BF16)
        hblk = [T(f"hblk{i}", [128, 8, 512], BF16) for i in range(1)]
        actb, b_actb = T("actb", [128, NFC, 512], BF16)
        sg = [T(f"sg{i}", [128, 512], F32) for i in range(1)]
        ye = [T(f"ye{i}", [128, 512], F32) for i in range(1)]
        ya = [T(f"ya{i}", [128, 512], F32) for i in range(1)]
        sc = {"stg": 0, "i": 0}

        def load_cast(dst_ap, src_ap, bdst, c):
            (st, bst) = stg[sc["stg"] % 2]
            sc["stg"] += 1
            sv = st[:, 0:src_ap.shape[1] * src_ap.shape[2]].rearrange("p (c n) -> p c n", c=src_ap.shape[1])
            k.dma(sp, sv, src_ap, W=[bst])
            eng = [dve, pool, act][c % 3]
            if eng is act:
                k.op(act, lambda e: e.copy(out=dst_ap, in_=sv), R=[bst], W=[bdst])
            else:
                k.op(eng, lambda e: e.tensor_copy(out=dst_ap, in_=sv), R=[bst], W=[bdst])

        for ex in range(n_exp):
            c = 0
            for (wsrc, wdst, bw) in ((w_gate, wg, b_wg), (w_up, wu, b_wu)):
                for n0 in range(0, FF, 128):
                    load_cast(wdst[:, :, n0:n0 + 128], wsrc[ex, :, n0:n0 + 128].rearrange("(c p) n -> p c n", p=128), bw, c)
                    c += 1
            for f0 in range(0, NFC):
                load_cast(wd[:, f0:f0 + 1, :], w_down[ex, f0 * 128:(f0 + 1) * 128, :].rearrange("(c p) n -> p c n", p=128), b_wd, c)
                c += 1
            for tb in range(NTB):
                hb_, bhb_ = hblk[0]
                k.dma(sp, hb_[:], hnT_d[:, tb * 512:(tb + 1) * 512].rearrange("(c p) t -> p c t", p=128), R=[b_hnT_d], W=[bhb_])
                for fc in range(NFC):
                    pG, bpG = PB[0 + fc % 2]
                    pU, bpU = PB[2 + fc % 2]
                    for kc in range(8):
                        k.op(pe, lambda e: e.matmul(pG[:, :], lhsT=wg[:, kc, fc * 128:(fc + 1) * 128], rhs=hb_[:, kc, :], start=(kc == 0), stop=(kc == 7)), R=[b_wg, bhb_], W=[bpG])
                    for kc in range(8):
                        k.op(pe, lambda e: e.matmul(pU[:, :], lhsT=wu[:, kc, fc * 128:(fc + 1) * 128], rhs=hb_[:, kc, :], start=(kc == 0), stop=(kc == 7)), R=[b_wu, bhb_], W=[bpU])
                    s_, bs_ = sg[0]
                    k.op(act, lambda e: e.activation(out=s_[:], in_=pG[:, :], func=AF.Silu), R=[bpG], W=[bs_])
                    k.op(dve, lambda e: e.tensor_tensor(out=actb[:, fc, :], in0=s_[:], in1=pU[:, :], op=ALU.mult), R=[bs_, bpU], W=[b_actb])
                for t in range(4):
                    tl = tb * 4 + t
                    for half in range(2):
                        i = sc["i"] % 2
                        sc["i"] += 1
                        pY, bpY = PB[4 + i]
                        for fc in range(NFC):
                            k.op(pe, lambda e: e.matmul(pY[:, :], lhsT=actb[:, fc, t * 128:(t + 1) * 128], rhs=wd[:, fc, half * 512:(half + 1) * 512], start=(fc == 0), stop=(fc == NFC - 1)), R=[b_actb, b_wd], W=[bpY])
                        yat, byat = ya[0]
                        yet, byet = ye[0]
                        dsl = yacc_d[tl * 128:(tl + 1) * 128, half * 512:(half + 1) * 512]
                        k.dma(sp, yat[:], dsl, R=[b_yacc[tb]], W=[byat])
                        k.op(dve, lambda e: e.scalar_tensor_tensor(out=yet[:], in0=pY[:, :], scalar=gm[:, tl, ex:ex + 1], in1=yat[:], op0=ALU.mult, op1=ALU.add), R=[bpY, b_gm, byat], W=[byet])
                        k.dma(sp, dsl, yet[:], R=[byet], W=[b_yacc[tb]])

    with k.scope():
        wpg, b_wpg = T("wpg", [128, 8, D], BF16)
        wple, b_wple = T("wple", [128, 2, D], BF16)
        gpg, b_gpg = T("gpg", [128, D], F32)
        gple, b_gple = T("gple", [128, D], F32)
        gfin, b_gfin = T("gfin", [128, D], F32)
        for (gt, bg, src) in ((gpg, b_gpg, g_pg), (gple, b_gple, g_ple), (gfin, b_gfin, g_fin)):
            k.dma(sp, gt[:], src[0:1, :].to_broadcast([128, D]), W=[bg])
        for c in range(8):
            st, bst = stg[c % 2]
            k.dma(sp, st[:, 0:D], w_pg[c * 128:(c + 1) * 128, :], W=[bst])
            k.op(dve, lambda e: e.tensor_copy(out=wpg[:, c, :], in_=st[:, 0:D]), R=[bst], W=[b_wpg])
        for c in range(2):
            st, bst = stg[c % 2]
            k.dma(sp, st[:, 0:D], w_ple[c * 128:(c + 1) * 128, :], W=[bst])
            k.op(dve, lambda e: e.tensor_copy(out=wple[:, c, :], in_=st[:, 0:D]), R=[bst], W=[b_wple])
        pt_, b_pt = T("pt", [128, 256], F32)
        pb_, b_pb = T("pbf", [128, 256], BF16)
        pT_, b_pT = T("pT", [128, 2, 128], BF16)
        er, b_er = T("er", [128, D], F32)
        ev, b_ev = T("ev", [128, D], F32)
        gs, b_gs = T("gs", [128, D], F32)
        yt, b_yt = T("yt", [128, D], F32)
        for t in range(NTL):
            xtile, bx = xt[0]
            k.dma(sp, xtile[:], yacc_d[t * 128:(t + 1) * 128, :], R=[b_yacc[t // 4]], W=[bx])
            k.dma(sp, pt_[:], p_in[t * 128:(t + 1) * 128, :], W=[b_pt])
            k.op(pool, lambda e: e.tensor_copy(out=pb_[:], in_=pt_[:]), R=[b_pt], W=[b_pb])
            ph, bph = PH[cnt["ph"] % 2]
            cnt["ph"] += 1
            for c in range(2):
                k.op(pe, lambda e: e.transpose(out=ph[:, c * 128:(c + 1) * 128], in_=pb_[:, c * 128:(c + 1) * 128], identity=ident[:]), R=[b_pb, b_id], W=[bph])
            k.op(act, lambda e: e.copy(out=pT_[:], in_=ph[:, 0:256].rearrange("p (c t) -> p c t", c=2)), R=[bph], W=[b_pT])
            for half in range(2):
                pE, bpE = PB[half]
                for c in range(2):
                    k.op(pe, lambda e: e.matmul(pE[:, :], lhsT=pT_[:, c, :], rhs=wple[:, c, half * 512:(half + 1) * 512], start=(c == 0), stop=(c == 1)), R=[b_pT, b_wple], W=[bpE])
                k.op(act, lambda e: e.copy(out=er[:, half * 512:(half + 1) * 512], in_=pE[:, :]), R=[bpE], W=[b_er])
            rmsnorm(er[:], b_er, gple[:], b_gple, ev[:], b_ev)
            rmsnorm(xtile[:], bx, gpg[:], b_gpg, hb[:], b_hb)
            ph, bph = PH[cnt["ph"] % 2]
            cnt["ph"] += 1
            for c in range(8):
                k.op(pe, lambda e: e.transpose(out=ph[:, c * 128:(c + 1) * 128], in_=hb[:, c * 128:(c + 1) * 128], identity=ident[:]), R=[b_hb, b_id], W=[bph])
            k.op(act, lambda e: e.copy(out=hnT[:], in_=ph[:, :].rearrange("p (c t) -> p c t", c=8)), R=[bph], W=[b_hnT])
            for half in range(2):
                pE, bpE = PB[2 + half]
                for c in range(8):
                    k.op(pe, lambda e: e.matmul(pE[:, :], lhsT=hnT[:, c, :], rhs=wpg[:, c, half * 512:(half + 1) * 512], start=(c == 0), stop=(c == 7)), R=[b_hnT, b_wpg], W=[bpE])
                k.op(act, lambda e: e.activation(out=gs[:, half * 512:(half + 1) * 512], in_=pE[:, :], func=AF.Sigmoid), R=[bpE], W=[b_gs])
            k.op(dve, lambda e: e.tensor_tensor(out=gs[:], in0=gs[:], in1=ev[:], op=ALU.mult), R=[b_gs, b_ev], W=[b_gs])
            k.op(dve, lambda e: e.tensor_tensor(out=gs[:], in0=gs[:], in1=xtile[:], op=ALU.add), R=[b_gs, bx], W=[b_gs])
            rmsnorm(gs[:], b_gs, gfin[:], b_gfin, yt[:], b_yt)
            k.dma(sp, y_out[t * 128:(t + 1) * 128, :], yt[:], R=[b_yt], W=[b_y])
    k.finish([b_y])
    return nc, k


_CACHE = {}


def kernel(x_prompt, x_sample, p_prompt, p_sample, rel_bias, g_mix, w_in, conv_w, conv_b, dt_bias, a_log, d_skip, g_ssd,
           w_out, g_ffn, w_router, w_gate, w_up, w_down, g_pg, w_pg, w_ple, g_ple, g_final):
    f = lambda a: np.ascontiguousarray(np.asarray(a, dtype=np.float32))
    xp, xs_ = f(x_prompt), f(x_sample)
    B, S1, _ = xp.shape
    B2, S2, _ = xs_.shape
    pb, sb_ = B // NCORES, B2 // NCORES
    seqs = []
    r = 0
    for i in range(pb):
        seqs.append((r, S1))
        r += S1
    for i in range(sb_):
        seqs.append((r, S2))
        r += S2
    NT = r
    nca, _ = build_a(seqs)
    common = dict(w_in=f(w_in[0]), w_out=f(w_out[0]), w_router=f(w_router[0]), g_mix=f(g_mix[0])[None], g_ffn=f(g_ffn[0])[None],
                  conv_w=f(conv_w[0]), conv_b=f(conv_b[0])[None], dt_bias=f(dt_bias[0]).reshape(1, 16), a_log=f(a_log[0]).reshape(1, 16),
                  d_skip=f(d_skip[0])[None], g_ssd=f(g_ssd[0])[None], rel_bias=f(rel_bias), oh=_onehot_tables())
    xcore = []
    for c in range(NCORES):
        xcore.append(np.concatenate([xp[c * pb:(c + 1) * pb].reshape(-1, D), xs_[c * sb_:(c + 1) * sb_].reshape(-1, D)], 0))
    ra = run_bass_kernel_spmd(nca, [dict(common, x=xcore[c]) for c in range(NCORES)], core_ids=list(range(NCORES)))
    x1 = [ra.results[c]["x1"] for c in range(NCORES)]
    aff = [ra.results[c]["aff"] for c in range(NCORES)]
    n1 = pb * S1
    aff_all = np.stack([np.concatenate([a[:n1] for a in aff], 0), np.concatenate([a[n1:] for a in aff], 0)], 0)
    TG = aff_all.shape[1]
    assert n1 == NT - n1 and TG == NCORES * n1
    ncb, _ = build_b(NT, TG)
    pp, ps_ = f(p_prompt[0]), f(p_sample[0])
    commonb = dict(aff_all=np.ascontiguousarray(aff_all), w_gate=f(w_gate[0]), w_up=f(w_up[0]), w_down=f(w_down[0]), g_ffn=f(g_ffn[0])[None],
                   g_pg=f(g_pg[0])[None], g_ple=f(g_ple[0])[None], g_final=f(g_final)[None], w_pg=f(w_pg[0]), w_ple=f(w_ple[0]))
    inb = []
    for c in range(NCORES):
        pc = np.concatenate([pp[c * pb:(c + 1) * pb].reshape(-1, 256), ps_[c * sb_:(c + 1) * sb_].reshape(-1, 256)], 0)
        inb.append(dict(commonb, x1=x1[c], aff_loc=aff[c], p=pc))
    rb = run_bass_kernel_spmd(ncb, inb, core_ids=list(range(NCORES)))
    ys = [rb.results[c]["y"] for c in range(NCORES)]
    y_p = np.concatenate([y[:n1] for y in ys], 0).reshape(B, S1, D)
    y_s = np.concatenate([y[n1:] for y in ys], 0).reshape(B2, S2, D)
    return y_p, y_s
```

```python
import math
from contextlib import ExitStack
import numpy as np
import concourse.bass as bass
import concourse.mybir as mybir
from concourse.bass_utils import run_bass_kernel_spmd

F32 = mybir.dt.float32
BF16 = mybir.dt.bfloat16
ALU = mybir.AluOpType
AF = mybir.ActivationFunctionType
AX = mybir.AxisListType

NCORES = 8
D = 1024
PAD = 1024
NEG = -30000.0
EPS = 1e-6
DILS = (1, 4, 16)


class Buf:
    __slots__ = ("name", "w", "r", "excl")

    def __init__(self, name, excl=False):
        self.name = name
        self.excl = excl
        self.w = {}
        self.r = {}


class Sem:
    __slots__ = ("h", "total")

    def __init__(self, h):
        self.h = h
        self.total = 0


class Eng:
    def __init__(self, name, handle, sem):
        self.name = name
        self.h = handle
        self.sem = sem
        self.waited = {}
        self.ring = []
        self.rpos = 0


class K:
    def __init__(self, nc, sp_ring=44, pool_ring=20, act_ring=12):
        self.nc = nc
        self.es = ExitStack()
        self.es_cur = self.es
        self.pe = self._eng("pe", nc.tensor)
        self.act = self._eng("act", nc.scalar)
        self.dve = self._eng("dve", nc.vector)
        self.pool = self._eng("pool", nc.gpsimd)
        self.sp = self._eng("sp", nc.sync)
        for e, n in ((self.sp, sp_ring), (self.pool, pool_ring), (self.act, act_ring)):
            e.ring = [self.new_sem(f"r_{e.name}{i}") for i in range(n)]
        self.n_ins = 0

    def new_sem(self, name):
        return Sem(self.es.enter_context(self.nc.semaphore(name)))

    def _eng(self, name, handle):
        return Eng(name, handle, self.new_sem("e_" + name))

    def sb(self, name, shape, dtype):
        self.uid = getattr(self, "uid", 0) + 1
        return self.es_cur.enter_context(self.nc.sbuf_tensor(f"{name}_{self.uid}", list(shape), dtype))

    def barrier(self):
        if getattr(self, "dead", False):
            return
        engs = [self.pe, self.act, self.dve, self.pool, self.sp]
        for e in engs:
            for o in engs:
                if o is not e and o.sem.total > 0:
                    self._wait(e, o.sem, o.sem.total)
                for s in o.ring:
                    if s.total > 0:
                        self._wait(e, s, s.total)

    def scope(self):
        return _Scope(self)

    def ps(self, name, shape, dtype):
        return self.es.enter_context(self.nc.psum_tensor(name, list(shape), dtype))

    def _wait(self, eng, s, v):
        if eng.waited.get(id(s), 0) < v:
            eng.h.wait_ge(s.h, v)
            eng.waited[id(s)] = v
            self.n_ins += 1

    def _deps(self, eng, reads, writes):
        for b in reads:
            for s, v in b.w.values():
                self._wait(eng, s, v)
        for b in writes:
            for s, v in b.w.values():
                self._wait(eng, s, v)
            for s, v in b.r.values():
                self._wait(eng, s, v)

    def _record(self, ev, reads, writes):
        s, v = ev
        for b in writes:
            b.w[id(s)] = ev
            b.r = {}
        for b in reads:
            b.r[id(s)] = ev

    def op(self, eng, fn, R=(), W=()):
        if getattr(self, "dead", False):
            return None
        ex = [b for b in R if b.excl]
        if ex:
            W = list(W) + ex
        self._deps(eng, R, W)
        ins = fn(eng.h)
        eng.sem.total += 1
        ins.then_inc(eng.sem.h, 1)
        self.n_ins += 1
        self._record((eng.sem, eng.sem.total), R, W)
        return ins

    def dma(self, eng, out, in_, R=(), W=(), **kw):
        if getattr(self, "dead", False):
            return None
        self._deps(eng, R, W)
        s = eng.ring[eng.rpos]
        eng.rpos = (eng.rpos + 1) % len(eng.ring)
        self._wait(eng, s, s.total)
        ins = eng.h.dma_start(out=out, in_=in_, **kw)
        s.total += 16
        ins.then_inc(s.h, 16)
        self.n_ins += 1
        self._record((s, s.total), R, W)
        return ins

    def finish(self, bufs):
        self._deps(self.sp, bufs, [])

    def close(self):
        self.es.close()

    def __del__(self):
        pass


class _Scope:
    def __init__(self, k):
        self.k = k

    def __enter__(self):
        self.k.barrier()
        self.old = self.k.es_cur
        self.es = ExitStack()
        self.es.__enter__()
        self.k.es_cur = self.es
        return self

    def __exit__(self, *a):
        self.k.barrier()
        self.k.es_cur = self.old
        return self.es.__exit__(*a)


def _t5_bucket(rel):
    half = 16
    max_exact = 8
    n = np.abs(rel)
    large = max_exact + (np.log(np.maximum(n, 1) / max_exact) / math.log(1024 / max_exact) * (half - max_exact)).astype(np.int32)
    large = np.minimum(large, half - 1)
    return (np.where(rel > 0, half, 0) + np.where(n < max_exact, n, large)).astype(np.int32)


def _onehot_tables():
    oh = np.zeros((3, 33, 384), np.float32)
    for p, d in enumerate(DILS):
        for i in range(384):
            ds = i - 191
            if abs(ds) <= 64:
                oh[p, int(_t5_bucket(np.array(ds * d))), i] = 1.0
            else:
                oh[p, 32, i] = 1.0
    return oh


def build_a(seqs, dbg=False, upto=9):
    nc, k = _build_a(seqs, dbg, upto)
    k.close()
    return nc, k.n_ins


def _build_a(seqs, dbg=False, upto=9):
    NT = sum(s for _, s in seqs)
    SMAX = max(s for _, s in seqs)
    nc = bass.Bass("TRN2", target_bir_lowering=False)
    k = K(nc)

    def din(name, shape):
        return nc.dram_tensor(name, list(shape), F32, kind="ExternalInput").ap()

    x_in = din("x", [NT, D])
    w_in = din("w_in", [D, 3088])
    w_out = din("w_out", [D, D])
    w_router = din("w_router", [D, 16])
    g_mix = din("g_mix", [1, D])
    g_ffn = din("g_ffn", [1, D])
    conv_w = din("conv_w", [5, 1024])
    conv_b = din("conv_b", [1, 1024])
    dt_bias = din("dt_bias", [1, 16])
    a_log = din("a_log", [1, 16])
    d_skip = din("d_skip", [1, 8])
    g_ssd = din("g_ssd", [1, 512])
    rel_bias = din("rel_bias", [32, 8])
    oh_in = din("oh", [3, 33, 384])
    x1_out = nc.dram_tensor("x1", [NT, D], F32, kind="ExternalOutput").ap()
    aff_out = nc.dram_tensor("aff", [NT, 16], F32, kind="ExternalOutput").ap()
    b_x1 = Buf("x1o")
    b_aff = Buf("affo")

    def dscr(name, shape, dt):
        return nc.dram_tensor(name, list(shape), dt).ap(), Buf(name)

    qT_d, b_qT = dscr("qT_d", [512, SMAX], BF16)
    kT_d, b_kT = dscr("kT_d", [512, SMAX], BF16)
    vT_d, b_vT = dscr("vT_d", [512, SMAX], BF16)
    xbc_d, b_xbc = dscr("xbc_d", [1024, SMAX + 4], F32)
    z_d, b_z = dscr("z_d", [SMAX, 512], F32)
    dt_d, b_dt = dscr("dt_d", [SMAX, 16], F32)
    yf_d, b_yf = dscr("yf_d", [SMAX, 512], F32)
    at_d, b_at = dscr("at_d", [8, 64, SMAX], BF16)
    tab_d_t = nc.dram_tensor("tab_d", [3, 8, 384], F32)
    tab_d = tab_d_t.ap()
    b_tab = Buf("tab_d")

    def T(name, shape, dt):
        return k.sb(name, shape, dt), Buf(name)

    w_in_bf, b_win = T("w_in_bf", [128, 8, 3088], BF16)
    wo_att, b_woa = T("wo_att", [64, 8, 1024], BF16)
    wo_ssd, b_wos = T("wo_ssd", [128, 4, 1024], BF16)
    wr_bf, b_wr = T("wr_bf", [128, 8, 16], BF16)
    gmix, b_gmix = T("gmix", [128, D], F32)
    gffn, b_gffn = T("gffn", [128, D], F32)
    gssd, b_gssd = T("gssd", [128, 512], F32)
    cw, b_cw = T("cw", [128, 5, 8], F32)
    cb, b_cb = T("cb", [128, 8], F32)
    dtb, b_dtb = T("dtb", [128, 16], F32)
    Abc, b_A = T("Abc", [128, 16], F32)
    dsk, b_dsk = T("dsk", [128, 8], F32)
    identf, b_idf = T("identf", [128, 128], F32)
    ident, b_id = T("ident", [128, 128], BF16)
    triU, b_triU = T("triU", [128, 128], F32)
    triL, b_triL = T("triL", [128, 128], F32)
    nmU, b_nmU = T("nmU", [128, 128], F32)
    nmL, b_nmL = T("nmL", [128, 128], F32)
    onesf, b_ones = T("onesf", [128, 128], F32)
    biasT, b_bias = T("biasT", [128, 12, 512], BF16)

    PB = []
    for i in range(6):
        PB.append((k.ps(f"pb{i}", [128, 512], F32), Buf(f"pb{i}", excl=True)))
    PH = []
    for i in range(2):
        PH.append((k.ps(f"ph{i}", [128, 1024], BF16), Buf(f"ph{i}", excl=True)))

    sp, act, dve, pool, pe = k.sp, k.act, k.dve, k.pool, k.pe

    def bc_load(dst, bdst, src_row, n):
        k.dma(sp, dst[:], src_row.to_broadcast([128, n]), W=[bdst])

    bc_load(gmix, b_gmix, g_mix[0:1, :], D)
    bc_load(gffn, b_gffn, g_ffn[0:1, :], D)
    bc_load(gssd, b_gssd, g_ssd[0:1, :], 512)
    bc_load(dtb, b_dtb, dt_bias[0:1, :], 16)
    bc_load(Abc, b_A, a_log[0:1, :], 16)
    bc_load(dsk, b_dsk, d_skip[0:1, :], 8)
    for kk in range(5):
        k.dma(sp, cw[:, kk, :], conv_w[kk:kk + 1, :].rearrange("o (c p) -> p (o c)", p=128), W=[b_cw], allow_slow_non_contiguous=True)
    k.dma(sp, cb[:], conv_b.rearrange("o (c p) -> p (o c)", p=128), W=[b_cb], allow_slow_non_contiguous=True)
    k.op(act, lambda e: e.activation(out=Abc[:], in_=Abc[:], func=AF.Exp), R=[b_A], W=[b_A])
    k.op(dve, lambda e: e.tensor_scalar(out=Abc[:], in0=Abc[:], scalar1=-1.0, scalar2=None, op0=ALU.mult), R=[b_A], W=[b_A])

    k.op(pool, lambda e: e.memset(onesf[:], 1.0), W=[b_ones])
    k.op(pool, lambda e: e.memset(identf[:], 0.0), W=[b_idf])
    k.op(pool, lambda e: e.affine_select(out=identf[:], in_=identf[:], pattern=[[-1, 128]], compare_op=ALU.not_equal, fill=1.0, base=0, channel_multiplier=1), R=[b_idf], W=[b_idf])
    k.op(dve, lambda e: e.tensor_copy(out=ident[:], in_=identf[:]), R=[b_idf], W=[b_id])
    k.op(pool, lambda e: e.affine_select(out=triU[:], in_=onesf[:], pattern=[[1, 128]], compare_op=ALU.is_ge, fill=0.0, base=0, channel_multiplier=-1), R=[b_ones], W=[b_triU])
    k.op(pool, lambda e: e.affine_select(out=triL[:], in_=onesf[:], pattern=[[-1, 128]], compare_op=ALU.is_ge, fill=0.0, base=0, channel_multiplier=1), R=[b_ones], W=[b_triL])
    k.op(dve, lambda e: e.tensor_scalar(out=nmU[:], in0=triU[:], scalar1=-1.0, scalar2=-NEG, op0=ALU.add, op1=ALU.mult), R=[b_triU], W=[b_nmU])
    k.op(dve, lambda e: e.tensor_scalar(out=nmL[:], in0=triL[:], scalar1=-1.0, scalar2=-NEG, op0=ALU.add, op1=ALU.mult), R=[b_triL], W=[b_nmL])

    with k.scope():
        stg, b_stg = T("stg", [128, 2048], F32)
        relx, b_relx = T("relx", [33, 8], F32)
        oh_sb, b_oh = T("oh_sb", [33, 3, 384], F32)
        tab_sb, b_tabsb = T("tab_sb", [8, 3, 384], F32)
        for c0 in range(0, 3088, 256):
            cn = min(256, 3088 - c0)
            sv = stg[:, 0:8 * cn].rearrange("p (c n) -> p c n", c=8)
            k.dma(sp, sv, w_in[:, c0:c0 + cn].rearrange("(c p) n -> p c n", p=128), W=[b_stg])
            k.op(dve, lambda e: e.tensor_copy(out=w_in_bf[:, :, c0:c0 + cn], in_=sv), R=[b_stg], W=[b_win])
        for hh in range(4):
            sv = stg[0:64, :].rearrange("p (h n) -> p h n", h=2)
            k.dma(sp, sv, w_out[hh * 128:(hh + 1) * 128, :].rearrange("(h p) n -> p h n", p=64), W=[b_stg])
            k.op(dve, lambda e: e.tensor_copy(out=wo_att[:, hh * 2:(hh + 1) * 2, :], in_=sv), R=[b_stg], W=[b_woa])
        for hh in range(2):
            sv = stg[:, :].rearrange("p (c n) -> p c n", c=2)
            k.dma(sp, sv, w_out[512 + hh * 256:512 + (hh + 1) * 256, :].rearrange("(c p) n -> p c n", p=128), W=[b_stg])
            k.op(dve, lambda e: e.tensor_copy(out=wo_ssd[:, hh * 2:(hh + 1) * 2, :], in_=sv), R=[b_stg], W=[b_wos])
        sv = stg[:, 0:128].rearrange("p (c n) -> p c n", c=8)
        k.dma(sp, sv, w_router.rearrange("(c p) n -> p c n", p=128), W=[b_stg])
        k.op(dve, lambda e: e.tensor_copy(out=wr_bf[:], in_=sv), R=[b_stg], W=[b_wr])

        k.op(pool, lambda e: e.memset(relx[:], NEG), W=[b_relx])
        k.dma(sp, relx[0:32, :], rel_bias[:, :], W=[b_relx])
        k.dma(sp, oh_sb[:], oh_in.rearrange("p b i -> b p i"), W=[b_oh])
        for p in range(3):
            pt, bpt = PB[p % 2]
            k.op(pe, lambda e: e.matmul(pt[0:8, 0:384], lhsT=relx[:, :], rhs=oh_sb[:, p, :], start=True, stop=True), R=[b_relx, b_oh], W=[bpt])
            k.op(act, lambda e: e.copy(out=tab_sb[:, p, :], in_=pt[0:8, 0:384]), R=[bpt], W=[b_tabsb])
        k.dma(sp, tab_d.rearrange("p h i -> h p i"), tab_sb[:], R=[b_tabsb], W=[b_tab])
        for hp in range(4):
            for p in range(3):
                for h in range(2):
                    for jj in range(2):
                        src = bass.AP(tensor=tab_d_t, offset=(p * 8 + hp * 2 + h) * 384 + 127 + 128 * jj, ap=[[1, 128], [-1, 128]])
                        c0 = (h * 2 + jj) * 128
                        k.dma(sp, stg[:, c0:c0 + 128], src, R=[b_tab], W=[b_stg], allow_slow_non_contiguous=True)
                k.op(dve, lambda e: e.tensor_copy(out=biasT[:, hp * 3 + p, :], in_=stg[:, 0:512]), R=[b_stg], W=[b_bias])

    xt = [T(f"xt{i}", [128, D], F32) for i in range(2)]
    junk, b_junk = T("junk", [128, D], F32)
    st_s, b_sts = T("st_s", [128, 8], F32)
    hb, b_hb = T("hb", [128, D], BF16)
    hT, b_hT = T("hT", [128, 8, 512], BF16)
    evb = [T(f"evb{i}", [128, 512], BF16) for i in range(3)]
    evf = [T(f"evf{i}", [128, 512], F32) for i in range(3)]
    zpad, b_zpad = T("zpad", [128, 8, 2], F32)
    k.op(pool, lambda e: e.memset(zpad[:], 0.0), W=[b_zpad])

    def rmsnorm_to_bf(xtile, bx, gtile, bg, out_bf, bout, n=D):
        k.op(act, lambda e: e.activation(out=junk[:, 0:n], in_=xtile, func=AF.Square, accum_out=st_s[:, 0:1]), R=[bx], W=[b_junk, b_sts])
        k.op(dve, lambda e: e.tensor_scalar(out=st_s[:, 1:2], in0=st_s[:, 0:1], scalar1=1.0 / n, scalar2=EPS, op0=ALU.mult, op1=ALU.add), R=[b_sts], W=[b_sts])
        k.op(act, lambda e: e.activation(out=st_s[:, 2:3], in_=st_s[:, 1:2], func=AF.Sqrt), R=[b_sts], W=[b_sts])
        k.op(dve, lambda e: e.reciprocal(out=st_s[:, 3:4], in_=st_s[:, 2:3]), R=[b_sts], W=[b_sts])
        k.op(dve, lambda e: e.scalar_tensor_tensor(out=out_bf, in0=xtile, scalar=st_s[:, 3:4], in1=gtile, op0=ALU.mult, op1=ALU.mult), R=[bx, b_sts, bg], W=[bout])

    cnt = {"ev": 0, "ph": 0, "pb": 0}

    def done():
        k.barrier()
        k.dead = True
        return nc, k

    if upto == 0:
        return done()

    for (row0, S) in seqs:
        k.dma(sp, xbc_d[:, 0:2].rearrange("(c p) t -> p c t", p=128), zpad[:], R=[b_zpad], W=[b_xbc])
        k.dma(sp, xbc_d[:, S + 2:S + 4].rearrange("(c p) t -> p c t", p=128), zpad[:], R=[b_zpad], W=[b_xbc])
        for blk in range(S // 512):
            for t in range(4):
                xtile, bx = xt[t % 2]
                r = row0 + blk * 512 + t * 128
                k.dma(sp, xtile[:], x_in[r:r + 128, :], W=[bx])
                rmsnorm_to_bf(xtile[:], bx, gmix[:], b_gmix, hb[:], b_hb)
                ph, bph = PH[cnt["ph"] % 2]
                cnt["ph"] += 1
                for c in range(8):
                    k.op(pe, lambda e: e.transpose(out=ph[:, c * 128:(c + 1) * 128], in_=hb[:, c * 128:(c + 1) * 128], identity=ident[:]), R=[b_hb, b_id], W=[bph])
                k.op(act, lambda e: e.copy(out=hT[:, :, t * 128:(t + 1) * 128], in_=ph[:, :].rearrange("p (c t) -> p c t", c=8)), R=[bph], W=[b_hT])
            fcs = [(i, i * 128) for i in range(12)] + [(12 + i, 2048 + i * 128) for i in range(8)]
            for (fi, col0) in fcs:
                pt, bpt = PB[cnt["pb"] % 2]
                cnt["pb"] += 1
                for kc in range(8):
                    k.op(pe, lambda e: e.matmul(pt[:, :], lhsT=w_in_bf[:, kc, col0:col0 + 128], rhs=hT[:, kc, :], start=(kc == 0), stop=(kc == 7)), R=[b_win, b_hT], W=[bpt])
                i = cnt["ev"] % 3
                cnt["ev"] += 1
                if fi < 12:
                    et, bet = evb[i]
                    dst, bd = [(qT_d, b_qT), (kT_d, b_kT), (vT_d, b_vT)][fi // 4]
                    drows = dst[(fi % 4) * 128:(fi % 4 + 1) * 128, blk * 512:(blk + 1) * 512]
                else:
                    et, bet = evf[i]
                    dst, bd = xbc_d, b_xbc
                    drows = dst[(fi - 12) * 128:(fi - 11) * 128, 2 + blk * 512:2 + (blk + 1) * 512]
                if fi % 2 == 0:
                    k.op(act, lambda e: e.copy(out=et[:], in_=pt[:, :]), R=[bpt], W=[bet])
                else:
                    k.op(dve, lambda e: e.tensor_copy(out=et[:], in_=pt[:, :]), R=[bpt], W=[bet])
                k.dma(sp, drows, et[:], R=[bet], W=[bd])
            for t in range(4):
                pt, bpt = PB[2 + t % 2]
                for kc in range(8):
                    k.op(pe, lambda e: e.matmul(pt[:, :], lhsT=hT[:, kc, t * 128:(t + 1) * 128], rhs=w_in_bf[:, kc, 1536:2048], start=(kc == 0), stop=(kc == 7)), R=[b_win, b_hT], W=[bpt])
                i = cnt["ev"] % 3
                cnt["ev"] += 1
                et, bet = evf[i]
                k.op(act, lambda e: e.copy(out=et[:], in_=pt[:, :]), R=[bpt], W=[bet])
                r = blk * 512 + t * 128
                k.dma(sp, z_d[r:r + 128, :], et[:], R=[bet], W=[b_z])
                pt2, bpt2 = PB[4 + t % 2]
                for kc in range(8):
                    k.op(pe, lambda e: e.matmul(pt2[:, 0:16], lhsT=hT[:, kc, t * 128:(t + 1) * 128], rhs=w_in_bf[:, kc, 3072:3088], start=(kc == 0), stop=(kc == 7)), R=[b_win, b_hT], W=[bpt2])
                i = cnt["ev"] % 3
                cnt["ev"] += 1
                et, bet = evf[i]
                k.op(dve, lambda e: e.tensor_copy(out=et[:, 0:16], in_=pt2[:, 0:16]), R=[bpt2], W=[bet])
                k.dma(sp, dt_d[r:r + 128, :], et[:, 0:16], R=[bet], W=[b_dt])

        if upto == 1:
            return done()
        with k.scope():
            qp, b_qp = T("qp", [128, SMAX], BF16)
            kp, b_kp = T("kp", [128, SMAX + 2 * PAD], BF16)
            vp, b_vp = T("vp", [128, SMAX + 2 * PAD], BF16)
            k.op(pool, lambda e: e.memset(kp[:], 0.0), W=[b_kp])
            k.op(pool, lambda e: e.memset(vp[:], 0.0), W=[b_vp])
            NVT = 17
            vts = [T(f"vt{i}", [128, 2, 65], BF16) for i in range(NVT + 2)]
            for i, (vt, bvt) in enumerate(vts):
                k.op(pool, lambda e: e.memset(vt[:], 1.0), W=[bvt])
            k.op(pool, lambda e: e.memset(vts[NVT][0][0:64, :, :], 0.0), W=[vts[NVT][1]])
            k.op(pool, lambda e: e.memset(vts[NVT + 1][0][64:128, :, :], 0.0), W=[vts[NVT + 1][1]])
            ef = [T(f"ef{i}", [128, 512], F32) for i in range(2)]
            ex = [T(f"ex{i}", [128, 512], BF16) for i in range(2)]
            acc, b_acc = T("acc", [65, 2, 2048], F32)
            ao, b_ao = T("ao", [64, 2, 2048], BF16)

            NSB = S // 2048
            for hp in range(4):
                k.dma(sp, qp[:, 0:S], qT_d[hp * 128:(hp + 1) * 128, 0:S], R=[b_qT], W=[b_qp])
                k.op(pool, lambda e: e.memset(kp[:, PAD + S:PAD + S + PAD], 0.0), W=[b_kp])
                k.op(pool, lambda e: e.memset(vp[:, PAD + S:PAD + S + PAD], 0.0), W=[b_vp])
                k.dma(sp, kp[:, PAD:PAD + S], kT_d[hp * 128:(hp + 1) * 128, 0:S], R=[b_kT], W=[b_kp])
                k.dma(sp, vp[:, PAD:PAD + S], vT_d[hp * 128:(hp + 1) * 128, 0:S], R=[b_vT], W=[b_vp])
                for sbk in range(NSB):
                    q0 = sbk * 2048
                    for p, d in enumerate(DILS):
                        nq = 2048 // d // 128
                        for r in range(d):
                            def kslice(j):
                                ks = PAD + q0 + r + d * (128 * j - 64)
                                return slice(ks, ks + 127 * d + 1, d)

                            def vtile(j):
                                if sbk == 0 and j == 0:
                                    return vts[NVT]
                                if sbk == NSB - 1 and j == nq:
                                    return vts[NVT + 1]
                                return vts[j]

                            for j in range(nq + 1):
                                ph, bph = PH[cnt["ph"] % 2]
                                cnt["ph"] += 1
                                k.op(pe, lambda e: e.transpose(out=ph[:, 0:128], in_=vp[:, kslice(j)], identity=ident[:]), R=[b_vp, b_id], W=[bph])
                                vt, bvt = vtile(j)
                                if j % 2 == 0:
                                    k.op(act, lambda e: e.copy(out=vt[:, :, 0:64], in_=ph[:, 0:128].rearrange("p (h e) -> p h e", h=2)), R=[bph], W=[bvt])
                                else:
                                    k.op(dve, lambda e: e.tensor_copy(out=vt[:, :, 0:64], in_=ph[:, 0:128].rearrange("p (h e) -> p h e", h=2)), R=[bph], W=[bvt])
                            for qi in range(nq):
                                qs = q0 + r + d * 128 * qi
                                qsl = slice(qs, qs + 127 * d + 1, d)
                                pS, bpS = PB[cnt["pb"] % 2]
                                cnt["pb"] += 1
                                for h in range(2):
                                    for jj in range(2):
                                        c0 = (h * 2 + jj) * 128
                                        k.op(pe, lambda e: e.matmul(pS[:, c0:c0 + 128], lhsT=kp[h * 64:(h + 1) * 64, kslice(qi + jj)], rhs=qp[h * 64:(h + 1) * 64, qsl], start=True, stop=True), R=[b_kp, b_qp], W=[bpS])
                                i = cnt["ev"] % 2
                                cnt["ev"] += 1
                                eft, beft = ef[i]
                                ext, bext = ex[i]
                                k.op(dve, lambda e: e.scalar_tensor_tensor(out=eft[:], in0=pS[:, :], scalar=0.125, in1=biasT[:, hp * 3 + p, :], op0=ALU.mult, op1=ALU.add), R=[bpS, b_bias], W=[beft])
                                k.op(act, lambda e: e.activation(out=ext[:], in_=eft[:], func=AF.Exp), R=[beft], W=[bext])
                                pO, bpO = PB[2 + i]
                                for h in range(2):
                                    for jj in range(2):
                                        c0 = (h * 2 + jj) * 128
                                        vt, bvt = vtile(qi + jj)
                                        k.op(pe, lambda e: e.matmul(pO[0:65, h * 128:(h + 1) * 128], lhsT=vt[:, h, :], rhs=ext[:, c0:c0 + 128], start=(jj == 0), stop=(jj == 1)), R=[bvt, bext], W=[bpO])
                                asl = slice(qs - q0, qs - q0 + 127 * d + 1, d)
                                pOv = pO[0:65, 0:256].rearrange("p (h q) -> p h q", h=2)
                                if p == 0:
                                    k.op(act, lambda e: e.copy(out=acc[:, :, asl], in_=pOv), R=[bpO], W=[b_acc])
                                else:
                                    k.op(dve, lambda e: e.tensor_tensor(out=acc[:, :, asl], in0=acc[:, :, asl], in1=pOv, op=ALU.add), R=[bpO, b_acc], W=[b_acc])
                    k.op(dve, lambda e: e.reciprocal(out=acc[64:65, :, :], in_=acc[64:65, :, :]), R=[b_acc], W=[b_acc])
                    for h in range(2):
                        for c in range(4):
                            pBt, bpB = PB[4 + c % 2]
                            k.op(pe, lambda e: e.matmul(pBt[0:64, :], lhsT=onesf[64:65, 0:64], rhs=acc[64:65, h, c * 512:(c + 1) * 512], start=True, stop=True), R=[b_ones, b_acc], W=[bpB])
                            k.op(dve, lambda e: e.tensor_tensor(out=ao[:, h, c * 512:(c + 1) * 512], in0=acc[0:64, h, c * 512:(c + 1) * 512], in1=pBt[0:64, :], op=ALU.mult), R=[b_acc, bpB], W=[b_ao])
                    k.dma(sp, at_d[hp * 2:hp * 2 + 2, :, q0:q0 + 2048].rearrange("h e t -> e h t"), ao[:], R=[b_ao], W=[b_at])

        if upto == 2:
            return done()
        with k.scope():
            xin = [T(f"xin{i}", [128, 8, 132], F32) for i in range(2)]
            cv, b_cv = T("cv", [128, 8, 128], F32)
            xbf, b_xbf = T("xbf", [128, 8, 128], BF16)
            xs_tok, b_xst = T("xs_tok", [128, 512], BF16)
            b_tok, b_btok = T("b_tok", [128, 2, 128], BF16)
            dtr, b_dtr = T("dtr", [128, 16], F32)
            dtv, b_dtv = T("dtv", [128, 16], F32)
            av, b_av = T("av", [128, 16], F32)
            cum, b_cum = T("cum", [128, 8], F32)
            ncum, b_ncum = T("ncum", [128, 8], F32)
            dsv, b_dsv = T("dsv", [128, 8], F32)
            Ev, b_Ev = T("Ev", [128, 8], F32)
            cdv, b_cdv = T("cdv", [128, 8], F32)
            xdt, b_xdt = T("xdt", [128, 8, 64], BF16)
            xdd, b_xdd = T("xdd", [128, 8, 64], BF16)
            cbs, b_cbs = T("cbs", [128, 2, 128], F32)
            abc = [T(f"abc{i}", [128, 128], F32) for i in range(2)]
            dec = [T(f"dec{i}", [128, 128], F32) for i in range(2)]
            Gb = [T(f"Gb{i}", [128, 128], BF16) for i in range(2)]
            Hf, b_Hf = T("Hf", [128, 8, 64], F32)
            Hb, b_Hb = T("Hb", [128, 8, 64], BF16)
            ytmp, b_ytmp = T("ytmp", [128, 512], F32)
            ydir, b_ydir = T("ydir", [128, 512], F32)
            yfw, b_yfw = T("yfw", [128, 512], F32)
            zt, b_zt = T("zt", [128, 512], F32)
            ssd_bf, b_ssdbf = T("ssd_bf", [128, 512], BF16)
            ssdT, b_ssdT = T("ssdT", [128, 4, 128], BF16)
            at_sb, b_atsb = T("at_sb", [64, 8, 128], BF16)
            x1t, b_x1t = T("x1t", [128, D], F32)
            hnT, b_hnT = T("hnT", [128, 8, 128], BF16)
            lg, b_lg = T("lg", [128, 16], F32)
            afft, b_afft = T("afft", [128, 16], F32)
            sm, b_sm = T("sm", [128, 8], F32)

            NCH = S // 128
            for direction in range(2):
                k.op(pool, lambda e: e.memset(Hf[:], 0.0), W=[b_Hf])
                k.op(pool, lambda e: e.memset(Hb[:], 0.0), W=[b_Hb])
                tri, b_tri = (triU, b_triU) if direction == 0 else (triL, b_triL)
                nm, b_nm = (nmU, b_nmU) if direction == 0 else (nmL, b_nmL)
                order = range(NCH) if direction == 0 else range(NCH - 1, -1, -1)
                dc = direction * 8
                for ci, c in enumerate(order):
                    t0 = c * 128
                    xi, bxi = xin[ci % 2]
                    k.dma(sp, xi[:], xbc_d[:, t0:t0 + 132].rearrange("(c p) t -> p c t", p=128), R=[b_xbc], W=[bxi])
                    k.dma(sp, dtr[:], dt_d[t0:t0 + 128, :], R=[b_dt], W=[b_dtr])
                    if upto == 30:
                        return done()
                    for cc in range(8):
                        k.op(dve, lambda e: e.tensor_scalar(out=cv[:, cc, :], in0=xi[:, cc, 0:128], scalar1=cw[:, 0, cc:cc + 1], scalar2=None, op0=ALU.mult), R=[bxi, b_cw], W=[b_cv])
                        for kk in range(1, 5):
                            k.op(dve, lambda e: e.scalar_tensor_tensor(out=cv[:, cc, :], in0=xi[:, cc, kk:kk + 128], scalar=cw[:, kk, cc:cc + 1], in1=cv[:, cc, :], op0=ALU.mult, op1=ALU.add), R=[bxi, b_cw, b_cv], W=[b_cv])
                        k.op(act, lambda e: e.activation(out=xbf[:, cc, :], in_=cv[:, cc, :], func=AF.Silu, bias=cb[:, cc:cc + 1]), R=[b_cv, b_cb], W=[b_xbf])
                    if upto == 31:
                        return done()
                    ph, bph = PH[cnt["ph"] % 2]
                    cnt["ph"] += 1
                    for cc in range(6):
                        k.op(pe, lambda e: e.transpose(out=ph[:, cc * 128:(cc + 1) * 128], in_=xbf[:, cc, :], identity=ident[:]), R=[b_xbf, b_id], W=[bph])
                    k.op(act, lambda e: e.copy(out=xs_tok[:], in_=ph[:, 0:512]), R=[bph], W=[b_xst])
                    k.op(dve, lambda e: e.tensor_copy(out=b_tok[:], in_=ph[:, 512:768].rearrange("p (g n) -> p g n", g=2)), R=[bph], W=[b_btok])
                    if upto == 32:
                        return done()
                    k.op(dve, lambda e: e.tensor_tensor(out=dtv[:], in0=dtr[:], in1=dtb[:], op=ALU.add), R=[b_dtr, b_dtb], W=[b_dtv])
                    k.op(act, lambda e: e.activation(out=dtv[:], in_=dtv[:], func=AF.Exp), R=[b_dtv], W=[b_dtv])
                    k.op(act, lambda e: e.activation(out=dtv[:], in_=dtv[:], func=AF.Ln, bias=1.0), R=[b_dtv], W=[b_dtv])
                    k.op(dve, lambda e: e.tensor_tensor(out=av[:], in0=dtv[:], in1=Abc[:], op=ALU.mult), R=[b_dtv, b_A], W=[b_av])
                    if upto == 3:
                        return done()
                    pC, bpC = PB[0]
                    k.op(pe, lambda e: e.matmul(pC[:, 0:8], lhsT=tri[:, :], rhs=av[:, dc:dc + 8], start=True, stop=True), R=[b_tri, b_av], W=[bpC])
                    k.op(pe, lambda e: e.matmul(pC[:, 8:16], lhsT=onesf[:, :], rhs=av[:, dc:dc + 8], start=True, stop=True), R=[b_ones, b_av], W=[bpC])
                    k.op(dve, lambda e: e.tensor_copy(out=cum[:], in_=pC[:, 0:8]), R=[bpC], W=[b_cum])
                    k.op(dve, lambda e: e.tensor_scalar(out=ncum[:], in0=pC[:, 0:8], scalar1=-1.0, scalar2=None, op0=ALU.mult), R=[bpC], W=[b_ncum])
                    k.op(dve, lambda e: e.tensor_tensor(out=dsv[:], in0=pC[:, 8:16], in1=cum[:], op=ALU.subtract), R=[bpC, b_cum], W=[b_dsv])
                    k.op(act, lambda e: e.activation(out=dsv[:], in_=dsv[:], func=AF.Exp), R=[b_dsv], W=[b_dsv])
                    k.op(act, lambda e: e.activation(out=Ev[:], in_=cum[:], func=AF.Exp), R=[b_cum], W=[b_Ev])
                    k.op(act, lambda e: e.activation(out=cdv[:], in_=pC[:, 8:16], func=AF.Exp), R=[bpC], W=[b_cdv])
                    xs3 = xs_tok[:, :].rearrange("p (h e) -> p h e", h=8)
                    k.op(dve, lambda e: e.tensor_tensor(out=xdt[:], in0=xs3, in1=dtv[:, dc:dc + 8].unsqueeze(2).to_broadcast([128, 8, 64]), op=ALU.mult), R=[b_xst, b_dtv], W=[b_xdt])
                    k.op(dve, lambda e: e.tensor_tensor(out=xdd[:], in0=xdt[:], in1=dsv[:, :].unsqueeze(2).to_broadcast([128, 8, 64]), op=ALU.mult), R=[b_xdt, b_dsv], W=[b_xdd])
                    if upto == 4:
                        return done()
                    pCB, bpCB = PB[1]
                    for g in range(2):
                        k.op(pe, lambda e: e.matmul(pCB[:, g * 128:(g + 1) * 128], lhsT=xbf[:, 4 + g, :], rhs=xbf[:, 6 + g, :], start=True, stop=True), R=[b_xbf], W=[bpCB])
                    k.op(act, lambda e: e.copy(out=cbs[:], in_=pCB[:, 0:256].rearrange("p (g l) -> p g l", g=2)), R=[bpCB], W=[b_cbs])
                    pY, bpY = PB[2]
                    pYo, bpYo = PB[3]
                    for h in range(8):
                        ab, bab = abc[h % 2]
                        de, bde = dec[h % 2]
                        G, bG = Gb[h % 2]
                        pD, bpD = PB[4 + h % 2]
                        k.op(pool, lambda e: e.tensor_copy(out=ab[:], in_=av[:, dc + h:dc + h + 1].to_broadcast([128, 128])), R=[b_av], W=[bab])
                        k.op(pe, lambda e: e.matmul(pD[:, 0:128], lhsT=ab[:, :], rhs=tri[:, :], start=True, stop=False), R=[bab, b_tri], W=[bpD])
                        k.op(pe, lambda e: e.matmul(pD[:, 0:128], lhsT=identf[:, :], rhs=nm[:, :], start=False, stop=True), R=[b_idf, b_nm], W=[bpD])
                        k.op(act, lambda e: e.activation(out=de[:], in_=pD[:, 0:128], func=AF.Exp, bias=ncum[:, h:h + 1]), R=[bpD, b_ncum], W=[bde])
                        k.op(dve, lambda e: e.tensor_tensor(out=G[:], in0=de[:], in1=cbs[:, h // 4, :], op=ALU.mult), R=[bde, b_cbs], W=[bG])
                        k.op(pe, lambda e: e.matmul(pY[:, h * 64:(h + 1) * 64], lhsT=G[:, :], rhs=xdt[:, h, :], start=True, stop=True), R=[bG, b_xdt], W=[bpY])
                    for g in range(2):
                        k.op(pe, lambda e: e.matmul(pYo[:, g * 256:(g + 1) * 256], lhsT=xbf[:, 6 + g, :], rhs=Hb[:, g * 4:(g + 1) * 4, :].rearrange("p h e -> p (h e)"), start=True, stop=True), R=[b_xbf, b_Hb], W=[bpYo])
                    k.op(dve, lambda e: e.tensor_tensor(out=ytmp[:, :].rearrange("p (h e) -> p h e", h=8), in0=pYo[:, :].rearrange("p (h e) -> p h e", h=8), in1=Ev[:, :].unsqueeze(2).to_broadcast([128, 8, 64]), op=ALU.mult), R=[bpYo, b_Ev], W=[b_ytmp])
                    k.op(dve, lambda e: e.tensor_tensor(out=ydir[:], in0=ytmp[:], in1=pY[:, :], op=ALU.add), R=[b_ytmp, bpY], W=[b_ydir])
                    if upto == 5:
                        return done()
                    pSt, bpSt = PB[0]
                    for g in range(2):
                        k.op(pe, lambda e: e.matmul(pSt[:, g * 256:(g + 1) * 256], lhsT=b_tok[:, g, :], rhs=xdd[:, g * 4:(g + 1) * 4, :].rearrange("p h e -> p (h e)"), start=True, stop=True), R=[b_btok, b_xdd], W=[bpSt])
                    k.op(dve, lambda e: e.tensor_tensor(out=Hf[:], in0=Hf[:], in1=cdv[:, :].unsqueeze(2).to_broadcast([128, 8, 64]), op=ALU.mult), R=[b_Hf, b_cdv], W=[b_Hf])
                    k.op(dve, lambda e: e.tensor_tensor(out=Hf[:], in0=Hf[:], in1=pSt[:, :].rearrange("p (h e) -> p h e", h=8), op=ALU.add), R=[b_Hf, bpSt], W=[b_Hf])
                    k.op(pool, lambda e: e.tensor_copy(out=Hb[:], in_=Hf[:]), R=[b_Hf], W=[b_Hb])
                    if direction == 0:
                        k.dma(sp, yf_d[t0:t0 + 128, :], ydir[:], R=[b_ydir], W=[b_yf])
                        continue
                    if upto == 6:
                        return done()
                    k.dma(sp, yfw[:], yf_d[t0:t0 + 128, :], R=[b_yf], W=[b_yfw])
                    k.dma(sp, zt[:], z_d[t0:t0 + 128, :], R=[b_z], W=[b_zt])
                    k.dma(sp, at_sb[:], at_d[:, :, t0:t0 + 128].rearrange("h e t -> e h t"), R=[b_at], W=[b_atsb])
                    xtile, bx = xt[ci % 2]
                    k.dma(sp, xtile[:], x_in[row0 + t0:row0 + t0 + 128, :], W=[bx])
                    k.op(dve, lambda e: e.tensor_tensor(out=ydir[:], in0=ydir[:], in1=yfw[:], op=ALU.add), R=[b_ydir, b_yfw], W=[b_ydir])
                    k.op(pool, lambda e: e.tensor_tensor(out=ytmp[:, :].rearrange("p (h e) -> p h e", h=8), in0=xs3, in1=dsk[:, :].unsqueeze(2).to_broadcast([128, 8, 64]), op=ALU.mult), R=[b_xst, b_dsk], W=[b_ytmp])
                    k.op(dve, lambda e: e.tensor_tensor(out=ydir[:], in0=ydir[:], in1=ytmp[:], op=ALU.add), R=[b_ydir, b_ytmp], W=[b_ydir])
                    k.op(act, lambda e: e.activation(out=zt[:], in_=zt[:], func=AF.Silu), R=[b_zt], W=[b_zt])
                    k.op(dve, lambda e: e.tensor_tensor(out=ydir[:], in0=ydir[:], in1=zt[:], op=ALU.mult), R=[b_ydir, b_zt], W=[b_ydir])
                    for g in range(2):
                        k.op(act, lambda e: e.activation(out=junk[:, g * 256:(g + 1) * 256], in_=ydir[:, g * 256:(g + 1) * 256], func=AF.Square, accum_out=sm[:, g:g + 1]), R=[b_ydir], W=[b_junk, b_sm])
                    k.op(dve, lambda e: e.tensor_scalar(out=sm[:, 2:4], in0=sm[:, 0:2], scalar1=1.0 / 256, scalar2=EPS, op0=ALU.mult, op1=ALU.add), R=[b_sm], W=[b_sm])
                    k.op(act, lambda e: e.activation(out=sm[:, 4:6], in_=sm[:, 2:4], func=AF.Sqrt), R=[b_sm], W=[b_sm])
                    k.op(dve, lambda e: e.reciprocal(out=sm[:, 6:8], in_=sm[:, 4:6]), R=[b_sm], W=[b_sm])
                    for g in range(2):
                        k.op(dve, lambda e: e.scalar_tensor_tensor(out=ssd_bf[:, g * 256:(g + 1) * 256], in0=ydir[:, g * 256:(g + 1) * 256], scalar=sm[:, 6 + g:7 + g], in1=gssd[:, g * 256:(g + 1) * 256], op0=ALU.mult, op1=ALU.mult), R=[b_ydir, b_sm, b_gssd], W=[b_ssdbf])
                    ph, bph = PH[cnt["ph"] % 2]
                    cnt["ph"] += 1
                    for cc in range(4):
                        k.op(pe, lambda e: e.transpose(out=ph[:, cc * 128:(cc + 1) * 128], in_=ssd_bf[:, cc * 128:(cc + 1) * 128], identity=ident[:]), R=[b_ssdbf, b_id], W=[bph])
                    k.op(act, lambda e: e.copy(out=ssdT[:], in_=ph[:, 0:512].rearrange("p (c t) -> p c t", c=4)), R=[bph], W=[b_ssdT])
                    for half in range(2):
                        pX, bpX = PB[4 + half]
                        hs = slice(half * 512, (half + 1) * 512)
                        for h in range(8):
                            k.op(pe, lambda e: e.matmul(pX[:, :], lhsT=at_sb[:, h, :], rhs=wo_att[:, h, hs], start=(h == 0), stop=False), R=[b_atsb, b_woa], W=[bpX])
                        for cc in range(4):
                            k.op(pe, lambda e: e.matmul(pX[:, :], lhsT=ssdT[:, cc, :], rhs=wo_ssd[:, cc, hs], start=False, stop=(cc == 3)), R=[b_ssdT, b_wos], W=[bpX])
                        k.op(dve, lambda e: e.tensor_tensor(out=x1t[:, hs], in0=xtile[:, hs], in1=pX[:, :], op=ALU.add), R=[bx, bpX], W=[b_x1t])
                    k.dma(sp, x1_out[row0 + t0:row0 + t0 + 128, :], x1t[:], R=[b_x1t], W=[b_x1])
                    rmsnorm_to_bf(x1t[:], b_x1t, gffn[:], b_gffn, hb[:], b_hb)
                    ph, bph = PH[cnt["ph"] % 2]
                    cnt["ph"] += 1
                    for cc in range(8):
                        k.op(pe, lambda e: e.transpose(out=ph[:, cc * 128:(cc + 1) * 128], in_=hb[:, cc * 128:(cc + 1) * 128], identity=ident[:]), R=[b_hb, b_id], W=[bph])
                    k.op(act, lambda e: e.copy(out=hnT[:], in_=ph[:, :].rearrange("p (c t) -> p c t", c=8)), R=[bph], W=[b_hnT])
                    pR, bpR = PB[1]
                    for kc in range(8):
                        k.op(pe, lambda e: e.matmul(pR[:, 0:16], lhsT=hnT[:, kc, :], rhs=wr_bf[:, kc, :], start=(kc == 0), stop=(kc == 7)), R=[b_hnT, b_wr], W=[bpR])
                    k.op(dve, lambda e: e.tensor_reduce(out=sm[:, 0:1], in_=pR[:, 0:16], axis=AX.X, op=ALU.max, negate=True), R=[bpR], W=[b_sm])
                    k.op(act, lambda e: e.activation(out=lg[:], in_=pR[:, 0:16], func=AF.Exp, bias=sm[:, 0:1], accum_out=sm[:, 1:2]), R=[bpR, b_sm], W=[b_lg, b_sm])
                    k.op(dve, lambda e: e.reciprocal(out=sm[:, 2:3], in_=sm[:, 1:2]), R=[b_sm], W=[b_sm])
                    k.op(dve, lambda e: e.tensor_scalar(out=afft[:], in0=lg[:], scalar1=sm[:, 2:3], scalar2=None, op0=ALU.mult), R=[b_lg, b_sm], W=[b_afft])
                    k.dma(sp, aff_out[row0 + t0:row0 + t0 + 128, :], afft[:], R=[b_afft], W=[b_aff])

    k.finish([b_x1, b_aff])
    return nc, k


def build_b(NT, TG, n_exp=16, FF=2816):
    nc, k = _build_b(NT, TG, n_exp, FF)
    k.close()
    return nc, k.n_ins


def _build_b(NT, TG, n_exp, FF):
    nc = bass.Bass("TRN2", target_bir_lowering=False)
    k = K(nc)
    sp, act, dve, pool, pe = k.sp, k.act, k.dve, k.pool, k.pe
    NTL = NT // 128
    NFC = FF // 128
    CAP = TG // 8
    TPP = TG // 128

    def din(name, shape):
        return nc.dram_tensor(name, list(shape), F32, kind="ExternalInput").ap()

    x1_in = din("x1", [NT, D])
    aff_all = din("aff_all", [2, TG, 16])
    aff_loc = din("aff_loc", [NT, 16])
    p_in = din("p", [NT, 256])
    w_gate = din("w_gate", [n_exp, D, FF])
    w_up = din("w_up", [n_exp, D, FF])
    w_down = din("w_down", [n_exp, FF, D])
    g_ffn = din("g_ffn", [1, D])
    g_pg = din("g_pg", [1, D])
    g_ple = din("g_ple", [1, D])
    g_fin = din("g_final", [1, D])
    w_pg = din("w_pg", [D, D])
    w_ple = din("w_ple", [256, D])
    y_out = nc.dram_tensor("y", [NT, D], F32, kind="ExternalOutput").ap()
    b_y = Buf("y")
    yacc_d = nc.dram_tensor("yacc_d", [NT, D], F32).ap()

    def T(name, shape, dt):
        return k.sb(name, shape, dt), Buf(name)

    PB = [(k.ps(f"pb{i}", [128, 512], F32), Buf(f"pb{i}", excl=True)) for i in range(6)]
    PH = [(k.ps(f"ph{i}", [128, 1024], BF16), Buf(f"ph{i}", excl=True)) for i in range(2)]

    onesf, b_ones = T("onesf", [128, 128], F32)
    identf, b_idf = T("identf", [128, 128], F32)
    ident, b_id = T("ident", [128, 128], BF16)
    k.op(pool, lambda e: e.memset(onesf[:], 1.0), W=[b_ones])
    k.op(pool, lambda e: e.memset(identf[:], 0.0), W=[b_idf])
    k.op(pool, lambda e: e.affine_select(out=identf[:], in_=identf[:], pattern=[[-1, 128]], compare_op=ALU.not_equal, fill=1.0, base=0, channel_multiplier=1), R=[b_idf], W=[b_idf])
    k.op(dve, lambda e: e.tensor_copy(out=ident[:], in_=identf[:]), R=[b_idf], W=[b_id])
    thr, b_thr = T("thr", [128, 32], F32)

    def rmsnorm(xtile, bx, gtile, bg, out, bout, n=D):
        k.op(act, lambda e: e.activation(out=junk[:, 0:n], in_=xtile, func=AF.Square, accum_out=st_s[:, 0:1]), R=[bx], W=[b_junk, b_sts])
        k.op(dve, lambda e: e.tensor_scalar(out=st_s[:, 1:2], in0=st_s[:, 0:1], scalar1=1.0 / n, scalar2=EPS, op0=ALU.mult, op1=ALU.add), R=[b_sts], W=[b_sts])
        k.op(act, lambda e: e.activation(out=st_s[:, 2:3], in_=st_s[:, 1:2], func=AF.Sqrt), R=[b_sts], W=[b_sts])
        k.op(dve, lambda e: e.reciprocal(out=st_s[:, 3:4], in_=st_s[:, 2:3]), R=[b_sts], W=[b_sts])
        k.op(dve, lambda e: e.scalar_tensor_tensor(out=out, in0=xtile, scalar=st_s[:, 3:4], in1=gtile, op0=ALU.mult, op1=ALU.mult), R=[bx, b_sts, bg], W=[bout])

    with k.scope():
        aff, b_affs = T("aff", [128, 2, TPP, 16], F32)
        cmp_, b_cmp = T("cmp", [128, 2, TPP, 16], BF16)
        cntt, b_cnt = T("cntt", [128, 32], F32)
        lo, b_lo = T("lo", [128, 32], F32)
        hi, b_hi = T("hi", [128, 32], F32)
        mid, b_mid = T("mid", [128, 32], F32)
        ge, b_ge = T("ge", [128, 32], F32)
        d1, b_d1 = T("d1", [128, 32], F32)
        for g in range(2):
            k.dma(sp, aff[:, g, :, :], aff_all[g].rearrange("(p t) e -> p t e", p=128), W=[b_affs])
        k.op(pool, lambda e: e.memset(lo[:], 0.0), W=[b_lo])
        k.op(pool, lambda e: e.memset(hi[:], 1.0), W=[b_hi])
        k.op(pool, lambda e: e.memset(mid[:], 0.5), W=[b_mid])
        for it in range(32):
            mb = mid[:, :].rearrange("p (g e) -> p g e", g=2).unsqueeze(2).to_broadcast([128, 2, TPP, 16])
            k.op(dve, lambda e: e.tensor_tensor(out=cmp_[:], in0=aff[:], in1=mb, op=ALU.is_ge), R=[b_affs, b_mid], W=[b_cmp])
            k.op(dve, lambda e: e.tensor_reduce(out=cntt[:, :].rearrange("p (g e) -> p g e", g=2), in_=cmp_[:].rearrange("p g t e -> p g e t"), axis=AX.X, op=ALU.add), R=[b_cmp], W=[b_cnt])
            pC, bpC = PB[it % 2]
            k.op(pe, lambda e: e.matmul(pC[:, 0:32], lhsT=onesf[:, :], rhs=cntt[:, :], start=True, stop=True), R=[b_ones, b_cnt], W=[bpC])
            k.op(dve, lambda e: e.tensor_scalar(out=ge[:], in0=pC[:, 0:32], scalar1=CAP - 0.5, scalar2=None, op0=ALU.is_ge), R=[bpC], W=[b_ge])
            k.op(dve, lambda e: e.tensor_tensor(out=d1[:], in0=mid[:], in1=lo[:], op=ALU.subtract), R=[b_mid, b_lo], W=[b_d1])
            k.op(dve, lambda e: e.tensor_tensor(out=d1[:], in0=d1[:], in1=ge[:], op=ALU.mult), R=[b_d1, b_ge], W=[b_d1])
            k.op(dve, lambda e: e.tensor_tensor(out=lo[:], in0=lo[:], in1=d1[:], op=ALU.add), R=[b_lo, b_d1], W=[b_lo])
            k.op(dve, lambda e: e.tensor_tensor(out=d1[:], in0=hi[:], in1=mid[:], op=ALU.subtract), R=[b_hi, b_mid], W=[b_d1])
            k.op(dve, lambda e: e.tensor_tensor(out=d1[:], in0=d1[:], in1=ge[:], op=ALU.mult), R=[b_d1, b_ge], W=[b_d1])
            k.op(dve, lambda e: e.tensor_tensor(out=hi[:], in0=mid[:], in1=d1[:], op=ALU.add), R=[b_mid, b_d1], W=[b_hi])
            k.op(dve, lambda e: e.tensor_tensor(out=mid[:], in0=lo[:], in1=hi[:], op=ALU.add), R=[b_lo, b_hi], W=[b_mid])
            k.op(dve, lambda e: e.tensor_scalar(out=mid[:], in0=mid[:], scalar1=0.5, scalar2=None, op0=ALU.mult), R=[b_mid], W=[b_mid])
        k.op(dve, lambda e: e.tensor_copy(out=thr[:], in_=lo[:]), R=[b_lo], W=[b_thr])

    TBT = 8
    RS = 256
    NBLK = NTL // TBT
    b_yacc = [Buf(f"yaccb{i}") for i in range(NBLK)]
    hn_d = nc.dram_tensor("hn_d", [NT, D], BF16).ap()
    b_hn_d = Buf("hn_d")
    gm, b_gm = T("gm2", [128, NTL, 16], F32)
    slotidx, b_slot = T("slotidx", [128, NTL, 16], F32)
    triU, b_triU = T("triU", [128, 128], F32)
    k.op(pool, lambda e: e.affine_select(out=triU[:], in_=onesf[:], pattern=[[1, 128]], compare_op=ALU.is_ge, fill=0.0, base=0, channel_multiplier=-1), R=[b_ones], W=[b_triU])
    iota_f, b_iota = T("iota_f", [128, RS], F32)
    k.op(pool, lambda e: e.iota(iota_f[:], pattern=[[1, RS]], base=0, channel_multiplier=0, allow_small_or_imprecise_dtypes=True), W=[b_iota])
    cnt = {"ph": 0}
    with k.scope():
        xt = [T(f"xts{i}", [128, D], F32) for i in range(2)]
        junk, b_junk = T("junk_s", [128, D], F32)
        st_s, b_sts = T("st_ss", [128, 8], F32)
        hbs = [T(f"hbs{i}", [128, D], BF16) for i in range(2)]
        gffn, b_gffn = T("gffn_s", [128, D], F32)
        k.dma(sp, gffn[:], g_ffn[0:1, :].to_broadcast([128, D]), W=[b_gffn])
        afl, b_afl = T("afl", [128, 16], F32)
        msk, b_msk = T("msk", [128, 16], F32)
        basev, b_base = T("basev", [128, 16], F32)
        slv, b_slv = T("slv", [128, 16], F32)
        for t in range(NTL):
            g = 0 if t < NTL // 2 else 1
            xtile, bx = xt[t % 2]
            k.dma(sp, xtile[:], x1_in[t * 128:(t + 1) * 128, :], W=[bx])
            k.dma(sp, yacc_d[t * 128:(t + 1) * 128, :], xtile[:], R=[bx], W=[b_yacc[t // TBT]])
            k.dma(sp, afl[:], aff_loc[t * 128:(t + 1) * 128, :], W=[b_afl])
            k.op(dve, lambda e: e.tensor_tensor(out=msk[:], in0=afl[:], in1=thr[:, g * 16:(g + 1) * 16], op=ALU.is_ge), R=[b_afl, b_thr], W=[b_msk])
            k.op(dve, lambda e: e.tensor_tensor(out=gm[:, t, :], in0=afl[:], in1=msk[:], op=ALU.mult), R=[b_afl, b_msk], W=[b_gm])
            if t % TBT == 0:
                k.op(pool, lambda e: e.memset(basev[:], 0.0), W=[b_base])
            pC, bpC = PB[t % 2]
            k.op(pe, lambda e: e.matmul(pC[:, 0:16], lhsT=triU[:, :], rhs=msk[:, :], start=True, stop=True), R=[b_triU, b_msk], W=[bpC])
            k.op(pe, lambda e: e.matmul(pC[:, 16:32], lhsT=onesf[:, :], rhs=msk[:, :], start=True, stop=True), R=[b_ones, b_msk], W=[bpC])
            k.op(dve, lambda e: e.tensor_tensor(out=slv[:], in0=pC[:, 0:16], in1=basev[:], op=ALU.add), R=[bpC, b_base], W=[b_slv])
            k.op(dve, lambda e: e.tensor_tensor(out=slv[:], in0=slv[:], in1=msk[:], op=ALU.mult), R=[b_slv, b_msk], W=[b_slv])
            k.op(dve, lambda e: e.tensor_scalar(out=slotidx[:, t, :], in0=slv[:], scalar1=-1.0, scalar2=None, op0=ALU.add), R=[b_slv], W=[b_slot])
            k.op(dve, lambda e: e.tensor_tensor(out=basev[:], in0=basev[:], in1=pC[:, 16:32], op=ALU.add), R=[b_base, bpC], W=[b_base])
            hb, b_hb = hbs[t % 2]
            rmsnorm(xtile[:], bx, gffn[:], b_gffn, hb[:], b_hb)
            k.dma(sp, hn_d[t * 128:(t + 1) * 128, :], hb[:], R=[b_hb], W=[b_hn_d])

    with k.scope():
        stg = [T(f"stgm{i}", [128, 1024], F32) for i in range(2)]
        wg, b_wg = T("wg", [128, 8, FF], BF16)
        wu, b_wu = T("wu", [128, 8, FF], BF16)
        wd, b_wd = T("wd", [128, NFC, D], BF16)
        hnt = [T(f"hnt{i}", [128, D], BF16) for i in range(2)]
        Sm, b_S = T("Sm", [128, TBT, RS], BF16)
        STm, b_ST = T("STm", [128, TBT, 2, 128], BF16)
        xsT, b_xsT = T("xsT", [128, 8, RS], BF16)
        actb, b_actb = T("actb", [128, NFC, RS], BF16)
        sg, b_sg = T("sg", [128, RS], F32)
        yeb, b_yeb = T("yeb", [128, 2, D], BF16)
        ya, b_ya = T("ya", [128, D], F32)
        yo, b_yo = T("yo", [128, D], F32)
        sc = {"stg": 0}

        def load_cast(dst_ap, src_ap, bdst, c):
            (st, bst) = stg[sc["stg"] % 2]
            sc["stg"] += 1
            sv = st[:, 0:src_ap.shape[1] * src_ap.shape[2]].rearrange("p (c n) -> p c n", c=src_ap.shape[1])
            k.dma(sp, sv, src_ap, W=[bst])
            eng = [dve, pool, act][c % 3]
            if eng is act:
                k.op(act, lambda e: e.copy(out=dst_ap, in_=sv), R=[bst], W=[bdst])
            else:
                k.op(eng, lambda e: e.tensor_copy(out=dst_ap, in_=sv), R=[bst], W=[bdst])

        for ex in range(n_exp):
            c = 0
            for (wsrc, wdst, bw) in ((w_gate, wg, b_wg), (w_up, wu, b_wu)):
                for n0 in range(0, FF, 128):
                    load_cast(wdst[:, :, n0:n0 + 128], wsrc[ex, :, n0:n0 + 128].rearrange("(c p) n -> p c n", p=128), bw, c)
                    c += 1
            for f0 in range(0, NFC):
                load_cast(wd[:, f0:f0 + 1, :], w_down[ex, f0 * 128:(f0 + 1) * 128, :].rearrange("(c p) n -> p c n", p=128), b_wd, c)
                c += 1
            for blk in range(NBLK):
                for j in range(TBT):
                    tl = blk * TBT + j
                    k.op(dve, lambda e: e.tensor_scalar(out=Sm[:, j, :], in0=iota_f[:], scalar1=slotidx[:, tl, ex:ex + 1], scalar2=None, op0=ALU.is_equal), R=[b_iota, b_slot], W=[b_S])
                    ht, bht = hnt[j % 2]
                    k.dma(sp, ht[:], hn_d[tl * 128:(tl + 1) * 128, :], R=[b_hn_d], W=[bht])
                    for kc in range(8):
                        pg_, bpg_ = PB[kc // 2]
                        k.op(pe, lambda e: e.matmul(pg_[:, (kc % 2) * RS:(kc % 2 + 1) * RS], lhsT=ht[:, kc * 128:(kc + 1) * 128], rhs=Sm[:, j, :], start=(j == 0 and kc % 2 == 0), stop=(j == TBT - 1), skip_group_check=True), R=[bht, b_S], W=[bpg_])
                for i in range(4):
                    pg_, bpg_ = PB[i]
                    if i % 2 == 0:
                        k.op(act, lambda e: e.copy(out=xsT[:, 2 * i:2 * i + 2, :], in_=pg_[:, :].rearrange("p (c s) -> p c s", c=2)), R=[bpg_], W=[b_xsT])
                    else:
                        k.op(dve, lambda e: e.tensor_copy(out=xsT[:, 2 * i:2 * i + 2, :], in_=pg_[:, :].rearrange("p (c s) -> p c s", c=2)), R=[bpg_], W=[b_xsT])
                for hh in range(2):
                    ph, bph = PH[hh]
                    for jj in range(4):
                        j = hh * 4 + jj
                        for st_ in range(2):
                            k.op(pe, lambda e: e.transpose(out=ph[:, (jj * 2 + st_) * 128:(jj * 2 + st_ + 1) * 128], in_=Sm[:, j, st_ * 128:(st_ + 1) * 128], identity=ident[:]), R=[b_S, b_id], W=[bph])
                    if hh == 0:
                        k.op(act, lambda e: e.copy(out=STm[:, 0:4, :, :], in_=ph[:, :].rearrange("p (j s t) -> p j s t", j=4, s=2)), R=[bph], W=[b_ST])
                    else:
                        k.op(dve, lambda e: e.tensor_copy(out=STm[:, 4:8, :, :], in_=ph[:, :].rearrange("p (j s t) -> p j s t", j=4, s=2)), R=[bph], W=[b_ST])
                for fc in range(NFC):
                    pGU, bpGU = PB[4 + fc % 2]
                    for kc in range(8):
                        k.op(pe, lambda e: e.matmul(pGU[:, 0:RS], lhsT=wg[:, kc, fc * 128:(fc + 1) * 128], rhs=xsT[:, kc, :], start=(kc == 0), stop=(kc == 7)), R=[b_wg, b_xsT], W=[bpGU])
                    for kc in range(8):
                        k.op(pe, lambda e: e.matmul(pGU[:, RS:2 * RS], lhsT=wu[:, kc, fc * 128:(fc + 1) * 128], rhs=xsT[:, kc, :], start=(kc == 0), stop=(kc == 7)), R=[b_wu, b_xsT], W=[bpGU])
                    k.op(act, lambda e: e.activation(out=sg[:], in_=pGU[:, 0:RS], func=AF.Silu), R=[bpGU], W=[b_sg])
                    k.op(dve, lambda e: e.tensor_tensor(out=actb[:, fc, :], in0=sg[:], in1=pGU[:, RS:2 * RS], op=ALU.mult), R=[b_sg, bpGU], W=[b_actb])
                for st_ in range(2):
                    for half in range(2):
                        pY, bpY = PB[st_ * 2 + half]
                        for fc in range(NFC):
                            k.op(pe, lambda e: e.matmul(pY[:, :], lhsT=actb[:, fc, st_ * 128:(st_ + 1) * 128], rhs=wd[:, fc, half * 512:(half + 1) * 512], start=(fc == 0), stop=(fc == NFC - 1)), R=[b_actb, b_wd], W=[bpY])
                        if half == 0:
                            k.op(act, lambda e: e.copy(out=yeb[:, st_, 0:512], in_=pY[:, :]), R=[bpY], W=[b_yeb])
                        else:
                            k.op(dve, lambda e: e.tensor_copy(out=yeb[:, st_, 512:1024], in_=pY[:, :]), R=[bpY], W=[b_yeb])
                for j in range(TBT):
                    tl = blk * TBT + j
                    trow = yacc_d[tl * 128:(tl + 1) * 128, :]
                    k.dma(sp, ya[:], trow, R=[b_yacc[blk]], W=[b_ya])
                    for half in range(2):
                        pZ, bpZ = PB[(j % 2) * 2 + half]
                        for st_ in range(2):
                            k.op(pe, lambda e: e.matmul(pZ[:, :], lhsT=STm[:, j, st_, :], rhs=yeb[:, st_, half * 512:(half + 1) * 512], start=(st_ == 0), stop=(st_ == 1)), R=[b_ST, b_yeb], W=[bpZ])
                        k.op(dve, lambda e: e.scalar_tensor_tensor(out=yo[:, half * 512:(half + 1) * 512], in0=pZ[:, :], scalar=gm[:, tl, ex:ex + 1], in1=ya[:, half * 512:(half + 1) * 512], op0=ALU.mult, op1=ALU.add), R=[bpZ, b_gm, b_ya], W=[b_yo])
                    k.dma(sp, trow, yo[:], R=[b_yo], W=[b_yacc[blk]])

    with k.scope():
        stg = [T(f"stgp{i}", [128, 1024], F32) for i in range(2)]
        xt = [T(f"xtp{i}", [128, D], F32) for i in range(2)]
        junk, b_junk = T("junk_p", [128, D], F32)
        st_s, b_sts = T("st_sp", [128, 8], F32)
        hb, b_hb = T("hb_p", [128, D], BF16)
        hnT, b_hnT = T("hnT_p", [128, 8, 128], BF16)
        wpg, b_wpg = T("wpg", [128, 8, D], BF16)
        wple, b_wple = T("wple", [128, 2, D], BF16)
        gpg, b_gpg = T("gpg", [128, D], F32)
        gple, b_gple = T("gple", [128, D], F32)
        gfin, b_gfin = T("gfin", [128, D], F32)
        for (gt, bg, src) in ((gpg, b_gpg, g_pg), (gple, b_gple, g_ple), (gfin, b_gfin, g_fin)):
            k.dma(sp, gt[:], src[0:1, :].to_broadcast([128, D]), W=[bg])
        for c in range(8):
            st, bst = stg[c % 2]
            k.dma(sp, st[:, 0:D], w_pg[c * 128:(c + 1) * 128, :], W=[bst])
            k.op(dve, lambda e: e.tensor_copy(out=wpg[:, c, :], in_=st[:, 0:D]), R=[bst], W=[b_wpg])
        for c in range(2):
            st, bst = stg[c % 2]
            k.dma(sp, st[:, 0:D], w_ple[c * 128:(c + 1) * 128, :], W=[bst])
            k.op(dve, lambda e: e.tensor_copy(out=wple[:, c, :], in_=st[:, 0:D]), R=[bst], W=[b_wple])
        pt_, b_pt = T("pt", [128, 256], F32)
        pb_, b_pb = T("pbf", [128, 256], BF16)
        pT_, b_pT = T("pT", [128, 2, 128], BF16)
        er, b_er = T("er", [128, D], F32)
        ev, b_ev = T("ev", [128, D], F32)
        gs, b_gs = T("gs", [128, D], F32)
        yt, b_yt = T("yt", [128, D], F32)
        for t in range(NTL):
            xtile, bx = xt[t % 2]
            k.dma(sp, xtile[:], yacc_d[t * 128:(t + 1) * 128, :], R=[b_yacc[t // TBT]], W=[bx])
            k.dma(sp, pt_[:], p_in[t * 128:(t + 1) * 128, :], W=[b_pt])
            k.op(pool, lambda e: e.tensor_copy(out=pb_[:], in_=pt_[:]), R=[b_pt], W=[b_pb])
            ph, bph = PH[cnt["ph"] % 2]
            cnt["ph"] += 1
            for c in range(2):
                k.op(pe, lambda e: e.transpose(out=ph[:, c * 128:(c + 1) * 128], in_=pb_[:, c * 128:(c + 1) * 128], identity=ident[:]), R=[b_pb, b_id], W=[bph])
            k.op(act, lambda e: e.copy(out=pT_[:], in_=ph[:, 0:256].rearrange("p (c t) -> p c t", c=2)), R=[bph], W=[b_pT])
            for half in range(2):
                pE, bpE = PB[half]
                for c in range(2):
                    k.op(pe, lambda e: e.matmul(pE[:, :], lhsT=pT_[:, c, :], rhs=wple[:, c, half * 512:(half + 1) * 512], start=(c == 0), stop=(c == 1)), R=[b_pT, b_wple], W=[bpE])
                k.op(act, lambda e: e.copy(out=er[:, half * 512:(half + 1) * 512], in_=pE[:, :]), R=[bpE], W=[b_er])
            rmsnorm(er[:], b_er, gple[:], b_gple, ev[:], b_ev)
            rmsnorm(xtile[:], bx, gpg[:], b_gpg, hb[:], b_hb)
            ph, bph = PH[cnt["ph"] % 2]
            cnt["ph"] += 1
            for c in range(8):
                k.op(pe, lambda e: e.transpose(out=ph[:, c * 128:(c + 1) * 128], in_=hb[:, c * 128:(c + 1) * 128], identity=ident[:]), R=[b_hb, b_id], W=[bph])
            k.op(act, lambda e: e.copy(out=hnT[:], in_=ph[:, :].rearrange("p (c t) -> p c t", c=8)), R=[bph], W=[b_hnT])
            for half in range(2):
                pE, bpE = PB[2 + half]
                for c in range(8):
                    k.op(pe, lambda e: e.matmul(pE[:, :], lhsT=hnT[:, c, :], rhs=wpg[:, c, half * 512:(half + 1) * 512], start=(c == 0), stop=(c == 7)), R=[b_hnT, b_wpg], W=[bpE])
                k.op(act, lambda e: e.activation(out=gs[:, half * 512:(half + 1) * 512], in_=pE[:, :], func=AF.Sigmoid), R=[bpE], W=[b_gs])
            k.op(dve, lambda e: e.tensor_tensor(out=gs[:], in0=gs[:], in1=ev[:], op=ALU.mult), R=[b_gs, b_ev], W=[b_gs])
            k.op(dve, lambda e: e.tensor_tensor(out=gs[:], in0=gs[:], in1=xtile[:], op=ALU.add), R=[b_gs, bx], W=[b_gs])
            rmsnorm(gs[:], b_gs, gfin[:], b_gfin, yt[:], b_yt)
            k.dma(sp, y_out[t * 128:(t + 1) * 128, :], yt[:], R=[b_yt], W=[b_y])
    k.finish([b_y])
    return nc, k


_CACHE = {}


def kernel(x_prompt, x_sample, p_prompt, p_sample, rel_bias, g_mix, w_in, conv_w, conv_b, dt_bias, a_log, d_skip, g_ssd,
           w_out, g_ffn, w_router, w_gate, w_up, w_down, g_pg, w_pg, w_ple, g_ple, g_final):
    f = lambda a: np.ascontiguousarray(np.asarray(a, dtype=np.float32))
    xp, xs_ = f(x_prompt), f(x_sample)
    B, S1, _ = xp.shape
    B2, S2, _ = xs_.shape
    pb, sb_ = B // NCORES, B2 // NCORES
    seqs = []
    r = 0
    for i in range(pb):
        seqs.append((r, S1))
        r += S1
    for i in range(sb_):
        seqs.append((r, S2))
        r += S2
    NT = r
    nca, _ = build_a(seqs)
    common = dict(w_in=f(w_in[0]), w_out=f(w_out[0]), w_router=f(w_router[0]), g_mix=f(g_mix[0])[None], g_ffn=f(g_ffn[0])[None],
                  conv_w=f(conv_w[0]), conv_b=f(conv_b[0])[None], dt_bias=f(dt_bias[0]).reshape(1, 16), a_log=f(a_log[0]).reshape(1, 16),
                  d_skip=f(d_skip[0])[None], g_ssd=f(g_ssd[0])[None], rel_bias=f(rel_bias), oh=_onehot_tables())
    xcore = []
    for c in range(NCORES):
        xcore.append(np.concatenate([xp[c * pb:(c + 1) * pb].reshape(-1, D), xs_[c * sb_:(c + 1) * sb_].reshape(-1, D)], 0))
    ra = run_bass_kernel_spmd(nca, [dict(common, x=xcore[c]) for c in range(NCORES)], core_ids=list(range(NCORES)))
    x1 = [ra.results[c]["x1"] for c in range(NCORES)]
    aff = [ra.results[c]["aff"] for c in range(NCORES)]
    n1 = pb * S1
    aff_all = np.stack([np.concatenate([a[:n1] for a in aff], 0), np.concatenate([a[n1:] for a in aff], 0)], 0)
    TG = aff_all.shape[1]
    assert n1 == NT - n1 and TG == NCORES * n1
    ncb, _ = build_b(NT, TG)
    pp, ps_ = f(p_prompt[0]), f(p_sample[0])
    commonb = dict(aff_all=np.ascontiguousarray(aff_all), w_gate=f(w_gate[0]), w_up=f(w_up[0]), w_down=f(w_down[0]), g_ffn=f(g_ffn[0])[None],
                   g_pg=f(g_pg[0])[None], g_ple=f(g_ple[0])[None], g_final=f(g_final)[None], w_pg=f(w_pg[0]), w_ple=f(w_ple[0]))
    inb = []
    for c in range(NCORES):
        pc = np.concatenate([pp[c * pb:(c + 1) * pb].reshape(-1, 256), ps_[c * sb_:(c + 1) * sb_].reshape(-1, 256)], 0)
        inb.append(dict(commonb, x1=x1[c], aff_loc=aff[c], p=pc))
    rb = run_bass_kernel_spmd(ncb, inb, core_ids=list(range(NCORES)))
    ys = [rb.results[c]["y"] for c in range(NCORES)]
    y_p = np.concatenate([y[:n1] for y in ys], 0).reshape(B, S1, D)
    y_s = np.concatenate([y[n1:] for y in ys], 0).reshape(B2, S2, D)
    return y_p, y_s
```

```python
import math
from contextlib import ExitStack
import numpy as np
import concourse.bass as bass
import concourse.mybir as mybir
from concourse.bass_utils import run_bass_kernel_spmd

F32 = mybir.dt.float32
BF16 = mybir.dt.bfloat16
ALU = mybir.AluOpType
AF = mybir.ActivationFunctionType
AX = mybir.AxisListType

NCORES = 8
D = 1024
PAD = 1024
NEG = -30000.0
EPS = 1e-6
DILS = (1, 4, 16)


class Buf:
    __slots__ = ("name", "w", "r", "excl")

    def __init__(self, name, excl=False):
        self.name = name
        self.excl = excl
        self.w = {}
        self.r = {}


class Sem:
    __slots__ = ("h", "total")

    def __init__(self, h):
        self.h = h
        self.total = 0


class Eng:
    def __init__(self, name, handle, sem):
        self.name = name
        self.h = handle
        self.sem = sem
        self.waited = {}
        self.ring = []
        self.rpos = 0


class K:
    def __init__(self, nc, sp_ring=44, pool_ring=20, act_ring=12):
        self.nc = nc
        self.es = ExitStack()
        self.es_cur = self.es
        self.pe = self._eng("pe", nc.tensor)
        self.act = self._eng("act", nc.scalar)
        self.dve = self._eng("dve", nc.vector)
        self.pool = self._eng("pool", nc.gpsimd)
        self.sp = self._eng("sp", nc.sync)
        for e, n in ((self.sp, sp_ring), (self.pool, pool_ring), (self.act, act_ring)):
            e.ring = [self.new_sem(f"r_{e.name}{i}") for i in range(n)]
        self.n_ins = 0

    def new_sem(self, name):
        return Sem(self.es.enter_context(self.nc.semaphore(name)))

    def _eng(self, name, handle):
        return Eng(name, handle, self.new_sem("e_" + name))

    def sb(self, name, shape, dtype):
        self.uid = getattr(self, "uid", 0) + 1
        return self.es_cur.enter_context(self.nc.sbuf_tensor(f"{name}_{self.uid}", list(shape), dtype))

    def barrier(self):
        if getattr(self, "dead", False):
            return
        engs = [self.pe, self.act, self.dve, self.pool, self.sp]
        for e in engs:
            for o in engs:
                if o is not e and o.sem.total > 0:
                    self._wait(e, o.sem, o.sem.total)
                for s in o.ring:
                    if s.total > 0:
                        self._wait(e, s, s.total)

    def scope(self):
        return _Scope(self)

    def ps(self, name, shape, dtype):
        return self.es.enter_context(self.nc.psum_tensor(name, list(shape), dtype))

    def _wait(self, eng, s, v):
        if eng.waited.get(id(s), 0) < v:
            eng.h.wait_ge(s.h, v)
            eng.waited[id(s)] = v
            self.n_ins += 1

    def _deps(self, eng, reads, writes):
        for b in reads:
            for s, v in b.w.values():
                self._wait(eng, s, v)
        for b in writes:
            for s, v in b.w.values():
                self._wait(eng, s, v)
            for s, v in b.r.values():
                self._wait(eng, s, v)

    def _record(self, ev, reads, writes):
        s, v = ev
        for b in writes:
            b.w[id(s)] = ev
            b.r = {}
        for b in reads:
            b.r[id(s)] = ev

    def op(self, eng, fn, R=(), W=()):
        if getattr(self, "dead", False):
            return None
        ex = [b for b in R if b.excl]
        if ex:
            W = list(W) + ex
        self._deps(eng, R, W)
        ins = fn(eng.h)
        eng.sem.total += 1
        ins.then_inc(eng.sem.h, 1)
        self.n_ins += 1
        self._record((eng.sem, eng.sem.total), R, W)
        return ins

    def dma(self, eng, out, in_, R=(), W=(), **kw):
        if getattr(self, "dead", False):
            return None
        self._deps(eng, R, W)
        s = eng.ring[eng.rpos]
        eng.rpos = (eng.rpos + 1) % len(eng.ring)
        self._wait(eng, s, s.total)
        ins = eng.h.dma_start(out=out, in_=in_, **kw)
        s.total += 16
        ins.then_inc(s.h, 16)
        self.n_ins += 1
        self._record((s, s.total), R, W)
        return ins

    def finish(self, bufs):
        self._deps(self.sp, bufs, [])

    def close(self):
        self.es.close()

    def __del__(self):
        pass


class _Scope:
    def __init__(self, k):
        self.k = k

    def __enter__(self):
        self.k.barrier()
        self.old = self.k.es_cur
        self.es = ExitStack()
        self.es.__enter__()
        self.k.es_cur = self.es
        return self

    def __exit__(self, *a):
        self.k.barrier()
        self.k.es_cur = self.old
        return self.es.__exit__(*a)


def _t5_bucket(rel):
    half = 16
    max_exact = 8
    n = np.abs(rel)
    large = max_exact + (np.log(np.maximum(n, 1) / max_exact) / math.log(1024 / max_exact) * (half - max_exact)).astype(np.int32)
    large = np.minimum(large, half - 1)
    return (np.where(rel > 0, half, 0) + np.where(n < max_exact, n, large)).astype(np.int32)


def _onehot_tables():
    oh = np.zeros((3, 33, 384), np.float32)
    for p, d in enumerate(DILS):
        for i in range(384):
            ds = i - 191
            if abs(ds) <= 64:
                oh[p, int(_t5_bucket(np.array(ds * d))), i] = 1.0
            else:
                oh[p, 32, i] = 1.0
    return oh


def build_a(seqs, dbg=False, upto=9):
    nc, k = _build_a(seqs, dbg, upto)
    k.close()
    return nc, k.n_ins


def _build_a(seqs, dbg=False, upto=9):
    NT = sum(s for _, s in seqs)
    SMAX = max(s for _, s in seqs)
    nc = bass.Bass("TRN2", target_bir_lowering=False)
    k = K(nc)

    def din(name, shape):
        return nc.dram_tensor(name, list(shape), F32, kind="ExternalInput").ap()

    x_in = din("x", [NT, D])
    w_in = din("w_in", [D, 3088])
    w_out = din("w_out", [D, D])
    w_router = din("w_router", [D, 16])
    g_mix = din("g_mix", [1, D])
    g_ffn = din("g_ffn", [1, D])
    conv_w = din("conv_w", [5, 1024])
    conv_b = din("conv_b", [1, 1024])
    dt_bias = din("dt_bias", [1, 16])
    a_log = din("a_log", [1, 16])
    d_skip = din("d_skip", [1, 8])
    g_ssd = din("g_ssd", [1, 512])
    rel_bias = din("rel_bias", [32, 8])
    oh_in = din("oh", [3, 33, 384])
    x1_out = nc.dram_tensor("x1", [NT, D], F32, kind="ExternalOutput").ap()
    aff_out = nc.dram_tensor("aff", [NT, 16], F32, kind="ExternalOutput").ap()
    b_x1 = Buf("x1o")
    b_aff = Buf("affo")

    def dscr(name, shape, dt):
        return nc.dram_tensor(name, list(shape), dt).ap(), Buf(name)

    qT_d, b_qT = dscr("qT_d", [512, SMAX], BF16)
    kT_d, b_kT = dscr("kT_d", [512, SMAX], BF16)
    vT_d, b_vT = dscr("vT_d", [512, SMAX], BF16)
    xbc_d, b_xbc = dscr("xbc_d", [1024, SMAX + 4], F32)
    z_d, b_z = dscr("z_d", [SMAX, 512], F32)
    dt_d, b_dt = dscr("dt_d", [SMAX, 16], F32)
    yf_d, b_yf = dscr("yf_d", [SMAX, 512], F32)
    at_d, b_at = dscr("at_d", [8, 64, SMAX], BF16)
    tab_d_t = nc.dram_tensor("tab_d", [3, 8, 384], F32)
    tab_d = tab_d_t.ap()
    b_tab = Buf("tab_d")

    def T(name, shape, dt):
        return k.sb(name, shape, dt), Buf(name)

    w_in_bf, b_win = T("w_in_bf", [128, 8, 3088], BF16)
    wo_att, b_woa = T("wo_att", [64, 8, 1024], BF16)
    wo_ssd, b_wos = T("wo_ssd", [128, 4, 1024], BF16)
    wr_bf, b_wr = T("wr_bf", [128, 8, 16], BF16)
    gmix, b_gmix = T("gmix", [128, D], F32)
    gffn, b_gffn = T("gffn", [128, D], F32)
    gssd, b_gssd = T("gssd", [128, 512], F32)
    cw, b_cw = T("cw", [128, 5, 8], F32)
    cb, b_cb = T("cb", [128, 8], F32)
    dtb, b_dtb = T("dtb", [128, 16], F32)
    Abc, b_A = T("Abc", [128, 16], F32)
    dsk, b_dsk = T("dsk", [128, 8], F32)
    identf, b_idf = T("identf", [128, 128], F32)
    ident, b_id = T("ident", [128, 128], BF16)
    triU, b_triU = T("triU", [128, 128], F32)
    triL, b_triL = T("triL", [128, 128], F32)
    nmU, b_nmU = T("nmU", [128, 128], F32)
    nmL, b_nmL = T("nmL", [128, 128], F32)
    onesf, b_ones = T("onesf", [128, 128], F32)
    biasT, b_bias = T("biasT", [128, 12, 512], BF16)

    PB = []
    for i in range(6):
        PB.append((k.ps(f"pb{i}", [128, 512], F32), Buf(f"pb{i}", excl=True)))
    PH = []
    for i in range(2):
        PH.append((k.ps(f"ph{i}", [128, 1024], BF16), Buf(f"ph{i}", excl=True)))

    sp, act, dve, pool, pe = k.sp, k.act, k.dve, k.pool, k.pe

    def bc_load(dst, bdst, src_row, n):
        k.dma(sp, dst[:], src_row.to_broadcast([128, n]), W=[bdst])

    bc_load(gmix, b_gmix, g_mix[0:1, :], D)
    bc_load(gffn, b_gffn, g_ffn[0:1, :], D)
    bc_load(gssd, b_gssd, g_ssd[0:1, :], 512)
    bc_load(dtb, b_dtb, dt_bias[0:1, :], 16)
    bc_load(Abc, b_A, a_log[0:1, :], 16)
    bc_load(dsk, b_dsk, d_skip[0:1, :], 8)
    for kk in range(5):
        k.dma(sp, cw[:, kk, :], conv_w[kk:kk + 1, :].rearrange("o (c p) -> p (o c)", p=128), W=[b_cw], allow_slow_non_contiguous=True)
    k.dma(sp, cb[:], conv_b.rearrange("o (c p) -> p (o c)", p=128), W=[b_cb], allow_slow_non_contiguous=True)
    k.op(act, lambda e: e.activation(out=Abc[:], in_=Abc[:], func=AF.Exp), R=[b_A], W=[b_A])
    k.op(dve, lambda e: e.tensor_scalar(out=Abc[:], in0=Abc[:], scalar1=-1.0, scalar2=None, op0=ALU.mult), R=[b_A], W=[b_A])

    k.op(pool, lambda e: e.memset(onesf[:], 1.0), W=[b_ones])
    k.op(pool, lambda e: e.memset(identf[:], 0.0), W=[b_idf])
    k.op(pool, lambda e: e.affine_select(out=identf[:], in_=identf[:], pattern=[[-1, 128]], compare_op=ALU.not_equal, fill=1.0, base=0, channel_multiplier=1), R=[b_idf], W=[b_idf])
    k.op(dve, lambda e: e.tensor_copy(out=ident[:], in_=identf[:]), R=[b_idf], W=[b_id])
    k.op(pool, lambda e: e.affine_select(out=triU[:], in_=onesf[:], pattern=[[1, 128]], compare_op=ALU.is_ge, fill=0.0, base=0, channel_multiplier=-1), R=[b_ones], W=[b_triU])
    k.op(pool, lambda e: e.affine_select(out=triL[:], in_=onesf[:], pattern=[[-1, 128]], compare_op=ALU.is_ge, fill=0.0, base=0, channel_multiplier=1), R=[b_ones], W=[b_triL])
    k.op(dve, lambda e: e.tensor_scalar(out=nmU[:], in0=triU[:], scalar1=-1.0, scalar2=-NEG, op0=ALU.add, op1=ALU.mult), R=[b_triU], W=[b_nmU])
    k.op(dve, lambda e: e.tensor_scalar(out=nmL[:], in0=triL[:], scalar1=-1.0, scalar2=-NEG, op0=ALU.add, op1=ALU.mult), R=[b_triL], W=[b_nmL])

    with k.scope():
        stg, b_stg = T("stg", [128, 2048], F32)
        relx, b_relx = T("relx", [33, 8], F32)
        oh_sb, b_oh = T("oh_sb", [33, 3, 384], F32)
        tab_sb, b_tabsb = T("tab_sb", [8, 3, 384], F32)
        for c0 in range(0, 3088, 256):
            cn = min(256, 3088 - c0)
            sv = stg[:, 0:8 * cn].rearrange("p (c n) -> p c n", c=8)
            k.dma(sp, sv, w_in[:, c0:c0 + cn].rearrange("(c p) n -> p c n", p=128), W=[b_stg])
            k.op(dve, lambda e: e.tensor_copy(out=w_in_bf[:, :, c0:c0 + cn], in_=sv), R=[b_stg], W=[b_win])
        for hh in range(4):
            sv = stg[0:64, :].rearrange("p (h n) -> p h n", h=2)
            k.dma(sp, sv, w_out[hh * 128:(hh + 1) * 128, :].rearrange("(h p) n -> p h n", p=64), W=[b_stg])
            k.op(dve, lambda e: e.tensor_copy(out=wo_att[:, hh * 2:(hh + 1) * 2, :], in_=sv), R=[b_stg], W=[b_woa])
        for hh in range(2):
            sv = stg[:, :].rearrange("p (c n) -> p c n", c=2)
            k.dma(sp, sv, w_out[512 + hh * 256:512 + (hh + 1) * 256, :].rearrange("(c p) n -> p c n", p=128), W=[b_stg])
            k.op(dve, lambda e: e.tensor_copy(out=wo_ssd[:, hh * 2:(hh + 1) * 2, :], in_=sv), R=[b_stg], W=[b_wos])
        sv = stg[:, 0:128].rearrange("p (c n) -> p c n", c=8)
        k.dma(sp, sv, w_router.rearrange("(c p) n -> p c n", p=128), W=[b_stg])
        k.op(dve, lambda e: e.tensor_copy(out=wr_bf[:], in_=sv), R=[b_stg], W=[b_wr])

        k.op(pool, lambda e: e.memset(relx[:], NEG), W=[b_relx])
        k.dma(sp, relx[0:32, :], rel_bias[:, :], W=[b_relx])
        k.dma(sp, oh_sb[:], oh_in.rearrange("p b i -> b p i"), W=[b_oh])
        for p in range(3):
            pt, bpt = PB[p % 2]
            k.op(pe, lambda e: e.matmul(pt[0:8, 0:384], lhsT=relx[:, :], rhs=oh_sb[:, p, :], start=True, stop=True), R=[b_relx, b_oh], W=[bpt])
            k.op(act, lambda e: e.copy(out=tab_sb[:, p, :], in_=pt[0:8, 0:384]), R=[bpt], W=[b_tabsb])
        k.dma(sp, tab_d.rearrange("p h i -> h p i"), tab_sb[:], R=[b_tabsb], W=[b_tab])
        for hp in range(4):
            for p in range(3):
                for h in range(2):
                    for jj in range(2):
                        src = bass.AP(tensor=tab_d_t, offset=(p * 8 + hp * 2 + h) * 384 + 127 + 128 * jj, ap=[[1, 128], [-1, 128]])
                        c0 = (h * 2 + jj) * 128
                        k.dma(sp, stg[:, c0:c0 + 128], src, R=[b_tab], W=[b_stg], allow_slow_non_contiguous=True)
                k.op(dve, lambda e: e.tensor_copy(out=biasT[:, hp * 3 + p, :], in_=stg[:, 0:512]), R=[b_stg], W=[b_bias])

    xt = [T(f"xt{i}", [128, D], F32) for i in range(2)]
    junk, b_junk = T("junk", [128, D], F32)
    st_s, b_sts = T("st_s", [128, 8], F32)
    hb, b_hb = T("hb", [128, D], BF16)
    hT, b_hT = T("hT", [128, 8, 512], BF16)
    evb = [T(f"evb{i}", [128, 512], BF16) for i in range(3)]
    evf = [T(f"evf{i}", [128, 512], F32) for i in range(3)]
    zpad, b_zpad = T("zpad", [128, 8, 2], F32)
    k.op(pool, lambda e: e.memset(zpad[:], 0.0), W=[b_zpad])

    def rmsnorm_to_bf(xtile, bx, gtile, bg, out_bf, bout, n=D):
        k.op(act, lambda e: e.activation(out=junk[:, 0:n], in_=xtile, func=AF.Square, accum_out=st_s[:, 0:1]), R=[bx], W=[b_junk, b_sts])
        k.op(dve, lambda e: e.tensor_scalar(out=st_s[:, 1:2], in0=st_s[:, 0:1], scalar1=1.0 / n, scalar2=EPS, op0=ALU.mult, op1=ALU.add), R=[b_sts], W=[b_sts])
        k.op(act, lambda e: e.activation(out=st_s[:, 2:3], in_=st_s[:, 1:2], func=AF.Sqrt), R=[b_sts], W=[b_sts])
        k.op(dve, lambda e: e.reciprocal(out=st_s[:, 3:4], in_=st_s[:, 2:3]), R=[b_sts], W=[b_sts])
        k.op(dve, lambda e: e.scalar_tensor_tensor(out=out_bf, in0=xtile, scalar=st_s[:, 3:4], in1=gtile, op0=ALU.mult, op1=ALU.mult), R=[bx, b_sts, bg], W=[bout])

    cnt = {"ev": 0, "ph": 0, "pb": 0}

    def done():
        k.barrier()
        k.dead = True
        return nc, k

    if upto == 0:
        return done()

    for (row0, S) in seqs:
        k.dma(sp, xbc_d[:, 0:2].rearrange("(c p) t -> p c t", p=128), zpad[:], R=[b_zpad], W=[b_xbc])
        k.dma(sp, xbc_d[:, S + 2:S + 4].rearrange("(c p) t -> p c t", p=128), zpad[:], R=[b_zpad], W=[b_xbc])
        for blk in range(S // 512):
            for t in range(4):
                xtile, bx = xt[t % 2]
                r = row0 + blk * 512 + t * 128
                k.dma(sp, xtile[:], x_in[r:r + 128, :], W=[bx])
                rmsnorm_to_bf(xtile[:], bx, gmix[:], b_gmix, hb[:], b_hb)
                ph, bph = PH[cnt["ph"] % 2]
                cnt["ph"] += 1
                for c in range(8):
                    k.op(pe, lambda e: e.transpose(out=ph[:, c * 128:(c + 1) * 128], in_=hb[:, c * 128:(c + 1) * 128], identity=ident[:]), R=[b_hb, b_id], W=[bph])
                k.op(act, lambda e: e.copy(out=hT[:, :, t * 128:(t + 1) * 128], in_=ph[:, :].rearrange("p (c t) -> p c t", c=8)), R=[bph], W=[b_hT])
            fcs = [(i, i * 128) for i in range(12)] + [(12 + i, 2048 + i * 128) for i in range(8)]
            for (fi, col0) in fcs:
                pt, bpt = PB[cnt["pb"] % 2]
                cnt["pb"] += 1
                for kc in range(8):
                    k.op(pe, lambda e: e.matmul(pt[:, :], lhsT=w_in_bf[:, kc, col0:col0 + 128], rhs=hT[:, kc, :], start=(kc == 0), stop=(kc == 7)), R=[b_win, b_hT], W=[bpt])
                i = cnt["ev"] % 3
                cnt["ev"] += 1
                if fi < 12:
                    et, bet = evb[i]
                    dst, bd = [(qT_d, b_qT), (kT_d, b_kT), (vT_d, b_vT)][fi // 4]
                    drows = dst[(fi % 4) * 128:(fi % 4 + 1) * 128, blk * 512:(blk + 1) * 512]
                else:
                    et, bet = evf[i]
                    dst, bd = xbc_d, b_xbc
                    drows = dst[(fi - 12) * 128:(fi - 11) * 128, 2 + blk * 512:2 + (blk + 1) * 512]
                if fi % 2 == 0:
                    k.op(act, lambda e: e.copy(out=et[:], in_=pt[:, :]), R=[bpt], W=[bet])
                else:
                    k.op(dve, lambda e: e.tensor_copy(out=et[:], in_=pt[:, :]), R=[bpt], W=[bet])
                k.dma(sp, drows, et[:], R=[bet], W=[bd])
            for t in range(4):
                pt, bpt = PB[2 + t % 2]
                for kc in range(8):
                    k.op(pe, lambda e: e.matmul(pt[:, :], lhsT=hT[:, kc, t * 128:(t + 1) * 128], rhs=w_in_bf[:, kc, 1536:2048], start=(kc == 0), stop=(kc == 7)), R=[b_win, b_hT], W=[bpt])
                i = cnt["ev"] % 3
                cnt["ev"] += 1
                et, bet = evf[i]
                k.op(act, lambda e: e.copy(out=et[:], in_=pt[:, :]), R=[bpt], W=[bet])
                r = blk * 512 + t * 128
                k.dma(sp, z_d[r:r + 128, :], et[:], R=[bet], W=[b_z])
                pt2, bpt2 = PB[4 + t % 2]
                for kc in range(8):
                    k.op(pe, lambda e: e.matmul(pt2[:, 0:16], lhsT=hT[:, kc, t * 128:(t + 1) * 128], rhs=w_in_bf[:, kc, 3072:3088], start=(kc == 0), stop=(kc == 7)), R=[b_win, b_hT], W=[bpt2])
                i = cnt["ev"] % 3
                cnt["ev"] += 1
                et, bet = evf[i]
                k.op(dve, lambda e: e.tensor_copy(out=et[:, 0:16], in_=pt2[:, 0:16]), R=[bpt2], W=[bet])
                k.dma(sp, dt_d[r:r + 128, :], et[:, 0:16], R=[bet], W=[b_dt])

        if upto == 1:
            return done()
        with k.scope():
            qp, b_qp = T("qp", [128, SMAX], BF16)
            kp, b_kp = T("kp", [128, SMAX + 2 * PAD], BF16)
            vp, b_vp = T("vp", [128, SMAX + 2 * PAD], BF16)
            k.op(pool, lambda e: e.memset(kp[:], 0.0), W=[b_kp])
            k.op(pool, lambda e: e.memset(vp[:], 0.0), W=[b_vp])
            NVT = 17
            vts = [T(f"vt{i}", [128, 2, 65], BF16) for i in range(NVT + 2)]
            for i, (vt, bvt) in enumerate(vts):
                k.op(pool, lambda e: e.memset(vt[:], 1.0), W=[bvt])
            k.op(pool, lambda e: e.memset(vts[NVT][0][0:64, :, :], 0.0), W=[vts[NVT][1]])
            k.op(pool, lambda e: e.memset(vts[NVT + 1][0][64:128, :, :], 0.0), W=[vts[NVT + 1][1]])
            ef = [T(f"ef{i}", [128, 512], F32) for i in range(2)]
            ex = [T(f"ex{i}", [128, 512], BF16) for i in range(2)]
            acc, b_acc = T("acc", [65, 2, 2048], F32)
            ao, b_ao = T("ao", [64, 2, 2048], BF16)

            NSB = S // 2048
            for hp in range(4):
                k.dma(sp, qp[:, 0:S], qT_d[hp * 128:(hp + 1) * 128, 0:S], R=[b_qT], W=[b_qp])
                k.op(pool, lambda e: e.memset(kp[:, PAD + S:PAD + S + PAD], 0.0), W=[b_kp])
                k.op(pool, lambda e: e.memset(vp[:, PAD + S:PAD + S + PAD], 0.0), W=[b_vp])
                k.dma(sp, kp[:, PAD:PAD + S], kT_d[hp * 128:(hp + 1) * 128, 0:S], R=[b_kT], W=[b_kp])
                k.dma(sp, vp[:, PAD:PAD + S], vT_d[hp * 128:(hp + 1) * 128, 0:S], R=[b_vT], W=[b_vp])
                for sbk in range(NSB):
                    q0 = sbk * 2048
                    for p, d in enumerate(DILS):
                        nq = 2048 // d // 128
                        for r in range(d):
                            def kslice(j):
                                ks = PAD + q0 + r + d * (128 * j - 64)
                                return slice(ks, ks + 127 * d + 1, d)

                            def vtile(j):
                                if sbk == 0 and j == 0:
                                    return vts[NVT]
                                if sbk == NSB - 1 and j == nq:
                                    return vts[NVT + 1]
                                return vts[j]

                            for j in range(nq + 1):
                                ph, bph = PH[cnt["ph"] % 2]
                                cnt["ph"] += 1
                                k.op(pe, lambda e: e.transpose(out=ph[:, 0:128], in_=vp[:, kslice(j)], identity=ident[:]), R=[b_vp, b_id], W=[bph])
                                vt, bvt = vtile(j)
                                if j % 2 == 0:
                                    k.op(act, lambda e: e.copy(out=vt[:, :, 0:64], in_=ph[:, 0:128].rearrange("p (h e) -> p h e", h=2)), R=[bph], W=[bvt])
                                else:
                                    k.op(dve, lambda e: e.tensor_copy(out=vt[:, :, 0:64], in_=ph[:, 0:128].rearrange("p (h e) -> p h e", h=2)), R=[bph], W=[bvt])
                            for qi in range(nq):
                                qs = q0 + r + d * 128 * qi
                                qsl = slice(qs, qs + 127 * d + 1, d)
                                pS, bpS = PB[cnt["pb"] % 2]
                                cnt["pb"] += 1
                                for h in range(2):
                                    for jj in range(2):
                                        c0 = (h * 2 + jj) * 128
                                        k.op(pe, lambda e: e.matmul(pS[:, c0:c0 + 128], lhsT=kp[h * 64:(h + 1) * 64, kslice(qi + jj)], rhs=qp[h * 64:(h + 1) * 64, qsl], start=True, stop=True), R=[b_kp, b_qp], W=[bpS])
                                i = cnt["ev"] % 2
                                cnt["ev"] += 1
                                eft, beft = ef[i]
                                ext, bext = ex[i]
                                k.op(dve, lambda e: e.scalar_tensor_tensor(out=eft[:], in0=pS[:, :], scalar=0.125, in1=biasT[:, hp * 3 + p, :], op0=ALU.mult, op1=ALU.add), R=[bpS, b_bias], W=[beft])
                                k.op(act, lambda e: e.activation(out=ext[:], in_=eft[:], func=AF.Exp), R=[beft], W=[bext])
                                pO, bpO = PB[2 + i]
                                for h in range(2):
                                    for jj in range(2):
                                        c0 = (h * 2 + jj) * 128
                                        vt, bvt = vtile(qi + jj)
                                        k.op(pe, lambda e: e.matmul(pO[0:65, h * 128:(h + 1) * 128], lhsT=vt[:, h, :], rhs=ext[:, c0:c0 + 128], start=(jj == 0), stop=(jj == 1)), R=[bvt, bext], W=[bpO])
                                asl = slice(qs - q0, qs - q0 + 127 * d + 1, d)
                                pOv = pO[0:65, 0:256].rearrange("p (h q) -> p h q", h=2)
                                if p == 0:
                                    k.op(act, lambda e: e.copy(out=acc[:, :, asl], in_=pOv), R=[bpO], W=[b_acc])
                                else:
                                    k.op(dve, lambda e: e.tensor_tensor(out=acc[:, :, asl], in0=acc[:, :, asl], in1=pOv, op=ALU.add), R=[bpO, b_acc], W=[b_acc])
                    k.op(dve, lambda e: e.reciprocal(out=acc[64:65, :, :], in_=acc[64:65, :, :]), R=[b_acc], W=[b_acc])
                    for h in range(2):
                        for c in range(4):
                            pBt, bpB = PB[4 + c % 2]
                            k.op(pe, lambda e: e.matmul(pBt[0:64, :], lhsT=onesf[64:65, 0:64], rhs=acc[64:65, h, c * 512:(c + 1) * 512], start=True, stop=True), R=[b_ones, b_acc], W=[bpB])
                            k.op(dve, lambda e: e.tensor_tensor(out=ao[:, h, c * 512:(c + 1) * 512], in0=acc[0:64, h, c * 512:(c + 1) * 512], in1=pBt[0:64, :], op=ALU.mult), R=[b_acc, bpB], W=[b_ao])
                    k.dma(sp, at_d[hp * 2:hp * 2 + 2, :, q0:q0 + 2048].rearrange("h e t -> e h t"), ao[:], R=[b_ao], W=[b_at])

        if upto == 2:
            return done()
        with k.scope():
            xin = [T(f"xin{i}", [128, 8, 132], F32) for i in range(2)]
            cv_2 = [T(f"cv{i}", [128, 8, 128], F32) for i in range(2)]
            xbf_2 = [T(f"xbf{i}", [128, 8, 128], BF16) for i in range(2)]
            xs_tok_2 = [T(f"xs_tok{i}", [128, 512], BF16) for i in range(2)]
            b_tok_2 = [T(f"b_tok{i}", [128, 2, 128], BF16) for i in range(2)]
            dtr_2 = [T(f"dtr{i}", [128, 16], F32) for i in range(2)]
            dtv_2 = [T(f"dtv{i}", [128, 16], F32) for i in range(2)]
            av_2 = [T(f"av{i}", [128, 16], F32) for i in range(2)]
            cum_2 = [T(f"cum{i}", [128, 8], F32) for i in range(2)]
            ncum_2 = [T(f"ncum{i}", [128, 8], F32) for i in range(2)]
            dsv_2 = [T(f"dsv{i}", [128, 8], F32) for i in range(2)]
            Ev_2 = [T(f"Ev{i}", [128, 8], F32) for i in range(2)]
            cdv_2 = [T(f"cdv{i}", [128, 8], F32) for i in range(2)]
            xdt_2 = [T(f"xdt{i}", [128, 8, 64], BF16) for i in range(2)]
            xdd_2 = [T(f"xdd{i}", [128, 8, 64], BF16) for i in range(2)]
            cbs_2 = [T(f"cbs{i}", [128, 2, 128], F32) for i in range(2)]
            abA, b_abA = T("abA", [128, 8, 128], F32)
            decA, b_decA = T("decA", [128, 8, 128], F32)
            GA, b_GA = T("GA", [128, 8, 128], BF16)
            Hf, b_Hf = T("Hf", [128, 8, 64], F32)
            Hb, b_Hb = T("Hb", [128, 8, 64], BF16)
            ytmp, b_ytmp = T("ytmp", [128, 512], F32)
            ydir, b_ydir = T("ydir", [128, 512], F32)
            yfw, b_yfw = T("yfw", [128, 512], F32)
            zt, b_zt = T("zt", [128, 512], F32)
            ssd_bf, b_ssdbf = T("ssd_bf", [128, 512], BF16)
            ssdT, b_ssdT = T("ssdT", [128, 4, 128], BF16)
            at_sb, b_atsb = T("at_sb", [64, 8, 128], BF16)
            x1t, b_x1t = T("x1t", [128, D], F32)
            hnT, b_hnT = T("hnT", [128, 8, 128], BF16)
            lg, b_lg = T("lg", [128, 16], F32)
            afft, b_afft = T("afft", [128, 16], F32)
            sm, b_sm = T("sm", [128, 8], F32)

            NCH = S // 128
            for direction in range(2):
                k.op(pool, lambda e: e.memset(Hf[:], 0.0), W=[b_Hf])
                k.op(pool, lambda e: e.memset(Hb[:], 0.0), W=[b_Hb])
                tri, b_tri = (triU, b_triU) if direction == 0 else (triL, b_triL)
                nm, b_nm = (nmU, b_nmU) if direction == 0 else (nmL, b_nmL)
                order = range(NCH) if direction == 0 else range(NCH - 1, -1, -1)
                dc = direction * 8
                for ci, c in enumerate(order):
                    t0 = c * 128
                    cv, b_cv = cv_2[ci % 2]
                    xbf, b_xbf = xbf_2[ci % 2]
                    xs_tok, b_xst = xs_tok_2[ci % 2]
                    b_tok, b_btok = b_tok_2[ci % 2]
                    dtr, b_dtr = dtr_2[ci % 2]
                    dtv, b_dtv = dtv_2[ci % 2]
                    av, b_av = av_2[ci % 2]
                    cum, b_cum = cum_2[ci % 2]
                    ncum, b_ncum = ncum_2[ci % 2]
                    dsv, b_dsv = dsv_2[ci % 2]
                    Ev, b_Ev = Ev_2[ci % 2]
                    cdv, b_cdv = cdv_2[ci % 2]
                    xdt, b_xdt = xdt_2[ci % 2]
                    xdd, b_xdd = xdd_2[ci % 2]
                    cbs, b_cbs = cbs_2[ci % 2]
                    xi, bxi = xin[ci % 2]
                    k.dma(sp, xi[:], xbc_d[:, t0:t0 + 132].rearrange("(c p) t -> p c t", p=128), R=[b_xbc], W=[bxi])
                    k.dma(sp, dtr[:], dt_d[t0:t0 + 128, :], R=[b_dt], W=[b_dtr])
                    if upto == 30:
                        return done()
                    for cc in range(8):
                        k.op(dve, lambda e: e.tensor_scalar(out=cv[:, cc, :], in0=xi[:, cc, 0:128], scalar1=cw[:, 0, cc:cc + 1], scalar2=None, op0=ALU.mult), R=[bxi, b_cw], W=[b_cv])
                        for kk in range(1, 5):
                            k.op(dve, lambda e: e.scalar_tensor_tensor(out=cv[:, cc, :], in0=xi[:, cc, kk:kk + 128], scalar=cw[:, kk, cc:cc + 1], in1=cv[:, cc, :], op0=ALU.mult, op1=ALU.add), R=[bxi, b_cw, b_cv], W=[b_cv])
                        k.op(act, lambda e: e.activation(out=xbf[:, cc, :], in_=cv[:, cc, :], func=AF.Silu, bias=cb[:, cc:cc + 1]), R=[b_cv, b_cb], W=[b_xbf])
                    if upto == 31:
                        return done()
                    ph, bph = PH[cnt["ph"] % 2]
                    cnt["ph"] += 1
                    for cc in range(6):
                        k.op(pe, lambda e: e.transpose(out=ph[:, cc * 128:(cc + 1) * 128], in_=xbf[:, cc, :], identity=ident[:]), R=[b_xbf, b_id], W=[bph])
                    k.op(act, lambda e: e.copy(out=xs_tok[:], in_=ph[:, 0:512]), R=[bph], W=[b_xst])
                    k.op(dve, lambda e: e.tensor_copy(out=b_tok[:], in_=ph[:, 512:768].rearrange("p (g n) -> p g n", g=2)), R=[bph], W=[b_btok])
                    if upto == 32:
                        return done()
                    k.op(dve, lambda e: e.tensor_tensor(out=dtv[:], in0=dtr[:], in1=dtb[:], op=ALU.add), R=[b_dtr, b_dtb], W=[b_dtv])
                    k.op(act, lambda e: e.activation(out=dtv[:], in_=dtv[:], func=AF.Exp), R=[b_dtv], W=[b_dtv])
                    k.op(act, lambda e: e.activation(out=dtv[:], in_=dtv[:], func=AF.Ln, bias=1.0), R=[b_dtv], W=[b_dtv])
                    k.op(dve, lambda e: e.tensor_tensor(out=av[:], in0=dtv[:], in1=Abc[:], op=ALU.mult), R=[b_dtv, b_A], W=[b_av])
                    if upto == 3:
                        return done()
                    pC, bpC = PB[0]
                    k.op(pe, lambda e: e.matmul(pC[:, 0:8], lhsT=tri[:, :], rhs=av[:, dc:dc + 8], start=True, stop=True), R=[b_tri, b_av], W=[bpC])
                    k.op(pe, lambda e: e.matmul(pC[:, 8:16], lhsT=onesf[:, :], rhs=av[:, dc:dc + 8], start=True, stop=True), R=[b_ones, b_av], W=[bpC])
                    k.op(dve, lambda e: e.tensor_copy(out=cum[:], in_=pC[:, 0:8]), R=[bpC], W=[b_cum])
                    k.op(dve, lambda e: e.tensor_scalar(out=ncum[:], in0=pC[:, 0:8], scalar1=-1.0, scalar2=None, op0=ALU.mult), R=[bpC], W=[b_ncum])
                    k.op(dve, lambda e: e.tensor_tensor(out=dsv[:], in0=pC[:, 8:16], in1=cum[:], op=ALU.subtract), R=[bpC, b_cum], W=[b_dsv])
                    k.op(act, lambda e: e.activation(out=dsv[:], in_=dsv[:], func=AF.Exp), R=[b_dsv], W=[b_dsv])
                    k.op(act, lambda e: e.activation(out=Ev[:], in_=cum[:], func=AF.Exp), R=[b_cum], W=[b_Ev])
                    k.op(act, lambda e: e.activation(out=cdv[:], in_=pC[:, 8:16], func=AF.Exp), R=[bpC], W=[b_cdv])
                    xs3 = xs_tok[:, :].rearrange("p (h e) -> p h e", h=8)
                    k.op(dve, lambda e: e.tensor_tensor(out=xdt[:], in0=xs3, in1=dtv[:, dc:dc + 8].unsqueeze(2).to_broadcast([128, 8, 64]), op=ALU.mult), R=[b_xst, b_dtv], W=[b_xdt])
                    k.op(dve, lambda e: e.tensor_tensor(out=xdd[:], in0=xdt[:], in1=dsv[:, :].unsqueeze(2).to_broadcast([128, 8, 64]), op=ALU.mult), R=[b_xdt, b_dsv], W=[b_xdd])
                    if upto == 4:
                        return done()
                    pCB, bpCB = PB[1]
                    for g in range(2):
                        k.op(pe, lambda e: e.matmul(pCB[:, g * 128:(g + 1) * 128], lhsT=xbf[:, 4 + g, :], rhs=xbf[:, 6 + g, :], start=True, stop=True), R=[b_xbf], W=[bpCB])
                    k.op(act, lambda e: e.copy(out=cbs[:], in_=pCB[:, 0:256].rearrange("p (g l) -> p g l", g=2)), R=[bpCB], W=[b_cbs])
                    pY, bpY = PB[2]
                    pYo, bpYo = PB[3]
                    k.op(pool, lambda e: e.tensor_copy(out=abA[:], in_=av[:, dc:dc + 8].unsqueeze(2).to_broadcast([128, 8, 128])), R=[b_av], W=[b_abA])
                    for h in range(8):
                        pD, bpD = PB[4 + h // 4]
                        c0 = (h % 4) * 128
                        k.op(pe, lambda e: e.matmul(pD[:, c0:c0 + 128], lhsT=abA[:, h, :], rhs=tri[:, :], start=True, stop=False), R=[b_abA, b_tri], W=[bpD])
                        k.op(pe, lambda e: e.matmul(pD[:, c0:c0 + 128], lhsT=identf[:, :], rhs=nm[:, :], start=False, stop=True), R=[b_idf, b_nm], W=[bpD])
                    for h in range(8):
                        pD, bpD = PB[4 + h // 4]
                        c0 = (h % 4) * 128
                        k.op(act, lambda e: e.activation(out=decA[:, h, :], in_=pD[:, c0:c0 + 128], func=AF.Exp, bias=ncum[:, h:h + 1]), R=[bpD, b_ncum], W=[b_decA])
                    for g in range(2):
                        k.op(dve, lambda e: e.tensor_tensor(out=GA[:, g * 4:(g + 1) * 4, :], in0=decA[:, g * 4:(g + 1) * 4, :], in1=cbs[:, g:g + 1, :].to_broadcast([128, 4, 128]), op=ALU.mult), R=[b_decA, b_cbs], W=[b_GA])
                    for h in range(8):
                        k.op(pe, lambda e: e.matmul(pY[:, h * 64:(h + 1) * 64], lhsT=GA[:, h, :], rhs=xdt[:, h, :], start=True, stop=True), R=[b_GA, b_xdt], W=[bpY])
                    for g in range(2):
                        k.op(pe, lambda e: e.matmul(pYo[:, g * 256:(g + 1) * 256], lhsT=xbf[:, 6 + g, :], rhs=Hb[:, g * 4:(g + 1) * 4, :].rearrange("p h e -> p (h e)"), start=True, stop=True), R=[b_xbf, b_Hb], W=[bpYo])
                    k.op(dve, lambda e: e.tensor_tensor(out=ytmp[:, :].rearrange("p (h e) -> p h e", h=8), in0=pYo[:, :].rearrange("p (h e) -> p h e", h=8), in1=Ev[:, :].unsqueeze(2).to_broadcast([128, 8, 64]), op=ALU.mult), R=[bpYo, b_Ev], W=[b_ytmp])
                    k.op(dve, lambda e: e.tensor_tensor(out=ydir[:], in0=ytmp[:], in1=pY[:, :], op=ALU.add), R=[b_ytmp, bpY], W=[b_ydir])
                    if upto == 5:
                        return done()
                    pSt, bpSt = PB[0]
                    for g in range(2):
                        k.op(pe, lambda e: e.matmul(pSt[:, g * 256:(g + 1) * 256], lhsT=b_tok[:, g, :], rhs=xdd[:, g * 4:(g + 1) * 4, :].rearrange("p h e -> p (h e)"), start=True, stop=True), R=[b_btok, b_xdd], W=[bpSt])
                    k.op(dve, lambda e: e.tensor_tensor(out=Hf[:], in0=Hf[:], in1=cdv[:, :].unsqueeze(2).to_broadcast([128, 8, 64]), op=ALU.mult), R=[b_Hf, b_cdv], W=[b_Hf])
                    k.op(dve, lambda e: e.tensor_tensor(out=Hf[:], in0=Hf[:], in1=pSt[:, :].rearrange("p (h e) -> p h e", h=8), op=ALU.add), R=[b_Hf, bpSt], W=[b_Hf])
                    k.op(pool, lambda e: e.tensor_copy(out=Hb[:], in_=Hf[:]), R=[b_Hf], W=[b_Hb])
                    if direction == 0:
                        k.dma(sp, yf_d[t0:t0 + 128, :], ydir[:], R=[b_ydir], W=[b_yf])
                        continue
                    if upto == 6:
                        return done()
                    k.dma(sp, yfw[:], yf_d[t0:t0 + 128, :], R=[b_yf], W=[b_yfw])
                    k.dma(sp, zt[:], z_d[t0:t0 + 128, :], R=[b_z], W=[b_zt])
                    k.dma(sp, at_sb[:], at_d[:, :, t0:t0 + 128].rearrange("h e t -> e h t"), R=[b_at], W=[b_atsb])
                    xtile, bx = xt[ci % 2]
                    k.dma(sp, xtile[:], x_in[row0 + t0:row0 + t0 + 128, :], W=[bx])
                    k.op(dve, lambda e: e.tensor_tensor(out=ydir[:], in0=ydir[:], in1=yfw[:], op=ALU.add), R=[b_ydir, b_yfw], W=[b_ydir])
                    k.op(pool, lambda e: e.tensor_tensor(out=ytmp[:, :].rearrange("p (h e) -> p h e", h=8), in0=xs3, in1=dsk[:, :].unsqueeze(2).to_broadcast([128, 8, 64]), op=ALU.mult), R=[b_xst, b_dsk], W=[b_ytmp])
                    k.op(dve, lambda e: e.tensor_tensor(out=ydir[:], in0=ydir[:], in1=ytmp[:], op=ALU.add), R=[b_ydir, b_ytmp], W=[b_ydir])
                    k.op(act, lambda e: e.activation(out=zt[:], in_=zt[:], func=AF.Silu), R=[b_zt], W=[b_zt])
                    k.op(dve, lambda e: e.tensor_tensor(out=ydir[:], in0=ydir[:], in1=zt[:], op=ALU.mult), R=[b_ydir, b_zt], W=[b_ydir])
                    for g in range(2):
                        k.op(act, lambda e: e.activation(out=junk[:, g * 256:(g + 1) * 256], in_=ydir[:, g * 256:(g + 1) * 256], func=AF.Square, accum_out=sm[:, g:g + 1]), R=[b_ydir], W=[b_junk, b_sm])
                    k.op(dve, lambda e: e.tensor_scalar(out=sm[:, 2:4], in0=sm[:, 0:2], scalar1=1.0 / 256, scalar2=EPS, op0=ALU.mult, op1=ALU.add), R=[b_sm], W=[b_sm])
                    k.op(act, lambda e: e.activation(out=sm[:, 4:6], in_=sm[:, 2:4], func=AF.Sqrt), R=[b_sm], W=[b_sm])
                    k.op(dve, lambda e: e.reciprocal(out=sm[:, 6:8], in_=sm[:, 4:6]), R=[b_sm], W=[b_sm])
                    for g in range(2):
                        k.op(dve, lambda e: e.scalar_tensor_tensor(out=ssd_bf[:, g * 256:(g + 1) * 256], in0=ydir[:, g * 256:(g + 1) * 256], scalar=sm[:, 6 + g:7 + g], in1=gssd[:, g * 256:(g + 1) * 256], op0=ALU.mult, op1=ALU.mult), R=[b_ydir, b_sm, b_gssd], W=[b_ssdbf])
                    ph, bph = PH[cnt["ph"] % 2]
                    cnt["ph"] += 1
                    for cc in range(4):
                        k.op(pe, lambda e: e.transpose(out=ph[:, cc * 128:(cc + 1) * 128], in_=ssd_bf[:, cc * 128:(cc + 1) * 128], identity=ident[:]), R=[b_ssdbf, b_id], W=[bph])
                    k.op(act, lambda e: e.copy(out=ssdT[:], in_=ph[:, 0:512].rearrange("p (c t) -> p c t", c=4)), R=[bph], W=[b_ssdT])
                    for half in range(2):
                        pX, bpX = PB[4 + half]
                        hs = slice(half * 512, (half + 1) * 512)
                        for h in range(8):
                            k.op(pe, lambda e: e.matmul(pX[:, :], lhsT=at_sb[:, h, :], rhs=wo_att[:, h, hs], start=(h == 0), stop=False), R=[b_atsb, b_woa], W=[bpX])
                        for cc in range(4):
                            k.op(pe, lambda e: e.matmul(pX[:, :], lhsT=ssdT[:, cc, :], rhs=wo_ssd[:, cc, hs], start=False, stop=(cc == 3)), R=[b_ssdT, b_wos], W=[bpX])
                        k.op(dve, lambda e: e.tensor_tensor(out=x1t[:, hs], in0=xtile[:, hs], in1=pX[:, :], op=ALU.add), R=[bx, bpX], W=[b_x1t])
                    k.dma(sp, x1_out[row0 + t0:row0 + t0 + 128, :], x1t[:], R=[b_x1t], W=[b_x1])
                    rmsnorm_to_bf(x1t[:], b_x1t, gffn[:], b_gffn, hb[:], b_hb)
                    ph, bph = PH[cnt["ph"] % 2]
                    cnt["ph"] += 1
                    for cc in range(8):
                        k.op(pe, lambda e: e.transpose(out=ph[:, cc * 128:(cc + 1) * 128], in_=hb[:, cc * 128:(cc + 1) * 128], identity=ident[:]), R=[b_hb, b_id], W=[bph])
                    k.op(act, lambda e: e.copy(out=hnT[:], in_=ph[:, :].rearrange("p (c t) -> p c t", c=8)), R=[bph], W=[b_hnT])
                    pR, bpR = PB[1]
                    for kc in range(8):
                        k.op(pe, lambda e: e.matmul(pR[:, 0:16], lhsT=hnT[:, kc, :], rhs=wr_bf[:, kc, :], start=(kc == 0), stop=(kc == 7)), R=[b_hnT, b_wr], W=[bpR])
                    k.op(dve, lambda e: e.tensor_reduce(out=sm[:, 0:1], in_=pR[:, 0:16], axis=AX.X, op=ALU.max, negate=True), R=[bpR], W=[b_sm])
                    k.op(act, lambda e: e.activation(out=lg[:], in_=pR[:, 0:16], func=AF.Exp, bias=sm[:, 0:1], accum_out=sm[:, 1:2]), R=[bpR, b_sm], W=[b_lg, b_sm])
                    k.op(dve, lambda e: e.reciprocal(out=sm[:, 2:3], in_=sm[:, 1:2]), R=[b_sm], W=[b_sm])
                    k.op(dve, lambda e: e.tensor_scalar(out=afft[:], in0=lg[:], scalar1=sm[:, 2:3], scalar2=None, op0=ALU.mult), R=[b_lg, b_sm], W=[b_afft])
                    k.dma(sp, aff_out[row0 + t0:row0 + t0 + 128, :], afft[:], R=[b_afft], W=[b_aff])

    k.finish([b_x1, b_aff])
    return nc, k


def build_b(NT, TG, n_exp=16, FF=2816):
    nc, k = _build_b(NT, TG, n_exp, FF)
    k.close()
    return nc, k.n_ins


def _build_b(NT, TG, n_exp, FF):
    nc = bass.Bass("TRN2", target_bir_lowering=False)
    k = K(nc)
    sp, act, dve, pool, pe = k.sp, k.act, k.dve, k.pool, k.pe
    NTL = NT // 128
    NFC = FF // 128
    CAP = TG // 8
    TPP = TG // 128

    def din(name, shape):
        return nc.dram_tensor(name, list(shape), F32, kind="ExternalInput").ap()

    x1_in = din("x1", [NT, D])
    aff_all = din("aff_all", [2, TG, 16])
    aff_loc = din("aff_loc", [NT, 16])
    p_in = din("p", [NT, 256])
    w_gate = din("w_gate", [n_exp, D, FF])
    w_up = din("w_up", [n_exp, D, FF])
    w_down = din("w_down", [n_exp, FF, D])
    g_ffn = din("g_ffn", [1, D])
    g_pg = din("g_pg", [1, D])
    g_ple = din("g_ple", [1, D])
    g_fin = din("g_final", [1, D])
    w_pg = din("w_pg", [D, D])
    w_ple = din("w_ple", [256, D])
    y_out = nc.dram_tensor("y", [NT, D], F32, kind="ExternalOutput").ap()
    b_y = Buf("y")
    yacc_d = nc.dram_tensor("yacc_d", [NT, D], F32).ap()

    def T(name, shape, dt):
        return k.sb(name, shape, dt), Buf(name)

    PB = [(k.ps(f"pb{i}", [128, 512], F32), Buf(f"pb{i}", excl=True)) for i in range(6)]
    PH = [(k.ps(f"ph{i}", [128, 1024], BF16), Buf(f"ph{i}", excl=True)) for i in range(2)]

    onesf, b_ones = T("onesf", [128, 128], F32)
    identf, b_idf = T("identf", [128, 128], F32)
    ident, b_id = T("ident", [128, 128], BF16)
    k.op(pool, lambda e: e.memset(onesf[:], 1.0), W=[b_ones])
    k.op(pool, lambda e: e.memset(identf[:], 0.0), W=[b_idf])
    k.op(pool, lambda e: e.affine_select(out=identf[:], in_=identf[:], pattern=[[-1, 128]], compare_op=ALU.not_equal, fill=1.0, base=0, channel_multiplier=1), R=[b_idf], W=[b_idf])
    k.op(dve, lambda e: e.tensor_copy(out=ident[:], in_=identf[:]), R=[b_idf], W=[b_id])
    thr, b_thr = T("thr", [128, 32], F32)

    def rmsnorm(xtile, bx, gtile, bg, out, bout, n=D):
        k.op(act, lambda e: e.activation(out=junk[:, 0:n], in_=xtile, func=AF.Square, accum_out=st_s[:, 0:1]), R=[bx], W=[b_junk, b_sts])
        k.op(dve, lambda e: e.tensor_scalar(out=st_s[:, 1:2], in0=st_s[:, 0:1], scalar1=1.0 / n, scalar2=EPS, op0=ALU.mult, op1=ALU.add), R=[b_sts], W=[b_sts])
        k.op(act, lambda e: e.activation(out=st_s[:, 2:3], in_=st_s[:, 1:2], func=AF.Sqrt), R=[b_sts], W=[b_sts])
        k.op(dve, lambda e: e.reciprocal(out=st_s[:, 3:4], in_=st_s[:, 2:3]), R=[b_sts], W=[b_sts])
        k.op(dve, lambda e: e.scalar_tensor_tensor(out=out, in0=xtile, scalar=st_s[:, 3:4], in1=gtile, op0=ALU.mult, op1=ALU.mult), R=[bx, b_sts, bg], W=[bout])

    with k.scope():
        aff, b_affs = T("aff", [128, 2, TPP, 16], F32)
        cmp_, b_cmp = T("cmp", [128, 2, TPP, 16], BF16)
        cntt, b_cnt = T("cntt", [128, 32], F32)
        lo, b_lo = T("lo", [128, 32], F32)
        hi, b_hi = T("hi", [128, 32], F32)
        mid, b_mid = T("mid", [128, 32], F32)
        ge, b_ge = T("ge", [128, 32], F32)
        d1, b_d1 = T("d1", [128, 32], F32)
        for g in range(2):
            k.dma(sp, aff[:, g, :, :], aff_all[g].rearrange("(p t) e -> p t e", p=128), W=[b_affs])
        k.op(pool, lambda e: e.memset(lo[:], 0.0), W=[b_lo])
        k.op(pool, lambda e: e.memset(hi[:], 1.0), W=[b_hi])
        k.op(pool, lambda e: e.memset(mid[:], 0.5), W=[b_mid])
        for it in range(32):
            mb = mid[:, :].rearrange("p (g e) -> p g e", g=2).unsqueeze(2).to_broadcast([128, 2, TPP, 16])
            k.op(dve, lambda e: e.tensor_tensor(out=cmp_[:], in0=aff[:], in1=mb, op=ALU.is_ge), R=[b_affs, b_mid], W=[b_cmp])
            k.op(dve, lambda e: e.tensor_reduce(out=cntt[:, :].rearrange("p (g e) -> p g e", g=2), in_=cmp_[:].rearrange("p g t e -> p g e t"), axis=AX.X, op=ALU.add), R=[b_cmp], W=[b_cnt])
            pC, bpC = PB[it % 2]
            k.op(pe, lambda e: e.matmul(pC[:, 0:32], lhsT=onesf[:, :], rhs=cntt[:, :], start=True, stop=True), R=[b_ones, b_cnt], W=[bpC])
            k.op(dve, lambda e: e.tensor_scalar(out=ge[:], in0=pC[:, 0:32], scalar1=CAP - 0.5, scalar2=None, op0=ALU.is_ge), R=[bpC], W=[b_ge])
            k.op(dve, lambda e: e.tensor_tensor(out=d1[:], in0=mid[:], in1=lo[:], op=ALU.subtract), R=[b_mid, b_lo], W=[b_d1])
            k.op(dve, lambda e: e.tensor_tensor(out=d1[:], in0=d1[:], in1=ge[:], op=ALU.mult), R=[b_d1, b_ge], W=[b_d1])
            k.op(dve, lambda e: e.tensor_tensor(out=lo[:], in0=lo[:], in1=d1[:], op=ALU.add), R=[b_lo, b_d1], W=[b_lo])
            k.op(dve, lambda e: e.tensor_tensor(out=d1[:], in0=hi[:], in1=mid[:], op=ALU.subtract), R=[b_hi, b_mid], W=[b_d1])
            k.op(dve, lambda e: e.tensor_tensor(out=d1[:], in0=d1[:], in1=ge[:], op=ALU.mult), R=[b_d1, b_ge], W=[b_d1])
            k.op(dve, lambda e: e.tensor_tensor(out=hi[:], in0=mid[:], in1=d1[:], op=ALU.add), R=[b_mid, b_d1], W=[b_hi])
            k.op(dve, lambda e: e.tensor_tensor(out=mid[:], in0=lo[:], in1=hi[:], op=ALU.add), R=[b_lo, b_hi], W=[b_mid])
            k.op(dve, lambda e: e.tensor_scalar(out=mid[:], in0=mid[:], scalar1=0.5, scalar2=None, op0=ALU.mult), R=[b_mid], W=[b_mid])
        k.op(dve, lambda e: e.tensor_copy(out=thr[:], in_=lo[:]), R=[b_lo], W=[b_thr])

    TBT = 8
    RS = 256
    NBLK = NTL // TBT
    b_ytl = [Buf(f"yacct{i}") for i in range(NTL)]
    hn_d = nc.dram_tensor("hn_d", [NT, D], BF16).ap()
    b_hn_d = Buf("hn_d")
    gm, b_gm = T("gm2", [128, NTL, 16], F32)
    slotidx, b_slot = T("slotidx", [128, NTL, 16], F32)
    triU, b_triU = T("triU", [128, 128], F32)
    k.op(pool, lambda e: e.affine_select(out=triU[:], in_=onesf[:], pattern=[[1, 128]], compare_op=ALU.is_ge, fill=0.0, base=0, channel_multiplier=-1), R=[b_ones], W=[b_triU])
    iota_f, b_iota = T("iota_f", [128, RS], F32)
    k.op(pool, lambda e: e.iota(iota_f[:], pattern=[[1, RS]], base=0, channel_multiplier=0, allow_small_or_imprecise_dtypes=True), W=[b_iota])
    cnt = {"ph": 0}
    with k.scope():
        xt = [T(f"xts{i}", [128, D], F32) for i in range(2)]
        junk, b_junk = T("junk_s", [128, D], F32)
        st_s, b_sts = T("st_ss", [128, 8], F32)
        hbs = [T(f"hbs{i}", [128, D], BF16) for i in range(2)]
        gffn, b_gffn = T("gffn_s", [128, D], F32)
        k.dma(sp, gffn[:], g_ffn[0:1, :].to_broadcast([128, D]), W=[b_gffn])
        afl, b_afl = T("afl", [128, 16], F32)
        msk, b_msk = T("msk", [128, 16], F32)
        basev, b_base = T("basev", [128, 16], F32)
        slv, b_slv = T("slv", [128, 16], F32)
        for t in range(NTL):
            g = 0 if t < NTL // 2 else 1
            xtile, bx = xt[t % 2]
            k.dma(sp, xtile[:], x1_in[t * 128:(t + 1) * 128, :], W=[bx])
            k.dma(sp, yacc_d[t * 128:(t + 1) * 128, :], xtile[:], R=[bx], W=[b_ytl[t]])
            k.dma(sp, afl[:], aff_loc[t * 128:(t + 1) * 128, :], W=[b_afl])
            k.op(dve, lambda e: e.tensor_tensor(out=msk[:], in0=afl[:], in1=thr[:, g * 16:(g + 1) * 16], op=ALU.is_ge), R=[b_afl, b_thr], W=[b_msk])
            k.op(dve, lambda e: e.tensor_tensor(out=gm[:, t, :], in0=afl[:], in1=msk[:], op=ALU.mult), R=[b_afl, b_msk], W=[b_gm])
            if t % TBT == 0:
                k.op(pool, lambda e: e.memset(basev[:], 0.0), W=[b_base])
            pC, bpC = PB[t % 2]
            k.op(pe, lambda e: e.matmul(pC[:, 0:16], lhsT=triU[:, :], rhs=msk[:, :], start=True, stop=True), R=[b_triU, b_msk], W=[bpC])
            k.op(pe, lambda e: e.matmul(pC[:, 16:32], lhsT=onesf[:, :], rhs=msk[:, :], start=True, stop=True), R=[b_ones, b_msk], W=[bpC])
            k.op(dve, lambda e: e.tensor_tensor(out=slv[:], in0=pC[:, 0:16], in1=basev[:], op=ALU.add), R=[bpC, b_base], W=[b_slv])
            k.op(dve, lambda e: e.tensor_tensor(out=slv[:], in0=slv[:], in1=msk[:], op=ALU.mult), R=[b_slv, b_msk], W=[b_slv])
            k.op(dve, lambda e: e.tensor_scalar(out=slotidx[:, t, :], in0=slv[:], scalar1=-1.0, scalar2=None, op0=ALU.add), R=[b_slv], W=[b_slot])
            k.op(dve, lambda e: e.tensor_tensor(out=basev[:], in0=basev[:], in1=pC[:, 16:32], op=ALU.add), R=[b_base, bpC], W=[b_base])
            hb, b_hb = hbs[t % 2]
            rmsnorm(xtile[:], bx, gffn[:], b_gffn, hb[:], b_hb)
            k.dma(sp, hn_d[t * 128:(t + 1) * 128, :], hb[:], R=[b_hb], W=[b_hn_d])

    with k.scope():
        stg = [T(f"stgm{i}", [128, 1024], F32) for i in range(2)]
        wg, b_wg = T("wg", [128, 8, FF], BF16)
        wu, b_wu = T("wu", [128, 8, FF], BF16)
        wd, b_wd = T("wd", [128, NFC, D], BF16)
        hnt = [T(f"hnt{i}", [128, D], BF16) for i in range(2)]
        Sm, b_S0 = T("Sm", [128, TBT, RS], BF16)
        b_Sj = [Buf(f"S{j}") for j in range(TBT)]
        STm, b_ST = T("STm", [128, TBT, 2, 128], BF16)
        xsT, b_xsT = T("xsT", [128, 8, RS], BF16)
        actb, b_actb = T("actb", [128, NFC, RS], BF16)
        sg, b_sg = T("sg", [128, RS], F32)
        yeb, b_yeb = T("yeb", [128, 2, D], BF16)
        yos = [T(f"yo{i}", [128, D], F32) for i in range(2)]
        sc = {"stg": 0}

        def load_cast(dst_ap, src_ap, bdst, c):
            (st, bst) = stg[sc["stg"] % 2]
            sc["stg"] += 1
            sv = st[:, 0:src_ap.shape[1] * src_ap.shape[2]].rearrange("p (c n) -> p c n", c=src_ap.shape[1])
            k.dma(sp, sv, src_ap, W=[bst])
            eng = [dve, pool, act][c % 3]
            if eng is act:
                k.op(act, lambda e: e.copy(out=dst_ap, in_=sv), R=[bst], W=[bdst])
            else:
                k.op(eng, lambda e: e.tensor_copy(out=dst_ap, in_=sv), R=[bst], W=[bdst])

        for ex in range(n_exp):
            c = 0
            for (wsrc, wdst, bw) in ((w_gate, wg, b_wg), (w_up, wu, b_wu)):
                for n0 in range(0, FF, 128):
                    load_cast(wdst[:, :, n0:n0 + 128], wsrc[ex, :, n0:n0 + 128].rearrange("(c p) n -> p c n", p=128), bw, c)
                    c += 1
            for f0 in range(0, NFC):
                load_cast(wd[:, f0:f0 + 1, :], w_down[ex, f0 * 128:(f0 + 1) * 128, :].rearrange("(c p) n -> p c n", p=128), b_wd, c)
                c += 1
            for blk in range(NBLK):
                for j in range(TBT):
                    tl = blk * TBT + j
                    k.op(dve, lambda e: e.tensor_scalar(out=Sm[:, j, :], in0=iota_f[:], scalar1=slotidx[:, tl, ex:ex + 1], scalar2=None, op0=ALU.is_equal), R=[b_iota, b_slot], W=[b_Sj[j]])
                    ht, bht = hnt[j % 2]
                    k.dma(sp, ht[:], hn_d[tl * 128:(tl + 1) * 128, :], R=[b_hn_d], W=[bht])
                    for kc in range(8):
                        pg_, bpg_ = PB[kc // 2]
                        k.op(pe, lambda e: e.matmul(pg_[:, (kc % 2) * RS:(kc % 2 + 1) * RS], lhsT=ht[:, kc * 128:(kc + 1) * 128], rhs=Sm[:, j, :], start=(j == 0 and kc % 2 == 0), stop=(j == TBT - 1), skip_group_check=True), R=[bht, b_Sj[j]], W=[bpg_])
                for i in range(4):
                    pg_, bpg_ = PB[i]
                    if i % 2 == 0:
                        k.op(act, lambda e: e.copy(out=xsT[:, 2 * i:2 * i + 2, :], in_=pg_[:, :].rearrange("p (c s) -> p c s", c=2)), R=[bpg_], W=[b_xsT])
                    else:
                        k.op(dve, lambda e: e.tensor_copy(out=xsT[:, 2 * i:2 * i + 2, :], in_=pg_[:, :].rearrange("p (c s) -> p c s", c=2)), R=[bpg_], W=[b_xsT])
                for hh in range(2):
                    ph, bph = PH[hh]
                    for jj in range(4):
                        j = hh * 4 + jj
                        for st_ in range(2):
                            k.op(pe, lambda e: e.transpose(out=ph[:, (jj * 2 + st_) * 128:(jj * 2 + st_ + 1) * 128], in_=Sm[:, j, st_ * 128:(st_ + 1) * 128], identity=ident[:]), R=[b_Sj[j], b_id], W=[bph])
                    if hh == 0:
                        k.op(act, lambda e: e.copy(out=STm[:, 0:4, :, :], in_=ph[:, :].rearrange("p (j s t) -> p j s t", j=4, s=2)), R=[bph], W=[b_ST])
                    else:
                        k.op(dve, lambda e: e.tensor_copy(out=STm[:, 4:8, :, :], in_=ph[:, :].rearrange("p (j s t) -> p j s t", j=4, s=2)), R=[bph], W=[b_ST])
                for fc in range(NFC):
                    pGU, bpGU = PB[4 + fc % 2]
                    for kc in range(8):
                        k.op(pe, lambda e: e.matmul(pGU[:, 0:RS], lhsT=wg[:, kc, fc * 128:(fc + 1) * 128], rhs=xsT[:, kc, :], start=(kc == 0), stop=(kc == 7)), R=[b_wg, b_xsT], W=[bpGU])
                    for kc in range(8):
                        k.op(pe, lambda e: e.matmul(pGU[:, RS:2 * RS], lhsT=wu[:, kc, fc * 128:(fc + 1) * 128], rhs=xsT[:, kc, :], start=(kc == 0), stop=(kc == 7)), R=[b_wu, b_xsT], W=[bpGU])
                    k.op(act, lambda e: e.activation(out=sg[:], in_=pGU[:, 0:RS], func=AF.Silu), R=[bpGU], W=[b_sg])
                    k.op(dve, lambda e: e.tensor_tensor(out=actb[:, fc, :], in0=sg[:], in1=pGU[:, RS:2 * RS], op=ALU.mult), R=[b_sg, bpGU], W=[b_actb])
                for st_ in range(2):
                    for half in range(2):
                        pY, bpY = PB[st_ * 2 + half]
                        for fc in range(NFC):
                            k.op(pe, lambda e: e.matmul(pY[:, :], lhsT=actb[:, fc, st_ * 128:(st_ + 1) * 128], rhs=wd[:, fc, half * 512:(half + 1) * 512], start=(fc == 0), stop=(fc == NFC - 1)), R=[b_actb, b_wd], W=[bpY])
                        if half == 0:
                            k.op(act, lambda e: e.copy(out=yeb[:, st_, 0:512], in_=pY[:, :]), R=[bpY], W=[b_yeb])
                        else:
                            k.op(dve, lambda e: e.tensor_copy(out=yeb[:, st_, 512:1024], in_=pY[:, :]), R=[bpY], W=[b_yeb])
                for j in range(TBT):
                    tl = blk * TBT + j
                    trow = yacc_d[tl * 128:(tl + 1) * 128, :]
                    yo, b_yo = yos[j % 2]
                    for half in range(2):
                        pZ, bpZ = PB[(j % 2) * 2 + half]
                        for st_ in range(2):
                            k.op(pe, lambda e: e.matmul(pZ[:, :], lhsT=STm[:, j, st_, :], rhs=yeb[:, st_, half * 512:(half + 1) * 512], start=(st_ == 0), stop=(st_ == 1)), R=[b_ST, b_yeb], W=[bpZ])
                        if half == 0:
                            k.op(dve, lambda e: e.tensor_scalar(out=yo[:, 0:512], in0=pZ[:, :], scalar1=gm[:, tl, ex:ex + 1], scalar2=None, op0=ALU.mult), R=[bpZ, b_gm], W=[b_yo])
                        else:
                            k.op(act, lambda e: e.activation(out=yo[:, 512:1024], in_=pZ[:, :], func=AF.Copy, scale=gm[:, tl, ex:ex + 1]), R=[bpZ, b_gm], W=[b_yo])
                    k.dma(pool, trow, yo[:], R=[b_yo], W=[b_ytl[tl]], accum_op=ALU.add)

    with k.scope():
        stg = [T(f"stgp{i}", [128, 1024], F32) for i in range(2)]
        xt = [T(f"xtp{i}", [128, D], F32) for i in range(2)]
        junk, b_junk = T("junk_p", [128, D], F32)
        st_s, b_sts = T("st_sp", [128, 8], F32)
        hb, b_hb = T("hb_p", [128, D], BF16)
        hnT, b_hnT = T("hnT_p", [128, 8, 128], BF16)
        wpg, b_wpg = T("wpg", [128, 8, D], BF16)
        wple, b_wple = T("wple", [128, 2, D], BF16)
        gpg, b_gpg = T("gpg", [128, D], F32)
        gple, b_gple = T("gple", [128, D], F32)
        gfin, b_gfin = T("gfin", [128, D], F32)
        for (gt, bg, src) in ((gpg, b_gpg, g_pg), (gple, b_gple, g_ple), (gfin, b_gfin, g_fin)):
            k.dma(sp, gt[:], src[0:1, :].to_broadcast([128, D]), W=[bg])
        for c in range(8):
            st, bst = stg[c % 2]
            k.dma(sp, st[:, 0:D], w_pg[c * 128:(c + 1) * 128, :], W=[bst])
            k.op(dve, lambda e: e.tensor_copy(out=wpg[:, c, :], in_=st[:, 0:D]), R=[bst], W=[b_wpg])
        for c in range(2):
            st, bst = stg[c % 2]
            k.dma(sp, st[:, 0:D], w_ple[c * 128:(c + 1) * 128, :], W=[bst])
            k.op(dve, lambda e: e.tensor_copy(out=wple[:, c, :], in_=st[:, 0:D]), R=[bst], W=[b_wple])
        pt_, b_pt = T("pt", [128, 256], F32)
        pb_, b_pb = T("pbf", [128, 256], BF16)
        pT_, b_pT = T("pT", [128, 2, 128], BF16)
        er, b_er = T("er", [128, D], F32)
        ev, b_ev = T("ev", [128, D], F32)
        gs, b_gs = T("gs", [128, D], F32)
        yt, b_yt = T("yt", [128, D], F32)
        for t in range(NTL):
            xtile, bx = xt[t % 2]
            k.dma(sp, xtile[:], yacc_d[t * 128:(t + 1) * 128, :], R=[b_ytl[t]], W=[bx])
            k.dma(sp, pt_[:], p_in[t * 128:(t + 1) * 128, :], W=[b_pt])
            k.op(pool, lambda e: e.tensor_copy(out=pb_[:], in_=pt_[:]), R=[b_pt], W=[b_pb])
            ph, bph = PH[cnt["ph"] % 2]
            cnt["ph"] += 1
            for c in range(2):
                k.op(pe, lambda e: e.transpose(out=ph[:, c * 128:(c + 1) * 128], in_=pb_[:, c * 128:(c + 1) * 128], identity=ident[:]), R=[b_pb, b_id], W=[bph])
            k.op(act, lambda e: e.copy(out=pT_[:], in_=ph[:, 0:256].rearrange("p (c t) -> p c t", c=2)), R=[bph], W=[b_pT])
            for half in range(2):
                pE, bpE = PB[half]
                for c in range(2):
                    k.op(pe, lambda e: e.matmul(pE[:, :], lhsT=pT_[:, c, :], rhs=wple[:, c, half * 512:(half + 1) * 512], start=(c == 0), stop=(c == 1)), R=[b_pT, b_wple], W=[bpE])
                k.op(act, lambda e: e.copy(out=er[:, half * 512:(half + 1) * 512], in_=pE[:, :]), R=[bpE], W=[b_er])
            rmsnorm(er[:], b_er, gple[:], b_gple, ev[:], b_ev)
            rmsnorm(xtile[:], bx, gpg[:], b_gpg, hb[:], b_hb)
            ph, bph = PH[cnt["ph"] % 2]
            cnt["ph"] += 1
            for c in range(8):
                k.op(pe, lambda e: e.transpose(out=ph[:, c * 128:(c + 1) * 128], in_=hb[:, c * 128:(c + 1) * 128], identity=ident[:]), R=[b_hb, b_id], W=[bph])
            k.op(act, lambda e: e.copy(out=hnT[:], in_=ph[:, :].rearrange("p (c t) -> p c t", c=8)), R=[bph], W=[b_hnT])
            for half in range(2):
                pE, bpE = PB[2 + half]
                for c in range(8):
                    k.op(pe, lambda e: e.matmul(pE[:, :], lhsT=hnT[:, c, :], rhs=wpg[:, c, half * 512:(half + 1) * 512], start=(c == 0), stop=(c == 7)), R=[b_hnT, b_wpg], W=[bpE])
                k.op(act, lambda e: e.activation(out=gs[:, half * 512:(half + 1) * 512], in_=pE[:, :], func=AF.Sigmoid), R=[bpE], W=[b_gs])
            k.op(dve, lambda e: e.tensor_tensor(out=gs[:], in0=gs[:], in1=ev[:], op=ALU.mult), R=[b_gs, b_ev], W=[b_gs])
            k.op(dve, lambda e: e.tensor_tensor(out=gs[:], in0=gs[:], in1=xtile[:], op=ALU.add), R=[b_gs, bx], W=[b_gs])
            rmsnorm(gs[:], b_gs, gfin[:], b_gfin, yt[:], b_yt)
            k.dma(sp, y_out[t * 128:(t + 1) * 128, :], yt[:], R=[b_yt], W=[b_y])
    k.finish([b_y])
    return nc, k


_CACHE = {}


def kernel(x_prompt, x_sample, p_prompt, p_sample, rel_bias, g_mix, w_in, conv_w, conv_b, dt_bias, a_log, d_skip, g_ssd,
           w_out, g_ffn, w_router, w_gate, w_up, w_down, g_pg, w_pg, w_ple, g_ple, g_final):
    f = lambda a: np.ascontiguousarray(np.asarray(a, dtype=np.float32))
    xp, xs_ = f(x_prompt), f(x_sample)
    B, S1, _ = xp.shape
    B2, S2, _ = xs_.shape
    pb, sb_ = B // NCORES, B2 // NCORES
    seqs = []
    r = 0
    for i in range(pb):
        seqs.append((r, S1))
        r += S1
    for i in range(sb_):
        seqs.append((r, S2))
        r += S2
    NT = r
    nca, _ = build_a(seqs)
    common = dict(w_in=f(w_in[0]), w_out=f(w_out[0]), w_router=f(w_router[0]), g_mix=f(g_mix[0])[None], g_ffn=f(g_ffn[0])[None],
                  conv_w=f(conv_w[0]), conv_b=f(conv_b[0])[None], dt_bias=f(dt_bias[0]).reshape(1, 16), a_log=f(a_log[0]).reshape(1, 16),
                  d_skip=f(d_skip[0])[None], g_ssd=f(g_ssd[0])[None], rel_bias=f(rel_bias), oh=_onehot_tables())
    xcore = []
    for c in range(NCORES):
        xcore.append(np.concatenate([xp[c * pb:(c + 1) * pb].reshape(-1, D), xs_[c * sb_:(c + 1) * sb_].reshape(-1, D)], 0))
    ra = run_bass_kernel_spmd(nca, [dict(common, x=xcore[c]) for c in range(NCORES)], core_ids=list(range(NCORES)))
    x1 = [ra.results[c]["x1"] for c in range(NCORES)]
    aff = [ra.results[c]["aff"] for c in range(NCORES)]
    n1 = pb * S1
    aff_all = np.stack([np.concatenate([a[:n1] for a in aff], 0), np.concatenate([a[n1:] for a in aff], 0)], 0)
    TG = aff_all.shape[1]
    assert n1 == NT - n1 and TG == NCORES * n1
    ncb, _ = build_b(NT, TG)
    pp, ps_ = f(p_prompt[0]), f(p_sample[0])
    commonb = dict(aff_all=np.ascontiguousarray(aff_all), w_gate=f(w_gate[0]), w_up=f(w_up[0]), w_down=f(w_down[0]), g_ffn=f(g_ffn[0])[None],
                   g_pg=f(g_pg[0])[None], g_ple=f(g_ple[0])[None], g_final=f(g_final)[None], w_pg=f(w_pg[0]), w_ple=f(w_ple[0]))
    inb = []
    for c in range(NCORES):
        pc = np.concatenate([pp[c * pb:(c + 1) * pb].reshape(-1, 256), ps_[c * sb_:(c + 1) * sb_].reshape(-1, 256)], 0)
        inb.append(dict(commonb, x1=x1[c], aff_loc=aff[c], p=pc))
    rb = run_bass_kernel_spmd(ncb, inb, core_ids=list(range(NCORES)))
    ys = [rb.results[c]["y"] for c in range(NCORES)]
    y_p = np.concatenate([y[:n1] for y in ys], 0).reshape(B, S1, D)
    y_s = np.concatenate([y[n1:] for y in ys], 0).reshape(B2, S2, D)
    return y_p, y_s
```

```python
import math
from contextlib import ExitStack
import numpy as np
import concourse.bass as bass
import concourse.mybir as mybir
from concourse.bass_utils import run_bass_kernel_spmd

F32 = mybir.dt.float32
BF16 = mybir.dt.bfloat16
ALU = mybir.AluOpType
AF = mybir.ActivationFunctionType
AX = mybir.AxisListType

NCORES = 8
D = 1024
PAD = 1024
NEG = -30000.0
EPS = 1e-6
DILS = (1, 4, 16)


class Buf:
    __slots__ = ("name", "w", "r", "excl")

    def __init__(self, name, excl=False):
        self.name = name
        self.excl = excl
        self.w = {}
        self.r = {}


class Sem:
    __slots__ = ("h", "total")

    def __init__(self, h):
        self.h = h
        self.total = 0


class Eng:
    def __init__(self, name, handle, sem):
        self.name = name
        self.h = handle
        self.sem = sem
        self.waited = {}
        self.ring = []
        self.rpos = 0


class K:
    def __init__(self, nc, sp_ring=44, pool_ring=20, act_ring=12):
        self.nc = nc
        self.es = ExitStack()
        self.es_cur = self.es
        self.pe = self._eng("pe", nc.tensor)
        self.act = self._eng("act", nc.scalar)
        self.dve = self._eng("dve", nc.vector)
        self.pool = self._eng("pool", nc.gpsimd)
        self.sp = self._eng("sp", nc.sync)
        for e, n in ((self.sp, sp_ring), (self.pool, pool_ring), (self.act, act_ring)):
            e.ring = [self.new_sem(f"r_{e.name}{i}") for i in range(n)]
        self.n_ins = 0

    def new_sem(self, name):
        return Sem(self.es.enter_context(self.nc.semaphore(name)))

    def _eng(self, name, handle):
        return Eng(name, handle, self.new_sem("e_" + name))

    def sb(self, name, shape, dtype):
        self.uid = getattr(self, "uid", 0) + 1
        return self.es_cur.enter_context(self.nc.sbuf_tensor(f"{name}_{self.uid}", list(shape), dtype))

    def barrier(self):
        if getattr(self, "dead", False):
            return
        self._close_pe()
        engs = [self.pe, self.act, self.dve, self.pool, self.sp]
        for e in engs:
            for o in engs:
                if o is not e and o.sem.total > 0:
                    self._wait(e, o.sem, o.sem.total)
                for s in o.ring:
                    if s.total > 0:
                        self._wait(e, s, s.total)

    def scope(self):
        return _Scope(self)

    def ps(self, name, shape, dtype):
        return self.es.enter_context(self.nc.psum_tensor(name, list(shape), dtype))

    def _close_pe(self):
        p = getattr(self, "pe_pending", None)
        if p is not None:
            self.pe.sem.total += 1
            p.then_inc(self.pe.sem.h, 1)
            self.pe_pending = None

    def _wait(self, eng, s, v):
        if eng is self.pe and s is self.pe.sem and getattr(self, "lazy_pe", False):
            return
        if eng.waited.get(id(s), 0) < v:
            eng.h.wait_ge(s.h, v)
            eng.waited[id(s)] = v
            self.n_ins += 1

    def _deps(self, eng, reads, writes):
        for b in reads:
            for s, v in b.w.values():
                self._wait(eng, s, v)
        for b in writes:
            for s, v in b.w.values():
                self._wait(eng, s, v)
            for s, v in b.r.values():
                self._wait(eng, s, v)

    def _record(self, ev, reads, writes):
        s, v = ev
        for b in writes:
            b.w[id(s)] = ev
            b.r = {}
        for b in reads:
            b.r[id(s)] = ev

    def op(self, eng, fn, R=(), W=()):
        if getattr(self, "dead", False):
            return None
        ex = [b for b in R if b.excl]
        if ex:
            W = list(W) + ex
        if eng is self.pe and getattr(self, "lazy_pe", False):
            self._deps(eng, R, W)
            ins = fn(eng.h)
            self.pe_pending = ins
            self.n_ins += 1
            self._record((eng.sem, eng.sem.total + 1), R, W)
            return ins
        self._close_pe()
        self._deps(eng, R, W)
        ins = fn(eng.h)
        eng.sem.total += 1
        ins.then_inc(eng.sem.h, 1)
        self.n_ins += 1
        self._record((eng.sem, eng.sem.total), R, W)
        return ins

    def dma(self, eng, out, in_, R=(), W=(), **kw):
        if getattr(self, "dead", False):
            return None
        self._close_pe()
        self._deps(eng, R, W)
        s = eng.ring[eng.rpos]
        eng.rpos = (eng.rpos + 1) % len(eng.ring)
        self._wait(eng, s, s.total)
        ins = eng.h.dma_start(out=out, in_=in_, **kw)
        s.total += 16
        ins.then_inc(s.h, 16)
        self.n_ins += 1
        self._record((s, s.total), R, W)
        return ins

    def finish(self, bufs):
        self._close_pe()
        self._deps(self.sp, bufs, [])

    def close(self):
        self.es.close()

    def __del__(self):
        pass


class _Scope:
    def __init__(self, k):
        self.k = k

    def __enter__(self):
        self.k.barrier()
        self.old = self.k.es_cur
        self.es = ExitStack()
        self.es.__enter__()
        self.k.es_cur = self.es
        return self

    def __exit__(self, *a):
        self.k.barrier()
        self.k.es_cur = self.old
        return self.es.__exit__(*a)


def _t5_bucket(rel):
    half = 16
    max_exact = 8
    n = np.abs(rel)
    large = max_exact + (np.log(np.maximum(n, 1) / max_exact) / math.log(1024 / max_exact) * (half - max_exact)).astype(np.int32)
    large = np.minimum(large, half - 1)
    return (np.where(rel > 0, half, 0) + np.where(n < max_exact, n, large)).astype(np.int32)


def _onehot_tables():
    oh = np.zeros((3, 33, 384), np.float32)
    for p, d in enumerate(DILS):
        for i in range(384):
            ds = i - 191
            if abs(ds) <= 64:
                oh[p, int(_t5_bucket(np.array(ds * d))), i] = 1.0
            else:
                oh[p, 32, i] = 1.0
    return oh


def build_a(seqs, dbg=False, upto=9):
    nc, k = _build_a(seqs, dbg, upto)
    k.close()
    return nc, k.n_ins


def _build_a(seqs, dbg=False, upto=9):
    NT = sum(s for _, s in seqs)
    SMAX = max(s for _, s in seqs)
    nc = bass.Bass("TRN2", target_bir_lowering=False)
    k = K(nc)

    def din(name, shape):
        return nc.dram_tensor(name, list(shape), F32, kind="ExternalInput").ap()

    x_in = din("x", [NT, D])
    w_in = din("w_in", [D, 3088])
    w_out = din("w_out", [D, D])
    w_router = din("w_router", [D, 16])
    g_mix = din("g_mix", [1, D])
    g_ffn = din("g_ffn", [1, D])
    conv_w = din("conv_w", [5, 1024])
    conv_b = din("conv_b", [1, 1024])
    dt_bias = din("dt_bias", [1, 16])
    a_log = din("a_log", [1, 16])
    d_skip = din("d_skip", [1, 8])
    g_ssd = din("g_ssd", [1, 512])
    rel_bias = din("rel_bias", [32, 8])
    oh_in = din("oh", [3, 33, 384])
    x1_out = nc.dram_tensor("x1", [NT, D], F32, kind="ExternalOutput").ap()
    aff_out = nc.dram_tensor("aff", [NT, 16], F32, kind="ExternalOutput").ap()
    b_x1 = Buf("x1o")
    b_aff = Buf("affo")

    def dscr(name, shape, dt):
        return nc.dram_tensor(name, list(shape), dt).ap(), Buf(name)

    qT_d, b_qT = dscr("qT_d", [512, SMAX], BF16)
    kT_d, b_kT = dscr("kT_d", [512, SMAX], BF16)
    vT_d, b_vT = dscr("vT_d", [512, SMAX], BF16)
    xbc_d, b_xbc = dscr("xbc_d", [1024, SMAX + 4], F32)
    z_d, b_z = dscr("z_d", [SMAX, 512], F32)
    dt_d, b_dt = dscr("dt_d", [SMAX, 16], F32)
    yf_d, b_yf = dscr("yf_d", [SMAX, 512], F32)
    at_d, b_at = dscr("at_d", [8, 64, SMAX], BF16)
    tab_d_t = nc.dram_tensor("tab_d", [3, 8, 384], F32)
    tab_d = tab_d_t.ap()
    b_tab = Buf("tab_d")

    def T(name, shape, dt):
        return k.sb(name, shape, dt), Buf(name)

    w_in_bf, b_win = T("w_in_bf", [128, 8, 3088], BF16)
    wo_att, b_woa = T("wo_att", [64, 8, 1024], BF16)
    wo_ssd, b_wos = T("wo_ssd", [128, 4, 1024], BF16)
    wr_bf, b_wr = T("wr_bf", [128, 8, 16], BF16)
    gmix, b_gmix = T("gmix", [128, D], F32)
    gffn, b_gffn = T("gffn", [128, D], F32)
    gssd, b_gssd = T("gssd", [128, 512], F32)
    cw, b_cw = T("cw", [128, 5, 8], F32)
    cb, b_cb = T("cb", [128, 8], F32)
    dtb, b_dtb = T("dtb", [128, 16], F32)
    Abc, b_A = T("Abc", [128, 16], F32)
    dsk, b_dsk = T("dsk", [128, 8], F32)
    identf, b_idf = T("identf", [128, 128], F32)
    ident, b_id = T("ident", [128, 128], BF16)
    triU, b_triU = T("triU", [128, 128], F32)
    triL, b_triL = T("triL", [128, 128], F32)
    nmU, b_nmU = T("nmU", [128, 128], F32)
    nmL, b_nmL = T("nmL", [128, 128], F32)
    onesf, b_ones = T("onesf", [128, 128], F32)
    biasT, b_bias = T("biasT", [128, 12, 512], BF16)

    PB = []
    for i in range(6):
        PB.append((k.ps(f"pb{i}", [128, 512], F32), Buf(f"pb{i}", excl=True)))
    PH = []
    for i in range(2):
        PH.append((k.ps(f"ph{i}", [128, 1024], BF16), Buf(f"ph{i}", excl=True)))

    sp, act, dve, pool, pe = k.sp, k.act, k.dve, k.pool, k.pe

    def bc_load(dst, bdst, src_row, n):
        k.dma(sp, dst[:], src_row.to_broadcast([128, n]), W=[bdst])

    bc_load(gmix, b_gmix, g_mix[0:1, :], D)
    bc_load(gffn, b_gffn, g_ffn[0:1, :], D)
    bc_load(gssd, b_gssd, g_ssd[0:1, :], 512)
    bc_load(dtb, b_dtb, dt_bias[0:1, :], 16)
    bc_load(Abc, b_A, a_log[0:1, :], 16)
    bc_load(dsk, b_dsk, d_skip[0:1, :], 8)
    for kk in range(5):
        k.dma(sp, cw[:, kk, :], conv_w[kk:kk + 1, :].rearrange("o (c p) -> p (o c)", p=128), W=[b_cw], allow_slow_non_contiguous=True)
    k.dma(sp, cb[:], conv_b.rearrange("o (c p) -> p (o c)", p=128), W=[b_cb], allow_slow_non_contiguous=True)
    k.op(act, lambda e: e.activation(out=Abc[:], in_=Abc[:], func=AF.Exp), R=[b_A], W=[b_A])
    k.op(dve, lambda e: e.tensor_scalar(out=Abc[:], in0=Abc[:], scalar1=-1.0, scalar2=None, op0=ALU.mult), R=[b_A], W=[b_A])

    k.op(pool, lambda e: e.memset(onesf[:], 1.0), W=[b_ones])
    k.op(pool, lambda e: e.memset(identf[:], 0.0), W=[b_idf])
    k.op(pool, lambda e: e.affine_select(out=identf[:], in_=identf[:], pattern=[[-1, 128]], compare_op=ALU.not_equal, fill=1.0, base=0, channel_multiplier=1), R=[b_idf], W=[b_idf])
    k.op(dve, lambda e: e.tensor_copy(out=ident[:], in_=identf[:]), R=[b_idf], W=[b_id])
    k.op(pool, lambda e: e.affine_select(out=triU[:], in_=onesf[:], pattern=[[1, 128]], compare_op=ALU.is_ge, fill=0.0, base=0, channel_multiplier=-1), R=[b_ones], W=[b_triU])
    k.op(pool, lambda e: e.affine_select(out=triL[:], in_=onesf[:], pattern=[[-1, 128]], compare_op=ALU.is_ge, fill=0.0, base=0, channel_multiplier=1), R=[b_ones], W=[b_triL])
    k.op(dve, lambda e: e.tensor_scalar(out=nmU[:], in0=triU[:], scalar1=-1.0, scalar2=-NEG, op0=ALU.add, op1=ALU.mult), R=[b_triU], W=[b_nmU])
    k.op(dve, lambda e: e.tensor_scalar(out=nmL[:], in0=triL[:], scalar1=-1.0, scalar2=-NEG, op0=ALU.add, op1=ALU.mult), R=[b_triL], W=[b_nmL])

    with k.scope():
        stg, b_stg = T("stg", [128, 2048], F32)
        relx, b_relx = T("relx", [33, 8], F32)
        oh_sb, b_oh = T("oh_sb", [33, 3, 384], F32)
        tab_sb, b_tabsb = T("tab_sb", [8, 3, 384], F32)
        for c0 in range(0, 3088, 256):
            cn = min(256, 3088 - c0)
            sv = stg[:, 0:8 * cn].rearrange("p (c n) -> p c n", c=8)
            k.dma(sp, sv, w_in[:, c0:c0 + cn].rearrange("(c p) n -> p c n", p=128), W=[b_stg])
            k.op(dve, lambda e: e.tensor_copy(out=w_in_bf[:, :, c0:c0 + cn], in_=sv), R=[b_stg], W=[b_win])
        for hh in range(4):
            sv = stg[0:64, :].rearrange("p (h n) -> p h n", h=2)
            k.dma(sp, sv, w_out[hh * 128:(hh + 1) * 128, :].rearrange("(h p) n -> p h n", p=64), W=[b_stg])
            k.op(dve, lambda e: e.tensor_copy(out=wo_att[:, hh * 2:(hh + 1) * 2, :], in_=sv), R=[b_stg], W=[b_woa])
        for hh in range(2):
            sv = stg[:, :].rearrange("p (c n) -> p c n", c=2)
            k.dma(sp, sv, w_out[512 + hh * 256:512 + (hh + 1) * 256, :].rearrange("(c p) n -> p c n", p=128), W=[b_stg])
            k.op(dve, lambda e: e.tensor_copy(out=wo_ssd[:, hh * 2:(hh + 1) * 2, :], in_=sv), R=[b_stg], W=[b_wos])
        sv = stg[:, 0:128].rearrange("p (c n) -> p c n", c=8)
        k.dma(sp, sv, w_router.rearrange("(c p) n -> p c n", p=128), W=[b_stg])
        k.op(dve, lambda e: e.tensor_copy(out=wr_bf[:], in_=sv), R=[b_stg], W=[b_wr])

        k.op(pool, lambda e: e.memset(relx[:], NEG), W=[b_relx])
        k.dma(sp, relx[0:32, :], rel_bias[:, :], W=[b_relx])
        k.dma(sp, oh_sb[:], oh_in.rearrange("p b i -> b p i"), W=[b_oh])
        for p in range(3):
            pt, bpt = PB[p % 2]
            k.op(pe, lambda e: e.matmul(pt[0:8, 0:384], lhsT=relx[:, :], rhs=oh_sb[:, p, :], start=True, stop=True), R=[b_relx, b_oh], W=[bpt])
            k.op(act, lambda e: e.copy(out=tab_sb[:, p, :], in_=pt[0:8, 0:384]), R=[bpt], W=[b_tabsb])
        k.dma(sp, tab_d.rearrange("p h i -> h p i"), tab_sb[:], R=[b_tabsb], W=[b_tab])
        for hp in range(4):
            for p in range(3):
                for h in range(2):
                    for jj in range(2):
                        src = bass.AP(tensor=tab_d_t, offset=(p * 8 + hp * 2 + h) * 384 + 127 + 128 * jj, ap=[[1, 128], [-1, 128]])
                        c0 = (h * 2 + jj) * 128
                        k.dma(sp, stg[:, c0:c0 + 128], src, R=[b_tab], W=[b_stg], allow_slow_non_contiguous=True)
                k.op(dve, lambda e: e.tensor_copy(out=biasT[:, hp * 3 + p, :], in_=stg[:, 0:512]), R=[b_stg], W=[b_bias])

    xt = [T(f"xt{i}", [128, D], F32) for i in range(2)]
    junk, b_junk = T("junk", [128, D], F32)
    st_s, b_sts = T("st_s", [128, 8], F32)
    hb, b_hb = T("hb", [128, D], BF16)
    hT, b_hT = T("hT", [128, 8, 512], BF16)
    evb = [T(f"evb{i}", [128, 512], BF16) for i in range(3)]
    evf = [T(f"evf{i}", [128, 512], F32) for i in range(3)]
    zpad, b_zpad = T("zpad", [128, 8, 2], F32)
    k.op(pool, lambda e: e.memset(zpad[:], 0.0), W=[b_zpad])

    def rmsnorm_to_bf(xtile, bx, gtile, bg, out_bf, bout, n=D):
        k.op(act, lambda e: e.activation(out=junk[:, 0:n], in_=xtile, func=AF.Square, accum_out=st_s[:, 0:1]), R=[bx], W=[b_junk, b_sts])
        k.op(dve, lambda e: e.tensor_scalar(out=st_s[:, 1:2], in0=st_s[:, 0:1], scalar1=1.0 / n, scalar2=EPS, op0=ALU.mult, op1=ALU.add), R=[b_sts], W=[b_sts])
        k.op(act, lambda e: e.activation(out=st_s[:, 2:3], in_=st_s[:, 1:2], func=AF.Sqrt), R=[b_sts], W=[b_sts])
        k.op(dve, lambda e: e.reciprocal(out=st_s[:, 3:4], in_=st_s[:, 2:3]), R=[b_sts], W=[b_sts])
        k.op(dve, lambda e: e.scalar_tensor_tensor(out=out_bf, in0=xtile, scalar=st_s[:, 3:4], in1=gtile, op0=ALU.mult, op1=ALU.mult), R=[bx, b_sts, bg], W=[bout])

    cnt = {"ev": 0, "ph": 0, "pb": 0}

    def done():
        k.barrier()
        k.dead = True
        return nc, k

    if upto == 0:
        return done()

    for (row0, S) in seqs:
        k.dma(sp, xbc_d[:, 0:2].rearrange("(c p) t -> p c t", p=128), zpad[:], R=[b_zpad], W=[b_xbc])
        k.dma(sp, xbc_d[:, S + 2:S + 4].rearrange("(c p) t -> p c t", p=128), zpad[:], R=[b_zpad], W=[b_xbc])
        for blk in range(S // 512):
            for t in range(4):
                xtile, bx = xt[t % 2]
                r = row0 + blk * 512 + t * 128
                k.dma(sp, xtile[:], x_in[r:r + 128, :], W=[bx])
                rmsnorm_to_bf(xtile[:], bx, gmix[:], b_gmix, hb[:], b_hb)
                ph, bph = PH[cnt["ph"] % 2]
                cnt["ph"] += 1
                for c in range(8):
                    k.op(pe, lambda e: e.transpose(out=ph[:, c * 128:(c + 1) * 128], in_=hb[:, c * 128:(c + 1) * 128], identity=ident[:]), R=[b_hb, b_id], W=[bph])
                k.op(act, lambda e: e.copy(out=hT[:, :, t * 128:(t + 1) * 128], in_=ph[:, :].rearrange("p (c t) -> p c t", c=8)), R=[bph], W=[b_hT])
            fcs = [(i, i * 128) for i in range(12)] + [(12 + i, 2048 + i * 128) for i in range(8)]
            for (fi, col0) in fcs:
                pt, bpt = PB[cnt["pb"] % 2]
                cnt["pb"] += 1
                for kc in range(8):
                    k.op(pe, lambda e: e.matmul(pt[:, :], lhsT=w_in_bf[:, kc, col0:col0 + 128], rhs=hT[:, kc, :], start=(kc == 0), stop=(kc == 7)), R=[b_win, b_hT], W=[bpt])
                i = cnt["ev"] % 3
                cnt["ev"] += 1
                if fi < 12:
                    et, bet = evb[i]
                    dst, bd = [(qT_d, b_qT), (kT_d, b_kT), (vT_d, b_vT)][fi // 4]
                    drows = dst[(fi % 4) * 128:(fi % 4 + 1) * 128, blk * 512:(blk + 1) * 512]
                else:
                    et, bet = evf[i]
                    dst, bd = xbc_d, b_xbc
                    drows = dst[(fi - 12) * 128:(fi - 11) * 128, 2 + blk * 512:2 + (blk + 1) * 512]
                if fi % 2 == 0:
                    k.op(act, lambda e: e.copy(out=et[:], in_=pt[:, :]), R=[bpt], W=[bet])
                else:
                    k.op(dve, lambda e: e.tensor_copy(out=et[:], in_=pt[:, :]), R=[bpt], W=[bet])
                k.dma(sp, drows, et[:], R=[bet], W=[bd])
            for t in range(4):
                pt, bpt = PB[2 + t % 2]
                for kc in range(8):
                    k.op(pe, lambda e: e.matmul(pt[:, :], lhsT=hT[:, kc, t * 128:(t + 1) * 128], rhs=w_in_bf[:, kc, 1536:2048], start=(kc == 0), stop=(kc == 7)), R=[b_win, b_hT], W=[bpt])
                i = cnt["ev"] % 3
                cnt["ev"] += 1
                et, bet = evf[i]
                k.op(act, lambda e: e.copy(out=et[:], in_=pt[:, :]), R=[bpt], W=[bet])
                r = blk * 512 + t * 128
                k.dma(sp, z_d[r:r + 128, :], et[:], R=[bet], W=[b_z])
                pt2, bpt2 = PB[4 + t % 2]
                for kc in range(8):
                    k.op(pe, lambda e: e.matmul(pt2[:, 0:16], lhsT=hT[:, kc, t * 128:(t + 1) * 128], rhs=w_in_bf[:, kc, 3072:3088], start=(kc == 0), stop=(kc == 7)), R=[b_win, b_hT], W=[bpt2])
                i = cnt["ev"] % 3
                cnt["ev"] += 1
                et, bet = evf[i]
                k.op(dve, lambda e: e.tensor_copy(out=et[:, 0:16], in_=pt2[:, 0:16]), R=[bpt2], W=[bet])
                k.dma(sp, dt_d[r:r + 128, :], et[:, 0:16], R=[bet], W=[b_dt])

        if upto == 1:
            return done()
        with k.scope():
            qp, b_qp = T("qp", [128, SMAX], BF16)
            kp, b_kp = T("kp", [128, SMAX + 2 * PAD], BF16)
            vp, b_vp = T("vp", [128, SMAX + 2 * PAD], BF16)
            k.op(pool, lambda e: e.memset(kp[:], 0.0), W=[b_kp])
            k.op(pool, lambda e: e.memset(vp[:], 0.0), W=[b_vp])
            NVT = 17
            vts = [T(f"vt{i}", [128, 2, 65], BF16) for i in range(NVT + 2)]
            for i, (vt, bvt) in enumerate(vts):
                k.op(pool, lambda e: e.memset(vt[:], 1.0), W=[bvt])
            k.op(pool, lambda e: e.memset(vts[NVT][0][0:64, :, :], 0.0), W=[vts[NVT][1]])
            k.op(pool, lambda e: e.memset(vts[NVT + 1][0][64:128, :, :], 0.0), W=[vts[NVT + 1][1]])
            ef = [T(f"ef{i}", [128, 512], F32) for i in range(2)]
            ex = [T(f"ex{i}", [128, 512], BF16) for i in range(2)]
            acc, b_acc = T("acc", [65, 2, 2048], F32)
            ao, b_ao = T("ao", [64, 2, 2048], BF16)

            NSB = S // 2048
            for hp in range(4):
                k.dma(sp, qp[:, 0:S], qT_d[hp * 128:(hp + 1) * 128, 0:S], R=[b_qT], W=[b_qp])
                k.op(pool, lambda e: e.memset(kp[:, PAD + S:PAD + S + PAD], 0.0), W=[b_kp])
                k.op(pool, lambda e: e.memset(vp[:, PAD + S:PAD + S + PAD], 0.0), W=[b_vp])
                k.dma(sp, kp[:, PAD:PAD + S], kT_d[hp * 128:(hp + 1) * 128, 0:S], R=[b_kT], W=[b_kp])
                k.dma(sp, vp[:, PAD:PAD + S], vT_d[hp * 128:(hp + 1) * 128, 0:S], R=[b_vT], W=[b_vp])
                for sbk in range(NSB):
                    q0 = sbk * 2048
                    for p, d in enumerate(DILS):
                        nq = 2048 // d // 128
                        for r in range(d):
                            def kslice(j):
                                ks = PAD + q0 + r + d * (128 * j - 64)
                                return slice(ks, ks + 127 * d + 1, d)

                            def vtile(j):
                                if sbk == 0 and j == 0:
                                    return vts[NVT]
                                if sbk == NSB - 1 and j == nq:
                                    return vts[NVT + 1]
                                return vts[j]

                            for j in range(nq + 1):
                                ph, bph = PH[cnt["ph"] % 2]
                                cnt["ph"] += 1
                                k.op(pe, lambda e: e.transpose(out=ph[:, 0:128], in_=vp[:, kslice(j)], identity=ident[:]), R=[b_vp, b_id], W=[bph])
                                vt, bvt = vtile(j)
                                if j % 2 == 0:
                                    k.op(act, lambda e: e.copy(out=vt[:, :, 0:64], in_=ph[:, 0:128].rearrange("p (h e) -> p h e", h=2)), R=[bph], W=[bvt])
                                else:
                                    k.op(dve, lambda e: e.tensor_copy(out=vt[:, :, 0:64], in_=ph[:, 0:128].rearrange("p (h e) -> p h e", h=2)), R=[bph], W=[bvt])
                            for qi in range(nq):
                                qs = q0 + r + d * 128 * qi
                                qsl = slice(qs, qs + 127 * d + 1, d)
                                pS, bpS = PB[cnt["pb"] % 2]
                                cnt["pb"] += 1
                                for h in range(2):
                                    for jj in range(2):
                                        c0 = (h * 2 + jj) * 128
                                        k.op(pe, lambda e: e.matmul(pS[:, c0:c0 + 128], lhsT=kp[h * 64:(h + 1) * 64, kslice(qi + jj)], rhs=qp[h * 64:(h + 1) * 64, qsl], start=True, stop=True), R=[b_kp, b_qp], W=[bpS])
                                i = cnt["ev"] % 2
                                cnt["ev"] += 1
                                eft, beft = ef[i]
                                ext, bext = ex[i]
                                k.op(dve, lambda e: e.scalar_tensor_tensor(out=eft[:], in0=pS[:, :], scalar=0.125, in1=biasT[:, hp * 3 + p, :], op0=ALU.mult, op1=ALU.add), R=[bpS, b_bias], W=[beft])
                                k.op(act, lambda e: e.activation(out=ext[:], in_=eft[:], func=AF.Exp), R=[beft], W=[bext])
                                pO, bpO = PB[2 + i]
                                for h in range(2):
                                    for jj in range(2):
                                        c0 = (h * 2 + jj) * 128
                                        vt, bvt = vtile(qi + jj)
                                        k.op(pe, lambda e: e.matmul(pO[0:65, h * 128:(h + 1) * 128], lhsT=vt[:, h, :], rhs=ext[:, c0:c0 + 128], start=(jj == 0), stop=(jj == 1)), R=[bvt, bext], W=[bpO])
                                asl = slice(qs - q0, qs - q0 + 127 * d + 1, d)
                                pOv = pO[0:65, 0:256].rearrange("p (h q) -> p h q", h=2)
                                if p == 0:
                                    k.op(act, lambda e: e.copy(out=acc[:, :, asl], in_=pOv), R=[bpO], W=[b_acc])
                                else:
                                    k.op(dve, lambda e: e.tensor_tensor(out=acc[:, :, asl], in0=acc[:, :, asl], in1=pOv, op=ALU.add), R=[bpO, b_acc], W=[b_acc])
                    k.op(dve, lambda e: e.reciprocal(out=acc[64:65, :, :], in_=acc[64:65, :, :]), R=[b_acc], W=[b_acc])
                    for h in range(2):
                        for c in range(4):
                            pBt, bpB = PB[4 + c % 2]
                            k.op(pe, lambda e: e.matmul(pBt[0:64, :], lhsT=onesf[64:65, 0:64], rhs=acc[64:65, h, c * 512:(c + 1) * 512], start=True, stop=True), R=[b_ones, b_acc], W=[bpB])
                            k.op(dve, lambda e: e.tensor_tensor(out=ao[:, h, c * 512:(c + 1) * 512], in0=acc[0:64, h, c * 512:(c + 1) * 512], in1=pBt[0:64, :], op=ALU.mult), R=[b_acc, bpB], W=[b_ao])
                    k.dma(sp, at_d[hp * 2:hp * 2 + 2, :, q0:q0 + 2048].rearrange("h e t -> e h t"), ao[:], R=[b_ao], W=[b_at])

        if upto == 2:
            return done()
        with k.scope():
            xin = [T(f"xin{i}", [128, 8, 132], F32) for i in range(2)]
            cv_2 = [T(f"cv{i}", [128, 8, 128], F32) for i in range(2)]
            xbf_2 = [T(f"xbf{i}", [128, 8, 128], BF16) for i in range(2)]
            xs_tok_2 = [T(f"xs_tok{i}", [128, 512], BF16) for i in range(2)]
            b_tok_2 = [T(f"b_tok{i}", [128, 2, 128], BF16) for i in range(2)]
            dtr_2 = [T(f"dtr{i}", [128, 16], F32) for i in range(2)]
            dtv_2 = [T(f"dtv{i}", [128, 16], F32) for i in range(2)]
            av_2 = [T(f"av{i}", [128, 16], F32) for i in range(2)]
            cum_2 = [T(f"cum{i}", [128, 8], F32) for i in range(2)]
            ncum_2 = [T(f"ncum{i}", [128, 8], F32) for i in range(2)]
            dsv_2 = [T(f"dsv{i}", [128, 8], F32) for i in range(2)]
            Ev_2 = [T(f"Ev{i}", [128, 8], F32) for i in range(2)]
            cdv_2 = [T(f"cdv{i}", [128, 8], F32) for i in range(2)]
            xdt_2 = [T(f"xdt{i}", [128, 8, 64], BF16) for i in range(2)]
            xdd_2 = [T(f"xdd{i}", [128, 8, 64], BF16) for i in range(2)]
            cbs_2 = [T(f"cbs{i}", [128, 2, 128], F32) for i in range(2)]
            abA, b_abA = T("abA", [128, 8, 128], F32)
            decA, b_decA = T("decA", [128, 8, 128], F32)
            GA, b_GA = T("GA", [128, 8, 128], BF16)
            Hf, b_Hf = T("Hf", [128, 8, 64], F32)
            Hb, b_Hb = T("Hb", [128, 8, 64], BF16)
            ytmp, b_ytmp = T("ytmp", [128, 512], F32)
            ydir, b_ydir = T("ydir", [128, 512], F32)
            yfw, b_yfw = T("yfw", [128, 512], F32)
            zt, b_zt = T("zt", [128, 512], F32)
            ssd_bf, b_ssdbf = T("ssd_bf", [128, 512], BF16)
            ssdT, b_ssdT = T("ssdT", [128, 4, 128], BF16)
            at_sb, b_atsb = T("at_sb", [64, 8, 128], BF16)
            x1t, b_x1t = T("x1t", [128, D], F32)
            hnT, b_hnT = T("hnT", [128, 8, 128], BF16)
            lg, b_lg = T("lg", [128, 16], F32)
            afft, b_afft = T("afft", [128, 16], F32)
            sm, b_sm = T("sm", [128, 8], F32)

            NCH = S // 128
            for direction in range(2):
                k.op(pool, lambda e: e.memset(Hf[:], 0.0), W=[b_Hf])
                k.op(pool, lambda e: e.memset(Hb[:], 0.0), W=[b_Hb])
                tri, b_tri = (triU, b_triU) if direction == 0 else (triL, b_triL)
                nm, b_nm = (nmU, b_nmU) if direction == 0 else (nmL, b_nmL)
                order = range(NCH) if direction == 0 else range(NCH - 1, -1, -1)
                dc = direction * 8
                for ci, c in enumerate(order):
                    t0 = c * 128
                    cv, b_cv = cv_2[ci % 2]
                    xbf, b_xbf = xbf_2[ci % 2]
                    xs_tok, b_xst = xs_tok_2[ci % 2]
                    b_tok, b_btok = b_tok_2[ci % 2]
                    dtr, b_dtr = dtr_2[ci % 2]
                    dtv, b_dtv = dtv_2[ci % 2]
                    av, b_av = av_2[ci % 2]
                    cum, b_cum = cum_2[ci % 2]
                    ncum, b_ncum = ncum_2[ci % 2]
                    dsv, b_dsv = dsv_2[ci % 2]
                    Ev, b_Ev = Ev_2[ci % 2]
                    cdv, b_cdv = cdv_2[ci % 2]
                    xdt, b_xdt = xdt_2[ci % 2]
                    xdd, b_xdd = xdd_2[ci % 2]
                    cbs, b_cbs = cbs_2[ci % 2]
                    xi, bxi = xin[ci % 2]
                    k.dma(sp, xi[:], xbc_d[:, t0:t0 + 132].rearrange("(c p) t -> p c t", p=128), R=[b_xbc], W=[bxi])
                    k.dma(sp, dtr[:], dt_d[t0:t0 + 128, :], R=[b_dt], W=[b_dtr])
                    if upto == 30:
                        return done()
                    for cc in range(8):
                        k.op(dve, lambda e: e.tensor_scalar(out=cv[:, cc, :], in0=xi[:, cc, 0:128], scalar1=cw[:, 0, cc:cc + 1], scalar2=None, op0=ALU.mult), R=[bxi, b_cw], W=[b_cv])
                        for kk in range(1, 5):
                            k.op(dve, lambda e: e.scalar_tensor_tensor(out=cv[:, cc, :], in0=xi[:, cc, kk:kk + 128], scalar=cw[:, kk, cc:cc + 1], in1=cv[:, cc, :], op0=ALU.mult, op1=ALU.add), R=[bxi, b_cw, b_cv], W=[b_cv])
                        k.op(act, lambda e: e.activation(out=xbf[:, cc, :], in_=cv[:, cc, :], func=AF.Silu, bias=cb[:, cc:cc + 1]), R=[b_cv, b_cb], W=[b_xbf])
                    if upto == 31:
                        return done()
                    ph, bph = PH[cnt["ph"] % 2]
                    cnt["ph"] += 1
                    for cc in range(6):
                        k.op(pe, lambda e: e.transpose(out=ph[:, cc * 128:(cc + 1) * 128], in_=xbf[:, cc, :], identity=ident[:]), R=[b_xbf, b_id], W=[bph])
                    k.op(act, lambda e: e.copy(out=xs_tok[:], in_=ph[:, 0:512]), R=[bph], W=[b_xst])
                    k.op(dve, lambda e: e.tensor_copy(out=b_tok[:], in_=ph[:, 512:768].rearrange("p (g n) -> p g n", g=2)), R=[bph], W=[b_btok])
                    if upto == 32:
                        return done()
                    k.op(dve, lambda e: e.tensor_tensor(out=dtv[:], in0=dtr[:], in1=dtb[:], op=ALU.add), R=[b_dtr, b_dtb], W=[b_dtv])
                    k.op(act, lambda e: e.activation(out=dtv[:], in_=dtv[:], func=AF.Exp), R=[b_dtv], W=[b_dtv])
                    k.op(act, lambda e: e.activation(out=dtv[:], in_=dtv[:], func=AF.Ln, bias=1.0), R=[b_dtv], W=[b_dtv])
                    k.op(dve, lambda e: e.tensor_tensor(out=av[:], in0=dtv[:], in1=Abc[:], op=ALU.mult), R=[b_dtv, b_A], W=[b_av])
                    if upto == 3:
                        return done()
                    pC, bpC = PB[0]
                    k.op(pe, lambda e: e.matmul(pC[:, 0:8], lhsT=tri[:, :], rhs=av[:, dc:dc + 8], start=True, stop=True), R=[b_tri, b_av], W=[bpC])
                    k.op(pe, lambda e: e.matmul(pC[:, 8:16], lhsT=onesf[:, :], rhs=av[:, dc:dc + 8], start=True, stop=True), R=[b_ones, b_av], W=[bpC])
                    k.op(dve, lambda e: e.tensor_copy(out=cum[:], in_=pC[:, 0:8]), R=[bpC], W=[b_cum])
                    k.op(dve, lambda e: e.tensor_scalar(out=ncum[:], in0=pC[:, 0:8], scalar1=-1.0, scalar2=None, op0=ALU.mult), R=[bpC], W=[b_ncum])
                    k.op(dve, lambda e: e.tensor_tensor(out=dsv[:], in0=pC[:, 8:16], in1=cum[:], op=ALU.subtract), R=[bpC, b_cum], W=[b_dsv])
                    k.op(act, lambda e: e.activation(out=dsv[:], in_=dsv[:], func=AF.Exp), R=[b_dsv], W=[b_dsv])
                    k.op(act, lambda e: e.activation(out=Ev[:], in_=cum[:], func=AF.Exp), R=[b_cum], W=[b_Ev])
                    k.op(act, lambda e: e.activation(out=cdv[:], in_=pC[:, 8:16], func=AF.Exp), R=[bpC], W=[b_cdv])
                    xs3 = xs_tok[:, :].rearrange("p (h e) -> p h e", h=8)
                    k.op(dve, lambda e: e.tensor_tensor(out=xdt[:], in0=xs3, in1=dtv[:, dc:dc + 8].unsqueeze(2).to_broadcast([128, 8, 64]), op=ALU.mult), R=[b_xst, b_dtv], W=[b_xdt])
                    k.op(dve, lambda e: e.tensor_tensor(out=xdd[:], in0=xdt[:], in1=dsv[:, :].unsqueeze(2).to_broadcast([128, 8, 64]), op=ALU.mult), R=[b_xdt, b_dsv], W=[b_xdd])
                    if upto == 4:
                        return done()
                    pCB, bpCB = PB[1]
                    for g in range(2):
                        k.op(pe, lambda e: e.matmul(pCB[:, g * 128:(g + 1) * 128], lhsT=xbf[:, 4 + g, :], rhs=xbf[:, 6 + g, :], start=True, stop=True), R=[b_xbf], W=[bpCB])
                    k.op(act, lambda e: e.copy(out=cbs[:], in_=pCB[:, 0:256].rearrange("p (g l) -> p g l", g=2)), R=[bpCB], W=[b_cbs])
                    pY, bpY = PB[2]
                    pYo, bpYo = PB[3]
                    k.op(pool, lambda e: e.tensor_copy(out=abA[:], in_=av[:, dc:dc + 8].unsqueeze(2).to_broadcast([128, 8, 128])), R=[b_av], W=[b_abA])
                    for h in range(8):
                        pD, bpD = PB[4 + h // 4]
                        c0 = (h % 4) * 128
                        k.op(pe, lambda e: e.matmul(pD[:, c0:c0 + 128], lhsT=abA[:, h, :], rhs=tri[:, :], start=True, stop=False), R=[b_abA, b_tri], W=[bpD])
                        k.op(pe, lambda e: e.matmul(pD[:, c0:c0 + 128], lhsT=identf[:, :], rhs=nm[:, :], start=False, stop=True), R=[b_idf, b_nm], W=[bpD])
                    for h in range(8):
                        pD, bpD = PB[4 + h // 4]
                        c0 = (h % 4) * 128
                        k.op(act, lambda e: e.activation(out=decA[:, h, :], in_=pD[:, c0:c0 + 128], func=AF.Exp, bias=ncum[:, h:h + 1]), R=[bpD, b_ncum], W=[b_decA])
                    for g in range(2):
                        k.op(dve, lambda e: e.tensor_tensor(out=GA[:, g * 4:(g + 1) * 4, :], in0=decA[:, g * 4:(g + 1) * 4, :], in1=cbs[:, g:g + 1, :].to_broadcast([128, 4, 128]), op=ALU.mult), R=[b_decA, b_cbs], W=[b_GA])
                    for h in range(8):
                        k.op(pe, lambda e: e.matmul(pY[:, h * 64:(h + 1) * 64], lhsT=GA[:, h, :], rhs=xdt[:, h, :], start=True, stop=True), R=[b_GA, b_xdt], W=[bpY])
                    for g in range(2):
                        k.op(pe, lambda e: e.matmul(pYo[:, g * 256:(g + 1) * 256], lhsT=xbf[:, 6 + g, :], rhs=Hb[:, g * 4:(g + 1) * 4, :].rearrange("p h e -> p (h e)"), start=True, stop=True), R=[b_xbf, b_Hb], W=[bpYo])
                    k.op(dve, lambda e: e.tensor_tensor(out=ytmp[:, :].rearrange("p (h e) -> p h e", h=8), in0=pYo[:, :].rearrange("p (h e) -> p h e", h=8), in1=Ev[:, :].unsqueeze(2).to_broadcast([128, 8, 64]), op=ALU.mult), R=[bpYo, b_Ev], W=[b_ytmp])
                    k.op(dve, lambda e: e.tensor_tensor(out=ydir[:], in0=ytmp[:], in1=pY[:, :], op=ALU.add), R=[b_ytmp, bpY], W=[b_ydir])
                    if upto == 5:
                        return done()
                    pSt, bpSt = PB[0]
                    for g in range(2):
                        k.op(pe, lambda e: e.matmul(pSt[:, g * 256:(g + 1) * 256], lhsT=b_tok[:, g, :], rhs=xdd[:, g * 4:(g + 1) * 4, :].rearrange("p h e -> p (h e)"), start=True, stop=True), R=[b_btok, b_xdd], W=[bpSt])
                    k.op(dve, lambda e: e.tensor_tensor(out=Hf[:], in0=Hf[:], in1=cdv[:, :].unsqueeze(2).to_broadcast([128, 8, 64]), op=ALU.mult), R=[b_Hf, b_cdv], W=[b_Hf])
                    k.op(dve, lambda e: e.tensor_tensor(out=Hf[:], in0=Hf[:], in1=pSt[:, :].rearrange("p (h e) -> p h e", h=8), op=ALU.add), R=[b_Hf, bpSt], W=[b_Hf])
                    k.op(pool, lambda e: e.tensor_copy(out=Hb[:], in_=Hf[:]), R=[b_Hf], W=[b_Hb])
                    if direction == 0:
                        k.dma(sp, yf_d[t0:t0 + 128, :], ydir[:], R=[b_ydir], W=[b_yf])
                        continue
                    if upto == 6:
                        return done()
                    k.dma(sp, yfw[:], yf_d[t0:t0 + 128, :], R=[b_yf], W=[b_yfw])
                    k.dma(sp, zt[:], z_d[t0:t0 + 128, :], R=[b_z], W=[b_zt])
                    k.dma(sp, at_sb[:], at_d[:, :, t0:t0 + 128].rearrange("h e t -> e h t"), R=[b_at], W=[b_atsb])
                    xtile, bx = xt[ci % 2]
                    k.dma(sp, xtile[:], x_in[row0 + t0:row0 + t0 + 128, :], W=[bx])
                    k.op(dve, lambda e: e.tensor_tensor(out=ydir[:], in0=ydir[:], in1=yfw[:], op=ALU.add), R=[b_ydir, b_yfw], W=[b_ydir])
                    k.op(pool, lambda e: e.tensor_tensor(out=ytmp[:, :].rearrange("p (h e) -> p h e", h=8), in0=xs3, in1=dsk[:, :].unsqueeze(2).to_broadcast([128, 8, 64]), op=ALU.mult), R=[b_xst, b_dsk], W=[b_ytmp])
                    k.op(dve, lambda e: e.tensor_tensor(out=ydir[:], in0=ydir[:], in1=ytmp[:], op=ALU.add), R=[b_ydir, b_ytmp], W=[b_ydir])
                    k.op(act, lambda e: e.activation(out=zt[:], in_=zt[:], func=AF.Silu), R=[b_zt], W=[b_zt])
                    k.op(dve, lambda e: e.tensor_tensor(out=ydir[:], in0=ydir[:], in1=zt[:], op=ALU.mult), R=[b_ydir, b_zt], W=[b_ydir])
                    for g in range(2):
                        k.op(act, lambda e: e.activation(out=junk[:, g * 256:(g + 1) * 256], in_=ydir[:, g * 256:(g + 1) * 256], func=AF.Square, accum_out=sm[:, g:g + 1]), R=[b_ydir], W=[b_junk, b_sm])
                    k.op(dve, lambda e: e.tensor_scalar(out=sm[:, 2:4], in0=sm[:, 0:2], scalar1=1.0 / 256, scalar2=EPS, op0=ALU.mult, op1=ALU.add), R=[b_sm], W=[b_sm])
                    k.op(act, lambda e: e.activation(out=sm[:, 4:6], in_=sm[:, 2:4], func=AF.Sqrt), R=[b_sm], W=[b_sm])
                    k.op(dve, lambda e: e.reciprocal(out=sm[:, 6:8], in_=sm[:, 4:6]), R=[b_sm], W=[b_sm])
                    for g in range(2):
                        k.op(dve, lambda e: e.scalar_tensor_tensor(out=ssd_bf[:, g * 256:(g + 1) * 256], in0=ydir[:, g * 256:(g + 1) * 256], scalar=sm[:, 6 + g:7 + g], in1=gssd[:, g * 256:(g + 1) * 256], op0=ALU.mult, op1=ALU.mult), R=[b_ydir, b_sm, b_gssd], W=[b_ssdbf])
                    ph, bph = PH[cnt["ph"] % 2]
                    cnt["ph"] += 1
                    for cc in range(4):
                        k.op(pe, lambda e: e.transpose(out=ph[:, cc * 128:(cc + 1) * 128], in_=ssd_bf[:, cc * 128:(cc + 1) * 128], identity=ident[:]), R=[b_ssdbf, b_id], W=[bph])
                    k.op(act, lambda e: e.copy(out=ssdT[:], in_=ph[:, 0:512].rearrange("p (c t) -> p c t", c=4)), R=[bph], W=[b_ssdT])
                    for half in range(2):
                        pX, bpX = PB[4 + half]
                        hs = slice(half * 512, (half + 1) * 512)
                        for h in range(8):
                            k.op(pe, lambda e: e.matmul(pX[:, :], lhsT=at_sb[:, h, :], rhs=wo_att[:, h, hs], start=(h == 0), stop=False), R=[b_atsb, b_woa], W=[bpX])
                        for cc in range(4):
                            k.op(pe, lambda e: e.matmul(pX[:, :], lhsT=ssdT[:, cc, :], rhs=wo_ssd[:, cc, hs], start=False, stop=(cc == 3)), R=[b_ssdT, b_wos], W=[bpX])
                        k.op(dve, lambda e: e.tensor_tensor(out=x1t[:, hs], in0=xtile[:, hs], in1=pX[:, :], op=ALU.add), R=[bx, bpX], W=[b_x1t])
                    k.dma(sp, x1_out[row0 + t0:row0 + t0 + 128, :], x1t[:], R=[b_x1t], W=[b_x1])
                    rmsnorm_to_bf(x1t[:], b_x1t, gffn[:], b_gffn, hb[:], b_hb)
                    ph, bph = PH[cnt["ph"] % 2]
                    cnt["ph"] += 1
                    for cc in range(8):
                        k.op(pe, lambda e: e.transpose(out=ph[:, cc * 128:(cc + 1) * 128], in_=hb[:, cc * 128:(cc + 1) * 128], identity=ident[:]), R=[b_hb, b_id], W=[bph])
                    k.op(act, lambda e: e.copy(out=hnT[:], in_=ph[:, :].rearrange("p (c t) -> p c t", c=8)), R=[bph], W=[b_hnT])
                    pR, bpR = PB[1]
                    for kc in range(8):
                        k.op(pe, lambda e: e.matmul(pR[:, 0:16], lhsT=hnT[:, kc, :], rhs=wr_bf[:, kc, :], start=(kc == 0), stop=(kc == 7)), R=[b_hnT, b_wr], W=[bpR])
                    k.op(dve, lambda e: e.tensor_reduce(out=sm[:, 0:1], in_=pR[:, 0:16], axis=AX.X, op=ALU.max, negate=True), R=[bpR], W=[b_sm])
                    k.op(act, lambda e: e.activation(out=lg[:], in_=pR[:, 0:16], func=AF.Exp, bias=sm[:, 0:1], accum_out=sm[:, 1:2]), R=[bpR, b_sm], W=[b_lg, b_sm])
                    k.op(dve, lambda e: e.reciprocal(out=sm[:, 2:3], in_=sm[:, 1:2]), R=[b_sm], W=[b_sm])
                    k.op(dve, lambda e: e.tensor_scalar(out=afft[:], in0=lg[:], scalar1=sm[:, 2:3], scalar2=None, op0=ALU.mult), R=[b_lg, b_sm], W=[b_afft])
                    k.dma(sp, aff_out[row0 + t0:row0 + t0 + 128, :], afft[:], R=[b_afft], W=[b_aff])

    k.finish([b_x1, b_aff])
    return nc, k


def build_b(NT, TG, n_exp=16, FF=2816):
    nc, k = _build_b(NT, TG, n_exp, FF)
    k.close()
    return nc, k.n_ins


def _build_b(NT, TG, n_exp, FF):
    nc = bass.Bass("TRN2", target_bir_lowering=False)
    k = K(nc)
    k.lazy_pe = True
    sp, act, dve, pool, pe = k.sp, k.act, k.dve, k.pool, k.pe
    NTL = NT // 128
    NFC = FF // 128
    CAP = TG // 8
    TPP = TG // 128

    def din(name, shape):
        return nc.dram_tensor(name, list(shape), F32, kind="ExternalInput").ap()

    x1_in = din("x1", [NT, D])
    aff_all = din("aff_all", [2, TG, 16])
    aff_loc = din("aff_loc", [NT, 16])
    p_in = din("p", [NT, 256])
    w_gate = din("w_gate", [n_exp, D, FF])
    w_up = din("w_up", [n_exp, D, FF])
    w_down = din("w_down", [n_exp, FF, D])
    g_ffn = din("g_ffn", [1, D])
    g_pg = din("g_pg", [1, D])
    g_ple = din("g_ple", [1, D])
    g_fin = din("g_final", [1, D])
    w_pg = din("w_pg", [D, D])
    w_ple = din("w_ple", [256, D])
    y_out = nc.dram_tensor("y", [NT, D], F32, kind="ExternalOutput").ap()
    b_y = Buf("y")
    yacc_d = nc.dram_tensor("yacc_d", [NT, D], F32).ap()

    def T(name, shape, dt):
        return k.sb(name, shape, dt), Buf(name)

    PB = [(k.ps(f"pb{i}", [128, 512], F32), Buf(f"pb{i}", excl=True)) for i in range(6)]
    PH = [(k.ps(f"ph{i}", [128, 1024], BF16), Buf(f"ph{i}", excl=True)) for i in range(2)]

    onesf, b_ones = T("onesf", [128, 128], F32)
    identf, b_idf = T("identf", [128, 128], F32)
    ident, b_id = T("ident", [128, 128], BF16)
    k.op(pool, lambda e: e.memset(onesf[:], 1.0), W=[b_ones])
    k.op(pool, lambda e: e.memset(identf[:], 0.0), W=[b_idf])
    k.op(pool, lambda e: e.affine_select(out=identf[:], in_=identf[:], pattern=[[-1, 128]], compare_op=ALU.not_equal, fill=1.0, base=0, channel_multiplier=1), R=[b_idf], W=[b_idf])
    k.op(dve, lambda e: e.tensor_copy(out=ident[:], in_=identf[:]), R=[b_idf], W=[b_id])
    thr, b_thr = T("thr", [128, 32], F32)

    def rmsnorm(xtile, bx, gtile, bg, out, bout, n=D):
        k.op(act, lambda e: e.activation(out=junk[:, 0:n], in_=xtile, func=AF.Square, accum_out=st_s[:, 0:1]), R=[bx], W=[b_junk, b_sts])
        k.op(dve, lambda e: e.tensor_scalar(out=st_s[:, 1:2], in0=st_s[:, 0:1], scalar1=1.0 / n, scalar2=EPS, op0=ALU.mult, op1=ALU.add), R=[b_sts], W=[b_sts])
        k.op(act, lambda e: e.activation(out=st_s[:, 2:3], in_=st_s[:, 1:2], func=AF.Sqrt), R=[b_sts], W=[b_sts])
        k.op(dve, lambda e: e.reciprocal(out=st_s[:, 3:4], in_=st_s[:, 2:3]), R=[b_sts], W=[b_sts])
        k.op(dve, lambda e: e.scalar_tensor_tensor(out=out, in0=xtile, scalar=st_s[:, 3:4], in1=gtile, op0=ALU.mult, op1=ALU.mult), R=[bx, b_sts, bg], W=[bout])

    with k.scope():
        aff, b_affs = T("aff", [128, 2, TPP, 16], F32)
        cmp_, b_cmp = T("cmp", [128, 2, TPP, 16], BF16)
        cntt, b_cnt = T("cntt", [128, 32], F32)
        lo, b_lo = T("lo", [128, 32], F32)
        hi, b_hi = T("hi", [128, 32], F32)
        mid, b_mid = T("mid", [128, 32], F32)
        ge, b_ge = T("ge", [128, 32], F32)
        d1, b_d1 = T("d1", [128, 32], F32)
        for g in range(2):
            k.dma(sp, aff[:, g, :, :], aff_all[g].rearrange("(p t) e -> p t e", p=128), W=[b_affs])
        k.op(pool, lambda e: e.memset(lo[:], 0.0), W=[b_lo])
        k.op(pool, lambda e: e.memset(hi[:], 1.0), W=[b_hi])
        k.op(pool, lambda e: e.memset(mid[:], 0.5), W=[b_mid])
        for it in range(32):
            mb = mid[:, :].rearrange("p (g e) -> p g e", g=2).unsqueeze(2).to_broadcast([128, 2, TPP, 16])
            k.op(dve, lambda e: e.tensor_tensor(out=cmp_[:], in0=aff[:], in1=mb, op=ALU.is_ge), R=[b_affs, b_mid], W=[b_cmp])
            k.op(dve, lambda e: e.tensor_reduce(out=cntt[:, :].rearrange("p (g e) -> p g e", g=2), in_=cmp_[:].rearrange("p g t e -> p g e t"), axis=AX.X, op=ALU.add), R=[b_cmp], W=[b_cnt])
            pC, bpC = PB[it % 2]
            k.op(pe, lambda e: e.matmul(pC[:, 0:32], lhsT=onesf[:, :], rhs=cntt[:, :], start=True, stop=True), R=[b_ones, b_cnt], W=[bpC])
            k.op(dve, lambda e: e.tensor_scalar(out=ge[:], in0=pC[:, 0:32], scalar1=CAP - 0.5, scalar2=None, op0=ALU.is_ge), R=[bpC], W=[b_ge])
            k.op(dve, lambda e: e.tensor_tensor(out=d1[:], in0=mid[:], in1=lo[:], op=ALU.subtract), R=[b_mid, b_lo], W=[b_d1])
            k.op(dve, lambda e: e.tensor_tensor(out=d1[:], in0=d1[:], in1=ge[:], op=ALU.mult), R=[b_d1, b_ge], W=[b_d1])
            k.op(dve, lambda e: e.tensor_tensor(out=lo[:], in0=lo[:], in1=d1[:], op=ALU.add), R=[b_lo, b_d1], W=[b_lo])
            k.op(dve, lambda e: e.tensor_tensor(out=d1[:], in0=hi[:], in1=mid[:], op=ALU.subtract), R=[b_hi, b_mid], W=[b_d1])
            k.op(dve, lambda e: e.tensor_tensor(out=d1[:], in0=d1[:], in1=ge[:], op=ALU.mult), R=[b_d1, b_ge], W=[b_d1])
            k.op(dve, lambda e: e.tensor_tensor(out=hi[:], in0=mid[:], in1=d1[:], op=ALU.add), R=[b_mid, b_d1], W=[b_hi])
            k.op(dve, lambda e: e.tensor_tensor(out=mid[:], in0=lo[:], in1=hi[:], op=ALU.add), R=[b_lo, b_hi], W=[b_mid])
            k.op(dve, lambda e: e.tensor_scalar(out=mid[:], in0=mid[:], scalar1=0.5, scalar2=None, op0=ALU.mult), R=[b_mid], W=[b_mid])
        k.op(dve, lambda e: e.tensor_copy(out=thr[:], in_=lo[:]), R=[b_lo], W=[b_thr])

    TBT = 8
    RS = 256
    NBLK = NTL // TBT
    b_ytl = [Buf(f"yacct{i}") for i in range(NTL)]
    hn_d = nc.dram_tensor("hn_d", [NT, D], BF16).ap()
    b_hn_d = Buf("hn_d")
    gm, b_gm = T("gm2", [128, NTL, 16], F32)
    slotidx, b_slot = T("slotidx", [128, NTL, 16], F32)
    triU, b_triU = T("triU", [128, 128], F32)
    k.op(pool, lambda e: e.affine_select(out=triU[:], in_=onesf[:], pattern=[[1, 128]], compare_op=ALU.is_ge, fill=0.0, base=0, channel_multiplier=-1), R=[b_ones], W=[b_triU])
    iota_f, b_iota = T("iota_f", [128, RS], F32)
    k.op(pool, lambda e: e.iota(iota_f[:], pattern=[[1, RS]], base=0, channel_multiplier=0, allow_small_or_imprecise_dtypes=True), W=[b_iota])
    cnt = {"ph": 0}
    with k.scope():
        xt = [T(f"xts{i}", [128, D], F32) for i in range(2)]
        junk, b_junk = T("junk_s", [128, D], F32)
        st_s, b_sts = T("st_ss", [128, 8], F32)
        hbs = [T(f"hbs{i}", [128, D], BF16) for i in range(2)]
        gffn, b_gffn = T("gffn_s", [128, D], F32)
        k.dma(sp, gffn[:], g_ffn[0:1, :].to_broadcast([128, D]), W=[b_gffn])
        afl, b_afl = T("afl", [128, 16], F32)
        msk, b_msk = T("msk", [128, 16], F32)
        basev, b_base = T("basev", [128, 16], F32)
        slv, b_slv = T("slv", [128, 16], F32)
        for t in range(NTL):
            g = 0 if t < NTL // 2 else 1
            xtile, bx = xt[t % 2]
            k.dma(sp, xtile[:], x1_in[t * 128:(t + 1) * 128, :], W=[bx])
            k.dma(sp, yacc_d[t * 128:(t + 1) * 128, :], xtile[:], R=[bx], W=[b_ytl[t]])
            k.dma(sp, afl[:], aff_loc[t * 128:(t + 1) * 128, :], W=[b_afl])
            k.op(dve, lambda e: e.tensor_tensor(out=msk[:], in0=afl[:], in1=thr[:, g * 16:(g + 1) * 16], op=ALU.is_ge), R=[b_afl, b_thr], W=[b_msk])
            k.op(dve, lambda e: e.tensor_tensor(out=gm[:, t, :], in0=afl[:], in1=msk[:], op=ALU.mult), R=[b_afl, b_msk], W=[b_gm])
            if t % TBT == 0:
                k.op(pool, lambda e: e.memset(basev[:], 0.0), W=[b_base])
            pC, bpC = PB[t % 2]
            k.op(pe, lambda e: e.matmul(pC[:, 0:16], lhsT=triU[:, :], rhs=msk[:, :], start=True, stop=True), R=[b_triU, b_msk], W=[bpC])
            k.op(pe, lambda e: e.matmul(pC[:, 16:32], lhsT=onesf[:, :], rhs=msk[:, :], start=True, stop=True), R=[b_ones, b_msk], W=[bpC])
            k.op(dve, lambda e: e.tensor_tensor(out=slv[:], in0=pC[:, 0:16], in1=basev[:], op=ALU.add), R=[bpC, b_base], W=[b_slv])
            k.op(dve, lambda e: e.tensor_tensor(out=slv[:], in0=slv[:], in1=msk[:], op=ALU.mult), R=[b_slv, b_msk], W=[b_slv])
            k.op(dve, lambda e: e.tensor_scalar(out=slotidx[:, t, :], in0=slv[:], scalar1=-1.0, scalar2=None, op0=ALU.add), R=[b_slv], W=[b_slot])
            k.op(dve, lambda e: e.tensor_tensor(out=basev[:], in0=basev[:], in1=pC[:, 16:32], op=ALU.add), R=[b_base, bpC], W=[b_base])
            hb, b_hb = hbs[t % 2]
            rmsnorm(xtile[:], bx, gffn[:], b_gffn, hb[:], b_hb)
            k.dma(sp, hn_d[t * 128:(t + 1) * 128, :], hb[:], R=[b_hb], W=[b_hn_d])

    with k.scope():
        stg = [T(f"stgm{i}", [128, 1024], F32) for i in range(2)]
        wg, b_wg = T("wg", [128, 8, FF], BF16)
        wu, b_wu = T("wu", [128, 8, FF], BF16)
        wd, b_wd = T("wd", [128, NFC, D], BF16)
        hnt = [T(f"hnt{i}", [128, D], BF16) for i in range(2)]
        Sm, b_S0 = T("Sm", [128, TBT, RS], BF16)
        b_Sj = [Buf(f"S{j}") for j in range(TBT)]
        STm, b_ST = T("STm", [128, TBT, 2, 128], BF16)
        xsT, b_xsT = T("xsT", [128, 8, RS], BF16)
        actb, b_actb = T("actb", [128, NFC, RS], BF16)
        sg, b_sg = T("sg", [128, RS], F32)
        yeb, b_yeb = T("yeb", [128, 2, D], BF16)
        yos = [T(f"yo{i}", [128, D], F32) for i in range(2)]
        sc = {"stg": 0}

        def load_cast(dst_ap, src_ap, bdst, c):
            (st, bst) = stg[sc["stg"] % 2]
            sc["stg"] += 1
            sv = st[:, 0:src_ap.shape[1] * src_ap.shape[2]].rearrange("p (c n) -> p c n", c=src_ap.shape[1])
            k.dma(sp, sv, src_ap, W=[bst])
            eng = [dve, pool, act][c % 3]
            if eng is act:
                k.op(act, lambda e: e.copy(out=dst_ap, in_=sv), R=[bst], W=[bdst])
            else:
                k.op(eng, lambda e: e.tensor_copy(out=dst_ap, in_=sv), R=[bst], W=[bdst])

        for ex in range(n_exp):
            c = 0
            for (wsrc, wdst, bw) in ((w_gate, wg, b_wg), (w_up, wu, b_wu)):
                for n0 in range(0, FF, 128):
                    load_cast(wdst[:, :, n0:n0 + 128], wsrc[ex, :, n0:n0 + 128].rearrange("(c p) n -> p c n", p=128), bw, c)
                    c += 1
            for f0 in range(0, NFC):
                load_cast(wd[:, f0:f0 + 1, :], w_down[ex, f0 * 128:(f0 + 1) * 128, :].rearrange("(c p) n -> p c n", p=128), b_wd, c)
                c += 1
            for blk in range(NBLK):
                for j in range(TBT):
                    tl = blk * TBT + j
                    k.op(dve, lambda e: e.tensor_scalar(out=Sm[:, j, :], in0=iota_f[:], scalar1=slotidx[:, tl, ex:ex + 1], scalar2=None, op0=ALU.is_equal), R=[b_iota, b_slot], W=[b_Sj[j]])
                    ht, bht = hnt[j % 2]
                    k.dma(sp, ht[:], hn_d[tl * 128:(tl + 1) * 128, :], R=[b_hn_d], W=[bht])
                    for kc in range(8):
                        pg_, bpg_ = PB[kc // 2]
                        k.op(pe, lambda e: e.matmul(pg_[:, (kc % 2) * RS:(kc % 2 + 1) * RS], lhsT=ht[:, kc * 128:(kc + 1) * 128], rhs=Sm[:, j, :], start=(j == 0 and kc % 2 == 0), stop=(j == TBT - 1), skip_group_check=True), R=[bht, b_Sj[j]], W=[bpg_])
                for i in range(4):
                    pg_, bpg_ = PB[i]
                    if i % 2 == 0:
                        k.op(act, lambda e: e.copy(out=xsT[:, 2 * i:2 * i + 2, :], in_=pg_[:, :].rearrange("p (c s) -> p c s", c=2)), R=[bpg_], W=[b_xsT])
                    else:
                        k.op(dve, lambda e: e.tensor_copy(out=xsT[:, 2 * i:2 * i + 2, :], in_=pg_[:, :].rearrange("p (c s) -> p c s", c=2)), R=[bpg_], W=[b_xsT])
                for hh in range(2):
                    ph, bph = PH[hh]
                    for jj in range(4):
                        j = hh * 4 + jj
                        for st_ in range(2):
                            k.op(pe, lambda e: e.transpose(out=ph[:, (jj * 2 + st_) * 128:(jj * 2 + st_ + 1) * 128], in_=Sm[:, j, st_ * 128:(st_ + 1) * 128], identity=ident[:]), R=[b_Sj[j], b_id], W=[bph])
                    if hh == 0:
                        k.op(act, lambda e: e.copy(out=STm[:, 0:4, :, :], in_=ph[:, :].rearrange("p (j s t) -> p j s t", j=4, s=2)), R=[bph], W=[b_ST])
                    else:
                        k.op(dve, lambda e: e.tensor_copy(out=STm[:, 4:8, :, :], in_=ph[:, :].rearrange("p (j s t) -> p j s t", j=4, s=2)), R=[bph], W=[b_ST])
                for fc in range(NFC):
                    pGU, bpGU = PB[4 + fc % 2]
                    for kc in range(8):
                        k.op(pe, lambda e: e.matmul(pGU[:, 0:RS], lhsT=wg[:, kc, fc * 128:(fc + 1) * 128], rhs=xsT[:, kc, :], start=(kc == 0), stop=(kc == 7)), R=[b_wg, b_xsT], W=[bpGU])
                    for kc in range(8):
                        k.op(pe, lambda e: e.matmul(pGU[:, RS:2 * RS], lhsT=wu[:, kc, fc * 128:(fc + 1) * 128], rhs=xsT[:, kc, :], start=(kc == 0), stop=(kc == 7)), R=[b_wu, b_xsT], W=[bpGU])
                    k.op(act, lambda e: e.activation(out=sg[:], in_=pGU[:, 0:RS], func=AF.Silu), R=[bpGU], W=[b_sg])
                    k.op(dve, lambda e: e.tensor_tensor(out=actb[:, fc, :], in0=sg[:], in1=pGU[:, RS:2 * RS], op=ALU.mult), R=[b_sg, bpGU], W=[b_actb])
                for st_ in range(2):
                    for half in range(2):
                        pY, bpY = PB[st_ * 2 + half]
                        for fc in range(NFC):
                            k.op(pe, lambda e: e.matmul(pY[:, :], lhsT=actb[:, fc, st_ * 128:(st_ + 1) * 128], rhs=wd[:, fc, half * 512:(half + 1) * 512], start=(fc == 0), stop=(fc == NFC - 1)), R=[b_actb, b_wd], W=[bpY])
                        if half == 0:
                            k.op(act, lambda e: e.copy(out=yeb[:, st_, 0:512], in_=pY[:, :]), R=[bpY], W=[b_yeb])
                        else:
                            k.op(dve, lambda e: e.tensor_copy(out=yeb[:, st_, 512:1024], in_=pY[:, :]), R=[bpY], W=[b_yeb])
                for j in range(TBT):
                    tl = blk * TBT + j
                    trow = yacc_d[tl * 128:(tl + 1) * 128, :]
                    yo, b_yo = yos[j % 2]
                    for half in range(2):
                        pZ, bpZ = PB[(j % 2) * 2 + half]
                        for st_ in range(2):
                            k.op(pe, lambda e: e.matmul(pZ[:, :], lhsT=STm[:, j, st_, :], rhs=yeb[:, st_, half * 512:(half + 1) * 512], start=(st_ == 0), stop=(st_ == 1)), R=[b_ST, b_yeb], W=[bpZ])
                        if half == 0:
                            k.op(dve, lambda e: e.tensor_scalar(out=yo[:, 0:512], in0=pZ[:, :], scalar1=gm[:, tl, ex:ex + 1], scalar2=None, op0=ALU.mult), R=[bpZ, b_gm], W=[b_yo])
                        else:
                            k.op(act, lambda e: e.activation(out=yo[:, 512:1024], in_=pZ[:, :], func=AF.Copy, scale=gm[:, tl, ex:ex + 1]), R=[bpZ, b_gm], W=[b_yo])
                    k.dma(pool, trow, yo[:], R=[b_yo], W=[b_ytl[tl]], accum_op=ALU.add)

    with k.scope():
        stg = [T(f"stgp{i}", [128, 1024], F32) for i in range(2)]
        xt = [T(f"xtp{i}", [128, D], F32) for i in range(2)]
        junk, b_junk = T("junk_p", [128, D], F32)
        st_s, b_sts = T("st_sp", [128, 8], F32)
        hb, b_hb = T("hb_p", [128, D], BF16)
        hnT, b_hnT = T("hnT_p", [128, 8, 128], BF16)
        wpg, b_wpg = T("wpg", [128, 8, D], BF16)
        wple, b_wple = T("wple", [128, 2, D], BF16)
        gpg, b_gpg = T("gpg", [128, D], F32)
        gple, b_gple = T("gple", [128, D], F32)
        gfin, b_gfin = T("gfin", [128, D], F32)
        for (gt, bg, src) in ((gpg, b_gpg, g_pg), (gple, b_gple, g_ple), (gfin, b_gfin, g_fin)):
            k.dma(sp, gt[:], src[0:1, :].to_broadcast([128, D]), W=[bg])
        for c in range(8):
            st, bst = stg[c % 2]
            k.dma(sp, st[:, 0:D], w_pg[c * 128:(c + 1) * 128, :], W=[bst])
            k.op(dve, lambda e: e.tensor_copy(out=wpg[:, c, :], in_=st[:, 0:D]), R=[bst], W=[b_wpg])
        for c in range(2):
            st, bst = stg[c % 2]
            k.dma(sp, st[:, 0:D], w_ple[c * 128:(c + 1) * 128, :], W=[bst])
            k.op(dve, lambda e: e.tensor_copy(out=wple[:, c, :], in_=st[:, 0:D]), R=[bst], W=[b_wple])
        pt_, b_pt = T("pt", [128, 256], F32)
        pb_, b_pb = T("pbf", [128, 256], BF16)
        pT_, b_pT = T("pT", [128, 2, 128], BF16)
        er, b_er = T("er", [128, D], F32)
        ev, b_ev = T("ev", [128, D], F32)
        gs, b_gs = T("gs", [128, D], F32)
        yt, b_yt = T("yt", [128, D], F32)
        for t in range(NTL):
            xtile, bx = xt[t % 2]
            k.dma(sp, xtile[:], yacc_d[t * 128:(t + 1) * 128, :], R=[b_ytl[t]], W=[bx])
            k.dma(sp, pt_[:], p_in[t * 128:(t + 1) * 128, :], W=[b_pt])
            k.op(pool, lambda e: e.tensor_copy(out=pb_[:], in_=pt_[:]), R=[b_pt], W=[b_pb])
            ph, bph = PH[cnt["ph"] % 2]
            cnt["ph"] += 1
            for c in range(2):
                k.op(pe, lambda e: e.transpose(out=ph[:, c * 128:(c + 1) * 128], in_=pb_[:, c * 128:(c + 1) * 128], identity=ident[:]), R=[b_pb, b_id], W=[bph])
            k.op(act, lambda e: e.copy(out=pT_[:], in_=ph[:, 0:256].rearrange("p (c t) -> p c t", c=2)), R=[bph], W=[b_pT])
            for half in range(2):
                pE, bpE = PB[half]
                for c in range(2):
                    k.op(pe, lambda e: e.matmul(pE[:, :], lhsT=pT_[:, c, :], rhs=wple[:, c, half * 512:(half + 1) * 512], start=(c == 0), stop=(c == 1)), R=[b_pT, b_wple], W=[bpE])
                k.op(act, lambda e: e.copy(out=er[:, half * 512:(half + 1) * 512], in_=pE[:, :]), R=[bpE], W=[b_er])
            rmsnorm(er[:], b_er, gple[:], b_gple, ev[:], b_ev)
            rmsnorm(xtile[:], bx, gpg[:], b_gpg, hb[:], b_hb)
            ph, bph = PH[cnt["ph"] % 2]
            cnt["ph"] += 1
            for c in range(8):
                k.op(pe, lambda e: e.transpose(out=ph[:, c * 128:(c + 1) * 128], in_=hb[:, c * 128:(c + 1) * 128], identity=ident[:]), R=[b_hb, b_id], W=[bph])
            k.op(act, lambda e: e.copy(out=hnT[:], in_=ph[:, :].rearrange("p (c t) -> p c t", c=8)), R=[bph], W=[b_hnT])
            for half in range(2):
                pE, bpE = PB[2 + half]
                for c in range(8):
                    k.op(pe, lambda e: e.matmul(pE[:, :], lhsT=hnT[:, c, :], rhs=wpg[:, c, half * 512:(half + 1) * 512], start=(c == 0), stop=(c == 7)), R=[b_hnT, b_wpg], W=[bpE])
                k.op(act, lambda e: e.activation(out=gs[:, half * 512:(half + 1) * 512], in_=pE[:, :], func=AF.Sigmoid), R=[bpE], W=[b_gs])
            k.op(dve, lambda e: e.tensor_tensor(out=gs[:], in0=gs[:], in1=ev[:], op=ALU.mult), R=[b_gs, b_ev], W=[b_gs])
            k.op(dve, lambda e: e.tensor_tensor(out=gs[:], in0=gs[:], in1=xtile[:], op=ALU.add), R=[b_gs, bx], W=[b_gs])
            rmsnorm(gs[:], b_gs, gfin[:], b_gfin, yt[:], b_yt)
            k.dma(sp, y_out[t * 128:(t + 1) * 128, :], yt[:], R=[b_yt], W=[b_y])
    k.finish([b_y])
    return nc, k


_CACHE = {}


def kernel(x_prompt, x_sample, p_prompt, p_sample, rel_bias, g_mix, w_in, conv_w, conv_b, dt_bias, a_log, d_skip, g_ssd,
           w_out, g_ffn, w_router, w_gate, w_up, w_down, g_pg, w_pg, w_ple, g_ple, g_final):
    f = lambda a: np.ascontiguousarray(np.asarray(a, dtype=np.float32))
    xp, xs_ = f(x_prompt), f(x_sample)
    B, S1, _ = xp.shape
    B2, S2, _ = xs_.shape
    pb, sb_ = B // NCORES, B2 // NCORES
    seqs = []
    r = 0
    for i in range(pb):
        seqs.append((r, S1))
        r += S1
    for i in range(sb_):
        seqs.append((r, S2))
        r += S2
    NT = r
    nca, _ = build_a(seqs)
    common = dict(w_in=f(w_in[0]), w_out=f(w_out[0]), w_router=f(w_router[0]), g_mix=f(g_mix[0])[None], g_ffn=f(g_ffn[0])[None],
                  conv_w=f(conv_w[0]), conv_b=f(conv_b[0])[None], dt_bias=f(dt_bias[0]).reshape(1, 16), a_log=f(a_log[0]).reshape(1, 16),
                  d_skip=f(d_skip[0])[None], g_ssd=f(g_ssd[0])[None], rel_bias=f(rel_bias), oh=_onehot_tables())
    xcore = []
    for c in range(NCORES):
        xcore.append(np.concatenate([xp[c * pb:(c + 1) * pb].reshape(-1, D), xs_[c * sb_:(c + 1) * sb_].reshape(-1, D)], 0))
    ra = run_bass_kernel_spmd(nca, [dict(common, x=xcore[c]) for c in range(NCORES)], core_ids=list(range(NCORES)))
    x1 = [ra.results[c]["x1"] for c in range(NCORES)]
    aff = [ra.results[c]["aff"] for c in range(NCORES)]
    n1 = pb * S1
    aff_all = np.stack([np.concatenate([a[:n1] for a in aff], 0), np.concatenate([a[n1:] for a in aff], 0)], 0)
    TG = aff_all.shape[1]
    assert n1 == NT - n1 and TG == NCORES * n1
    ncb, _ = build_b(NT, TG)
    pp, ps_ = f(p_prompt[0]), f(p_sample[0])
    commonb = dict(aff_all=np.ascontiguousarray(aff_all), w_gate=f(w_gate[0]), w_up=f(w_up[0]), w_down=f(w_down[0]), g_ffn=f(g_ffn[0])[None],
                   g_pg=f(g_pg[0])[None], g_ple=f(g_ple[0])[None], g_final=f(g_final)[None], w_pg=f(w_pg[0]), w_ple=f(w_ple[0]))
    inb = []
    for c in range(NCORES):
        pc = np.concatenate([pp[c * pb:(c + 1) * pb].reshape(-1, 256), ps_[c * sb_:(c + 1) * sb_].reshape(-1, 256)], 0)
        inb.append(dict(commonb, x1=x1[c], aff_loc=aff[c], p=pc))
    rb = run_bass_kernel_spmd(ncb, inb, core_ids=list(range(NCORES)))
    ys = [rb.results[c]["y"] for c in range(NCORES)]
    y_p = np.concatenate([y[:n1] for y in ys], 0).reshape(B, S1, D)
    y_s = np.concatenate([y[n1:] for y in ys], 0).reshape(B2, S2, D)
    return y_p, y_s
```

```python
import math
from contextlib import ExitStack
import numpy as np
import concourse.bass as bass
import concourse.mybir as mybir
from concourse.bass_utils import run_bass_kernel_spmd

F32 = mybir.dt.float32
BF16 = mybir.dt.bfloat16
ALU = mybir.AluOpType
AF = mybir.ActivationFunctionType
AX = mybir.AxisListType

NCORES = 8
D = 1024
PAD = 1024
NEG = -30000.0
EPS = 1e-6
DILS = (1, 4, 16)
LAZY_A = True


class Buf:
    __slots__ = ("name", "w", "r", "excl")

    def __init__(self, name, excl=False):
        self.name = name
        self.excl = excl
        self.w = {}
        self.r = {}


class Sem:
    __slots__ = ("h", "total")

    def __init__(self, h):
        self.h = h
        self.total = 0


class Eng:
    def __init__(self, name, handle, sem):
        self.name = name
        self.h = handle
        self.sem = sem
        self.waited = {}
        self.ring = []
        self.rpos = 0


class K:
    def __init__(self, nc, sp_ring=44, pool_ring=20, act_ring=12):
        self.nc = nc
        self.es = ExitStack()
        self.es_cur = self.es
        self.pe = self._eng("pe", nc.tensor)
        self.act = self._eng("act", nc.scalar)
        self.dve = self._eng("dve", nc.vector)
        self.pool = self._eng("pool", nc.gpsimd)
        self.sp = self._eng("sp", nc.sync)
        for e, n in ((self.sp, sp_ring), (self.pool, pool_ring), (self.act, act_ring)):
            e.ring = [self.new_sem(f"r_{e.name}{i}") for i in range(n)]
        self.n_ins = 0

    def new_sem(self, name):
        return Sem(self.es.enter_context(self.nc.semaphore(name)))

    def _eng(self, name, handle):
        return Eng(name, handle, self.new_sem("e_" + name))

    def sb(self, name, shape, dtype):
        self.uid = getattr(self, "uid", 0) + 1
        return self.es_cur.enter_context(self.nc.sbuf_tensor(f"{name}_{self.uid}", list(shape), dtype))

    def barrier(self):
        if getattr(self, "dead", False):
            return
        self._close_pe()
        engs = [self.pe, self.act, self.dve, self.pool, self.sp]
        for e in engs:
            for o in engs:
                if o is not e and o.sem.total > 0:
                    self._wait(e, o.sem, o.sem.total)
                for s in o.ring:
                    if s.total > 0:
                        self._wait(e, s, s.total)

    def scope(self):
        return _Scope(self)

    def ps(self, name, shape, dtype):
        return self.es.enter_context(self.nc.psum_tensor(name, list(shape), dtype))

    def _close_pe(self):
        p = getattr(self, "pe_pending", None)
        if p is not None:
            self.pe.sem.total += 1
            p.then_inc(self.pe.sem.h, 1)
            self.pe_pending = None

    def _wait(self, eng, s, v):
        if eng is self.pe and s is self.pe.sem and getattr(self, "lazy_pe", False):
            return
        if eng.waited.get(id(s), 0) < v:
            eng.h.wait_ge(s.h, v)
            eng.waited[id(s)] = v
            self.n_ins += 1

    def _deps(self, eng, reads, writes):
        for b in reads:
            for s, v in b.w.values():
                self._wait(eng, s, v)
        for b in writes:
            for s, v in b.w.values():
                self._wait(eng, s, v)
            for s, v in b.r.values():
                self._wait(eng, s, v)

    def _record(self, ev, reads, writes):
        s, v = ev
        for b in writes:
            b.w[id(s)] = ev
            b.r = {}
        for b in reads:
            b.r[id(s)] = ev

    def op(self, eng, fn, R=(), W=()):
        if getattr(self, "dead", False):
            return None
        ex = [b for b in R if b.excl]
        if ex:
            W = list(W) + ex
        if eng is self.pe and getattr(self, "lazy_pe", False):
            self._deps(eng, R, W)
            ins = fn(eng.h)
            self.pe_pending = ins
            self.n_ins += 1
            self._record((eng.sem, eng.sem.total + 1), R, W)
            return ins
        self._close_pe()
        self._deps(eng, R, W)
        ins = fn(eng.h)
        eng.sem.total += 1
        ins.then_inc(eng.sem.h, 1)
        self.n_ins += 1
        self._record((eng.sem, eng.sem.total), R, W)
        return ins

    def dma(self, eng, out, in_, R=(), W=(), **kw):
        if getattr(self, "dead", False):
            return None
        self._close_pe()
        self._deps(eng, R, W)
        s = eng.ring[eng.rpos]
        eng.rpos = (eng.rpos + 1) % len(eng.ring)
        self._wait(eng, s, s.total)
        ins = eng.h.dma_start(out=out, in_=in_, **kw)
        s.total += 16
        ins.then_inc(s.h, 16)
        self.n_ins += 1
        self._record((s, s.total), R, W)
        return ins

    def finish(self, bufs):
        self._close_pe()
        self._deps(self.sp, bufs, [])

    def close(self):
        self.es.close()

    def __del__(self):
        pass


class _Scope:
    def __init__(self, k):
        self.k = k

    def __enter__(self):
        self.k.barrier()
        self.old = self.k.es_cur
        self.es = ExitStack()
        self.es.__enter__()
        self.k.es_cur = self.es
        return self

    def __exit__(self, *a):
        self.k.barrier()
        self.k.es_cur = self.old
        return self.es.__exit__(*a)


def _t5_bucket(rel):
    half = 16
    max_exact = 8
    n = np.abs(rel)
    large = max_exact + (np.log(np.maximum(n, 1) / max_exact) / math.log(1024 / max_exact) * (half - max_exact)).astype(np.int32)
    large = np.minimum(large, half - 1)
    return (np.where(rel > 0, half, 0) + np.where(n < max_exact, n, large)).astype(np.int32)


def _onehot_tables():
    oh = np.zeros((3, 33, 384), np.float32)
    for p, d in enumerate(DILS):
        for i in range(384):
            ds = i - 191
            if abs(ds) <= 64:
                oh[p, int(_t5_bucket(np.array(ds * d))), i] = 1.0
            else:
                oh[p, 32, i] = 1.0
    return oh


def build_a(seqs, dbg=False, upto=9):
    nc, k = _build_a(seqs, dbg, upto)
    k.close()
    return nc, k.n_ins


def _build_a(seqs, dbg=False, upto=9):
    NT = sum(s for _, s in seqs)
    SMAX = max(s for _, s in seqs)
    nc = bass.Bass("TRN2", target_bir_lowering=False)
    k = K(nc)
    k.lazy_pe = LAZY_A

    def din(name, shape):
        return nc.dram_tensor(name, list(shape), F32, kind="ExternalInput").ap()

    x_in = din("x", [NT, D])
    w_in = din("w_in", [D, 3088])
    w_out = din("w_out", [D, D])
    w_router = din("w_router", [D, 16])
    g_mix = din("g_mix", [1, D])
    g_ffn = din("g_ffn", [1, D])
    conv_w = din("conv_w", [5, 1024])
    conv_b = din("conv_b", [1, 1024])
    dt_bias = din("dt_bias", [1, 16])
    a_log = din("a_log", [1, 16])
    d_skip = din("d_skip", [1, 8])
    g_ssd = din("g_ssd", [1, 512])
    rel_bias = din("rel_bias", [32, 8])
    oh_in = din("oh", [3, 33, 384])
    x1_out = nc.dram_tensor("x1", [NT, D], F32, kind="ExternalOutput").ap()
    aff_out = nc.dram_tensor("aff", [NT, 16], F32, kind="ExternalOutput").ap()
    b_x1 = Buf("x1o")
    b_aff = Buf("affo")

    def dscr(name, shape, dt):
        return nc.dram_tensor(name, list(shape), dt).ap(), Buf(name)

    qT_d, b_qT = dscr("qT_d", [512, SMAX], BF16)
    kT_d, b_kT = dscr("kT_d", [512, SMAX], BF16)
    vT_d, b_vT = dscr("vT_d", [512, SMAX], BF16)
    xbc_d, b_xbc = dscr("xbc_d", [1024, SMAX + 4], F32)
    z_d, b_z = dscr("z_d", [SMAX, 512], F32)
    dt_d, b_dt = dscr("dt_d", [SMAX, 16], F32)
    yf_d, b_yf = dscr("yf_d", [SMAX, 512], F32)
    at_d, b_at = dscr("at_d", [8, 64, SMAX], BF16)
    tab_d_t = nc.dram_tensor("tab_d", [3, 8, 384], F32)
    tab_d = tab_d_t.ap()
    b_tab = Buf("tab_d")

    def T(name, shape, dt):
        return k.sb(name, shape, dt), Buf(name)

    w_in_bf, b_win = T("w_in_bf", [128, 8, 3088], BF16)
    wo_att, b_woa = T("wo_att", [64, 8, 1024], BF16)
    wo_ssd, b_wos = T("wo_ssd", [128, 4, 1024], BF16)
    wr_bf, b_wr = T("wr_bf", [128, 8, 16], BF16)
    gmix, b_gmix = T("gmix", [128, D], F32)
    gffn, b_gffn = T("gffn", [128, D], F32)
    gssd, b_gssd = T("gssd", [128, 512], F32)
    cw, b_cw = T("cw", [128, 5, 8], F32)
    cb, b_cb = T("cb", [128, 8], F32)
    dtb, b_dtb = T("dtb", [128, 16], F32)
    Abc, b_A = T("Abc", [128, 16], F32)
    dsk, b_dsk = T("dsk", [128, 8], F32)
    identf, b_idf = T("identf", [128, 128], F32)
    ident, b_id = T("ident", [128, 128], BF16)
    triU, b_triU = T("triU", [128, 128], F32)
    triL, b_triL = T("triL", [128, 128], F32)
    nmU, b_nmU = T("nmU", [128, 128], F32)
    nmL, b_nmL = T("nmL", [128, 128], F32)
    onesf, b_ones = T("onesf", [128, 128], F32)
    biasT, b_bias = T("biasT", [128, 12, 512], BF16)

    PB = []
    for i in range(6):
        PB.append((k.ps(f"pb{i}", [128, 512], F32), Buf(f"pb{i}", excl=True)))
    PH = []
    for i in range(2):
        PH.append((k.ps(f"ph{i}", [128, 1024], BF16), Buf(f"ph{i}", excl=True)))

    sp, act, dve, pool, pe = k.sp, k.act, k.dve, k.pool, k.pe

    def bc_load(dst, bdst, src_row, n):
        k.dma(sp, dst[:], src_row.to_broadcast([128, n]), W=[bdst])

    bc_load(gmix, b_gmix, g_mix[0:1, :], D)
    bc_load(gffn, b_gffn, g_ffn[0:1, :], D)
    bc_load(gssd, b_gssd, g_ssd[0:1, :], 512)
    bc_load(dtb, b_dtb, dt_bias[0:1, :], 16)
    bc_load(Abc, b_A, a_log[0:1, :], 16)
    bc_load(dsk, b_dsk, d_skip[0:1, :], 8)
    for kk in range(5):
        k.dma(sp, cw[:, kk, :], conv_w[kk:kk + 1, :].rearrange("o (c p) -> p (o c)", p=128), W=[b_cw], allow_slow_non_contiguous=True)
    k.dma(sp, cb[:], conv_b.rearrange("o (c p) -> p (o c)", p=128), W=[b_cb], allow_slow_non_contiguous=True)
    k.op(act, lambda e: e.activation(out=Abc[:], in_=Abc[:], func=AF.Exp), R=[b_A], W=[b_A])
    k.op(dve, lambda e: e.tensor_scalar(out=Abc[:], in0=Abc[:], scalar1=-1.0, scalar2=None, op0=ALU.mult), R=[b_A], W=[b_A])

    k.op(pool, lambda e: e.memset(onesf[:], 1.0), W=[b_ones])
    k.op(pool, lambda e: e.memset(identf[:], 0.0), W=[b_idf])
    k.op(pool, lambda e: e.affine_select(out=identf[:], in_=identf[:], pattern=[[-1, 128]], compare_op=ALU.not_equal, fill=1.0, base=0, channel_multiplier=1), R=[b_idf], W=[b_idf])
    k.op(dve, lambda e: e.tensor_copy(out=ident[:], in_=identf[:]), R=[b_idf], W=[b_id])
    k.op(pool, lambda e: e.affine_select(out=triU[:], in_=onesf[:], pattern=[[1, 128]], compare_op=ALU.is_ge, fill=0.0, base=0, channel_multiplier=-1), R=[b_ones], W=[b_triU])
    k.op(pool, lambda e: e.affine_select(out=triL[:], in_=onesf[:], pattern=[[-1, 128]], compare_op=ALU.is_ge, fill=0.0, base=0, channel_multiplier=1), R=[b_ones], W=[b_triL])
    k.op(dve, lambda e: e.tensor_scalar(out=nmU[:], in0=triU[:], scalar1=-1.0, scalar2=-NEG, op0=ALU.add, op1=ALU.mult), R=[b_triU], W=[b_nmU])
    k.op(dve, lambda e: e.tensor_scalar(out=nmL[:], in0=triL[:], scalar1=-1.0, scalar2=-NEG, op0=ALU.add, op1=ALU.mult), R=[b_triL], W=[b_nmL])

    with k.scope():
        stg, b_stg = T("stg", [128, 2048], F32)
        relx, b_relx = T("relx", [33, 8], F32)
        oh_sb, b_oh = T("oh_sb", [33, 3, 384], F32)
        tab_sb, b_tabsb = T("tab_sb", [8, 3, 384], F32)
        for c0 in range(0, 3088, 256):
            cn = min(256, 3088 - c0)
            sv = stg[:, 0:8 * cn].rearrange("p (c n) -> p c n", c=8)
            k.dma(sp, sv, w_in[:, c0:c0 + cn].rearrange("(c p) n -> p c n", p=128), W=[b_stg])
            k.op(dve, lambda e: e.tensor_copy(out=w_in_bf[:, :, c0:c0 + cn], in_=sv), R=[b_stg], W=[b_win])
        for hh in range(4):
            sv = stg[0:64, :].rearrange("p (h n) -> p h n", h=2)
            k.dma(sp, sv, w_out[hh * 128:(hh + 1) * 128, :].rearrange("(h p) n -> p h n", p=64), W=[b_stg])
            k.op(dve, lambda e: e.tensor_copy(out=wo_att[:, hh * 2:(hh + 1) * 2, :], in_=sv), R=[b_stg], W=[b_woa])
        for hh in range(2):
            sv = stg[:, :].rearrange("p (c n) -> p c n", c=2)
            k.dma(sp, sv, w_out[512 + hh * 256:512 + (hh + 1) * 256, :].rearrange("(c p) n -> p c n", p=128), W=[b_stg])
            k.op(dve, lambda e: e.tensor_copy(out=wo_ssd[:, hh * 2:(hh + 1) * 2, :], in_=sv), R=[b_stg], W=[b_wos])
        sv = stg[:, 0:128].rearrange("p (c n) -> p c n", c=8)
        k.dma(sp, sv, w_router.rearrange("(c p) n -> p c n", p=128), W=[b_stg])
        k.op(dve, lambda e: e.tensor_copy(out=wr_bf[:], in_=sv), R=[b_stg], W=[b_wr])

        k.op(pool, lambda e: e.memset(relx[:], NEG), W=[b_relx])
        k.dma(sp, relx[0:32, :], rel_bias[:, :], W=[b_relx])
        k.dma(sp, oh_sb[:], oh_in.rearrange("p b i -> b p i"), W=[b_oh])
        for p in range(3):
            pt, bpt = PB[p % 2]
            k.op(pe, lambda e: e.matmul(pt[0:8, 0:384], lhsT=relx[:, :], rhs=oh_sb[:, p, :], start=True, stop=True), R=[b_relx, b_oh], W=[bpt])
            k.op(act, lambda e: e.copy(out=tab_sb[:, p, :], in_=pt[0:8, 0:384]), R=[bpt], W=[b_tabsb])
        k.dma(sp, tab_d.rearrange("p h i -> h p i"), tab_sb[:], R=[b_tabsb], W=[b_tab])
        for hp in range(4):
            for p in range(3):
                for h in range(2):
                    for jj in range(2):
                        src = bass.AP(tensor=tab_d_t, offset=(p * 8 + hp * 2 + h) * 384 + 127 + 128 * jj, ap=[[1, 128], [-1, 128]])
                        c0 = (h * 2 + jj) * 128
                        k.dma(sp, stg[:, c0:c0 + 128], src, R=[b_tab], W=[b_stg], allow_slow_non_contiguous=True)
                k.op(dve, lambda e: e.tensor_copy(out=biasT[:, hp * 3 + p, :], in_=stg[:, 0:512]), R=[b_stg], W=[b_bias])

    xt = [T(f"xt{i}", [128, D], F32) for i in range(2)]
    junk, b_junk = T("junk", [128, D], F32)
    st_s, b_sts = T("st_s", [128, 8], F32)
    hb, b_hb = T("hb", [128, D], BF16)
    hT, b_hT = T("hT", [128, 8, 512], BF16)
    evb = [T(f"evb{i}", [128, 512], BF16) for i in range(3)]
    evf = [T(f"evf{i}", [128, 512], F32) for i in range(3)]
    zpad, b_zpad = T("zpad", [128, 8, 2], F32)
    k.op(pool, lambda e: e.memset(zpad[:], 0.0), W=[b_zpad])

    def rmsnorm_to_bf(xtile, bx, gtile, bg, out_bf, bout, n=D):
        k.op(act, lambda e: e.activation(out=junk[:, 0:n], in_=xtile, func=AF.Square, accum_out=st_s[:, 0:1]), R=[bx], W=[b_junk, b_sts])
        k.op(dve, lambda e: e.tensor_scalar(out=st_s[:, 1:2], in0=st_s[:, 0:1], scalar1=1.0 / n, scalar2=EPS, op0=ALU.mult, op1=ALU.add), R=[b_sts], W=[b_sts])
        k.op(act, lambda e: e.activation(out=st_s[:, 2:3], in_=st_s[:, 1:2], func=AF.Sqrt), R=[b_sts], W=[b_sts])
        k.op(dve, lambda e: e.reciprocal(out=st_s[:, 3:4], in_=st_s[:, 2:3]), R=[b_sts], W=[b_sts])
        k.op(dve, lambda e: e.scalar_tensor_tensor(out=out_bf, in0=xtile, scalar=st_s[:, 3:4], in1=gtile, op0=ALU.mult, op1=ALU.mult), R=[bx, b_sts, bg], W=[bout])

    cnt = {"ev": 0, "ph": 0, "pb": 0}

    def done():
        k.barrier()
        k.dead = True
        return nc, k

    if upto == 0:
        return done()

    for (row0, S) in seqs:
        k.dma(sp, xbc_d[:, 0:2].rearrange("(c p) t -> p c t", p=128), zpad[:], R=[b_zpad], W=[b_xbc])
        k.dma(sp, xbc_d[:, S + 2:S + 4].rearrange("(c p) t -> p c t", p=128), zpad[:], R=[b_zpad], W=[b_xbc])
        for blk in range(S // 512):
            for t in range(4):
                xtile, bx = xt[t % 2]
                r = row0 + blk * 512 + t * 128
                k.dma(sp, xtile[:], x_in[r:r + 128, :], W=[bx])
                rmsnorm_to_bf(xtile[:], bx, gmix[:], b_gmix, hb[:], b_hb)
                ph, bph = PH[cnt["ph"] % 2]
                cnt["ph"] += 1
                for c in range(8):
                    k.op(pe, lambda e: e.transpose(out=ph[:, c * 128:(c + 1) * 128], in_=hb[:, c * 128:(c + 1) * 128], identity=ident[:]), R=[b_hb, b_id], W=[bph])
                k.op(act, lambda e: e.copy(out=hT[:, :, t * 128:(t + 1) * 128], in_=ph[:, :].rearrange("p (c t) -> p c t", c=8)), R=[bph], W=[b_hT])
            fcs = [(i, i * 128) for i in range(12)] + [(12 + i, 2048 + i * 128) for i in range(8)]
            for (fi, col0) in fcs:
                pt, bpt = PB[cnt["pb"] % 2]
                cnt["pb"] += 1
                for kc in range(8):
                    k.op(pe, lambda e: e.matmul(pt[:, :], lhsT=w_in_bf[:, kc, col0:col0 + 128], rhs=hT[:, kc, :], start=(kc == 0), stop=(kc == 7)), R=[b_win, b_hT], W=[bpt])
                i = cnt["ev"] % 3
                cnt["ev"] += 1
                if fi < 12:
                    et, bet = evb[i]
                    dst, bd = [(qT_d, b_qT), (kT_d, b_kT), (vT_d, b_vT)][fi // 4]
                    drows = dst[(fi % 4) * 128:(fi % 4 + 1) * 128, blk * 512:(blk + 1) * 512]
                else:
                    et, bet = evf[i]
                    dst, bd = xbc_d, b_xbc
                    drows = dst[(fi - 12) * 128:(fi - 11) * 128, 2 + blk * 512:2 + (blk + 1) * 512]
                if fi % 2 == 0:
                    k.op(act, lambda e: e.copy(out=et[:], in_=pt[:, :]), R=[bpt], W=[bet])
                else:
                    k.op(dve, lambda e: e.tensor_copy(out=et[:], in_=pt[:, :]), R=[bpt], W=[bet])
                k.dma(sp, drows, et[:], R=[bet], W=[bd])
            for t in range(4):
                pt, bpt = PB[2 + t % 2]
                for kc in range(8):
                    k.op(pe, lambda e: e.matmul(pt[:, :], lhsT=hT[:, kc, t * 128:(t + 1) * 128], rhs=w_in_bf[:, kc, 1536:2048], start=(kc == 0), stop=(kc == 7)), R=[b_win, b_hT], W=[bpt])
                i = cnt["ev"] % 3
                cnt["ev"] += 1
                et, bet = evf[i]
                k.op(act, lambda e: e.copy(out=et[:], in_=pt[:, :]), R=[bpt], W=[bet])
                r = blk * 512 + t * 128
                k.dma(sp, z_d[r:r + 128, :], et[:], R=[bet], W=[b_z])
                pt2, bpt2 = PB[4 + t % 2]
                for kc in range(8):
                    k.op(pe, lambda e: e.matmul(pt2[:, 0:16], lhsT=hT[:, kc, t * 128:(t + 1) * 128], rhs=w_in_bf[:, kc, 3072:3088], start=(kc == 0), stop=(kc == 7)), R=[b_win, b_hT], W=[bpt2])
                i = cnt["ev"] % 3
                cnt["ev"] += 1
                et, bet = evf[i]
                k.op(dve, lambda e: e.tensor_copy(out=et[:, 0:16], in_=pt2[:, 0:16]), R=[bpt2], W=[bet])
                k.dma(sp, dt_d[r:r + 128, :], et[:, 0:16], R=[bet], W=[b_dt])

        if upto == 1:
            return done()
        with k.scope():
            qp, b_qp = T("qp", [128, SMAX], BF16)
            kp, b_kp = T("kp", [128, SMAX + 2 * PAD], BF16)
            vp, b_vp = T("vp", [128, SMAX + 2 * PAD], BF16)
            k.op(pool, lambda e: e.memset(kp[:], 0.0), W=[b_kp])
            k.op(pool, lambda e: e.memset(vp[:], 0.0), W=[b_vp])
            NVT = 17
            vts = [T(f"vt{i}", [128, 2, 65], BF16) for i in range(NVT + 2)]
            for i, (vt, bvt) in enumerate(vts):
                k.op(pool, lambda e: e.memset(vt[:], 1.0), W=[bvt])
            k.op(pool, lambda e: e.memset(vts[NVT][0][0:64, :, :], 0.0), W=[vts[NVT][1]])
            k.op(pool, lambda e: e.memset(vts[NVT + 1][0][64:128, :, :], 0.0), W=[vts[NVT + 1][1]])
            ef = [T(f"ef{i}", [128, 512], F32) for i in range(2)]
            ex = [T(f"ex{i}", [128, 512], BF16) for i in range(2)]
            acc, b_acc = T("acc", [65, 2, 2048], F32)
            ao, b_ao = T("ao", [64, 2, 2048], BF16)

            k._close_pe()
            k.lazy_pe = False
            NSB = S // 2048
            for hp in range(4):
                k.dma(sp, qp[:, 0:S], qT_d[hp * 128:(hp + 1) * 128, 0:S], R=[b_qT], W=[b_qp])
                k.op(pool, lambda e: e.memset(kp[:, PAD + S:PAD + S + PAD], 0.0), W=[b_kp])
                k.op(pool, lambda e: e.memset(vp[:, PAD + S:PAD + S + PAD], 0.0), W=[b_vp])
                k.dma(sp, kp[:, PAD:PAD + S], kT_d[hp * 128:(hp + 1) * 128, 0:S], R=[b_kT], W=[b_kp])
                k.dma(sp, vp[:, PAD:PAD + S], vT_d[hp * 128:(hp + 1) * 128, 0:S], R=[b_vT], W=[b_vp])
                for sbk in range(NSB):
                    q0 = sbk * 2048
                    for p, d in enumerate(DILS):
                        nq = 2048 // d // 128
                        for r in range(d):
                            def kslice(j):
                                ks = PAD + q0 + r + d * (128 * j - 64)
                                return slice(ks, ks + 127 * d + 1, d)

                            def vtile(j):
                                if sbk == 0 and j == 0:
                                    return vts[NVT]
                                if sbk == NSB - 1 and j == nq:
                                    return vts[NVT + 1]
                                return vts[j]

                            for j in range(nq + 1):
                                ph, bph = PH[cnt["ph"] % 2]
                                cnt["ph"] += 1
                                k.op(pe, lambda e: e.transpose(out=ph[:, 0:128], in_=vp[:, kslice(j)], identity=ident[:]), R=[b_vp, b_id], W=[bph])
                                vt, bvt = vtile(j)
                                if j % 2 == 0:
                                    k.op(act, lambda e: e.copy(out=vt[:, :, 0:64], in_=ph[:, 0:128].rearrange("p (h e) -> p h e", h=2)), R=[bph], W=[bvt])
                                else:
                                    k.op(dve, lambda e: e.tensor_copy(out=vt[:, :, 0:64], in_=ph[:, 0:128].rearrange("p (h e) -> p h e", h=2)), R=[bph], W=[bvt])
                            for qi in range(nq):
                                qs = q0 + r + d * 128 * qi
                                qsl = slice(qs, qs + 127 * d + 1, d)
                                pS, bpS = PB[cnt["pb"] % 2]
                                cnt["pb"] += 1
                                for h in range(2):
                                    for jj in range(2):
                                        c0 = (h * 2 + jj) * 128
                                        k.op(pe, lambda e: e.matmul(pS[:, c0:c0 + 128], lhsT=kp[h * 64:(h + 1) * 64, kslice(qi + jj)], rhs=qp[h * 64:(h + 1) * 64, qsl], start=True, stop=True), R=[b_kp, b_qp], W=[bpS])
                                i = cnt["ev"] % 2
                                cnt["ev"] += 1
                                eft, beft = ef[i]
                                ext, bext = ex[i]
                                k.op(dve, lambda e: e.scalar_tensor_tensor(out=eft[:], in0=pS[:, :], scalar=0.125, in1=biasT[:, hp * 3 + p, :], op0=ALU.mult, op1=ALU.add), R=[bpS, b_bias], W=[beft])
                                k.op(act, lambda e: e.activation(out=ext[:], in_=eft[:], func=AF.Exp), R=[beft], W=[bext])
                                pO, bpO = PB[2 + i]
                                for h in range(2):
                                    for jj in range(2):
                                        c0 = (h * 2 + jj) * 128
                                        vt, bvt = vtile(qi + jj)
                                        k.op(pe, lambda e: e.matmul(pO[0:65, h * 128:(h + 1) * 128], lhsT=vt[:, h, :], rhs=ext[:, c0:c0 + 128], start=(jj == 0), stop=(jj == 1)), R=[bvt, bext], W=[bpO])
                                asl = slice(qs - q0, qs - q0 + 127 * d + 1, d)
                                pOv = pO[0:65, 0:256].rearrange("p (h q) -> p h q", h=2)
                                if p == 0:
                                    k.op(act, lambda e: e.copy(out=acc[:, :, asl], in_=pOv), R=[bpO], W=[b_acc])
                                else:
                                    k.op(dve, lambda e: e.tensor_tensor(out=acc[:, :, asl], in0=acc[:, :, asl], in1=pOv, op=ALU.add), R=[bpO, b_acc], W=[b_acc])
                    k.op(dve, lambda e: e.reciprocal(out=acc[64:65, :, :], in_=acc[64:65, :, :]), R=[b_acc], W=[b_acc])
                    for h in range(2):
                        for c in range(4):
                            pBt, bpB = PB[4 + c % 2]
                            k.op(pe, lambda e: e.matmul(pBt[0:64, :], lhsT=onesf[64:65, 0:64], rhs=acc[64:65, h, c * 512:(c + 1) * 512], start=True, stop=True), R=[b_ones, b_acc], W=[bpB])
                            k.op(dve, lambda e: e.tensor_tensor(out=ao[:, h, c * 512:(c + 1) * 512], in0=acc[0:64, h, c * 512:(c + 1) * 512], in1=pBt[0:64, :], op=ALU.mult), R=[b_acc, bpB], W=[b_ao])
                    k.dma(sp, at_d[hp * 2:hp * 2 + 2, :, q0:q0 + 2048].rearrange("h e t -> e h t"), ao[:], R=[b_ao], W=[b_at])

        k.lazy_pe = LAZY_A
        if upto == 2:
            return done()
        with k.scope():
            xin = [T(f"xin{i}", [128, 8, 132], F32) for i in range(2)]
            cv_2 = [T(f"cv{i}", [128, 8, 128], F32) for i in range(2)]
            xbf_2 = [T(f"xbf{i}", [128, 8, 128], BF16) for i in range(2)]
            xs_tok_2 = [T(f"xs_tok{i}", [128, 512], BF16) for i in range(2)]
            b_tok_2 = [T(f"b_tok{i}", [128, 2, 128], BF16) for i in range(2)]
            dtr_2 = [T(f"dtr{i}", [128, 16], F32) for i in range(2)]
            dtv_2 = [T(f"dtv{i}", [128, 16], F32) for i in range(2)]
            av_2 = [T(f"av{i}", [128, 16], F32) for i in range(2)]
            cum_2 = [T(f"cum{i}", [128, 8], F32) for i in range(2)]
            ncum_2 = [T(f"ncum{i}", [128, 8], F32) for i in range(2)]
            dsv_2 = [T(f"dsv{i}", [128, 8], F32) for i in range(2)]
            Ev_2 = [T(f"Ev{i}", [128, 8], F32) for i in range(2)]
            cdv_2 = [T(f"cdv{i}", [128, 8], F32) for i in range(2)]
            xdt_2 = [T(f"xdt{i}", [128, 8, 64], BF16) for i in range(2)]
            xdd_2 = [T(f"xdd{i}", [128, 8, 64], BF16) for i in range(2)]
            cbs_2 = [T(f"cbs{i}", [128, 2, 128], F32) for i in range(2)]
            abA, b_abA = T("abA", [128, 8, 128], F32)
            decA, b_decA = T("decA", [128, 8, 128], F32)
            GA, b_GA = T("GA", [128, 8, 128], BF16)
            Hf, b_Hf = T("Hf", [128, 8, 64], F32)
            Hb, b_Hb = T("Hb", [128, 8, 64], BF16)
            ytmp, b_ytmp = T("ytmp", [128, 512], F32)
            ydir, b_ydir = T("ydir", [128, 512], F32)
            yfw, b_yfw = T("yfw", [128, 512], F32)
            zt, b_zt = T("zt", [128, 512], F32)
            ssd_bf, b_ssdbf = T("ssd_bf", [128, 512], BF16)
            ssdT, b_ssdT = T("ssdT", [128, 4, 128], BF16)
            at_sb, b_atsb = T("at_sb", [64, 8, 128], BF16)
            x1t, b_x1t = T("x1t", [128, D], F32)
            hnT, b_hnT = T("hnT", [128, 8, 128], BF16)
            lg, b_lg = T("lg", [128, 16], F32)
            afft, b_afft = T("afft", [128, 16], F32)
            sm, b_sm = T("sm", [128, 8], F32)

            NCH = S // 128
            for direction in range(2):
                k.op(pool, lambda e: e.memset(Hf[:], 0.0), W=[b_Hf])
                k.op(pool, lambda e: e.memset(Hb[:], 0.0), W=[b_Hb])
                tri, b_tri = (triU, b_triU) if direction == 0 else (triL, b_triL)
                nm, b_nm = (nmU, b_nmU) if direction == 0 else (nmL, b_nmL)
                order = range(NCH) if direction == 0 else range(NCH - 1, -1, -1)
                dc = direction * 8
                for ci, c in enumerate(order):
                    t0 = c * 128
                    cv, b_cv = cv_2[ci % 2]
                    xbf, b_xbf = xbf_2[ci % 2]
                    xs_tok, b_xst = xs_tok_2[ci % 2]
                    b_tok, b_btok = b_tok_2[ci % 2]
                    dtr, b_dtr = dtr_2[ci % 2]
                    dtv, b_dtv = dtv_2[ci % 2]
                    av, b_av = av_2[ci % 2]
                    cum, b_cum = cum_2[ci % 2]
                    ncum, b_ncum = ncum_2[ci % 2]
                    dsv, b_dsv = dsv_2[ci % 2]
                    Ev, b_Ev = Ev_2[ci % 2]
                    cdv, b_cdv = cdv_2[ci % 2]
                    xdt, b_xdt = xdt_2[ci % 2]
                    xdd, b_xdd = xdd_2[ci % 2]
                    cbs, b_cbs = cbs_2[ci % 2]
                    xi, bxi = xin[ci % 2]
                    k.dma(sp, xi[:], xbc_d[:, t0:t0 + 132].rearrange("(c p) t -> p c t", p=128), R=[b_xbc], W=[bxi])
                    k.dma(sp, dtr[:], dt_d[t0:t0 + 128, :], R=[b_dt], W=[b_dtr])
                    if upto == 30:
                        return done()
                    for cc in range(8):
                        k.op(dve, lambda e: e.tensor_scalar(out=cv[:, cc, :], in0=xi[:, cc, 0:128], scalar1=cw[:, 0, cc:cc + 1], scalar2=None, op0=ALU.mult), R=[bxi, b_cw], W=[b_cv])
                        for kk in range(1, 5):
                            k.op(dve, lambda e: e.scalar_tensor_tensor(out=cv[:, cc, :], in0=xi[:, cc, kk:kk + 128], scalar=cw[:, kk, cc:cc + 1], in1=cv[:, cc, :], op0=ALU.mult, op1=ALU.add), R=[bxi, b_cw, b_cv], W=[b_cv])
                        k.op(act, lambda e: e.activation(out=xbf[:, cc, :], in_=cv[:, cc, :], func=AF.Silu, bias=cb[:, cc:cc + 1]), R=[b_cv, b_cb], W=[b_xbf])
                    if upto == 31:
                        return done()
                    ph, bph = PH[cnt["ph"] % 2]
                    cnt["ph"] += 1
                    for cc in range(6):
                        k.op(pe, lambda e: e.transpose(out=ph[:, cc * 128:(cc + 1) * 128], in_=xbf[:, cc, :], identity=ident[:]), R=[b_xbf, b_id], W=[bph])
                    k.op(act, lambda e: e.copy(out=xs_tok[:], in_=ph[:, 0:512]), R=[bph], W=[b_xst])
                    k.op(dve, lambda e: e.tensor_copy(out=b_tok[:], in_=ph[:, 512:768].rearrange("p (g n) -> p g n", g=2)), R=[bph], W=[b_btok])
                    if upto == 32:
                        return done()
                    k.op(dve, lambda e: e.tensor_tensor(out=dtv[:], in0=dtr[:], in1=dtb[:], op=ALU.add), R=[b_dtr, b_dtb], W=[b_dtv])
                    k.op(act, lambda e: e.activation(out=dtv[:], in_=dtv[:], func=AF.Exp), R=[b_dtv], W=[b_dtv])
                    k.op(act, lambda e: e.activation(out=dtv[:], in_=dtv[:], func=AF.Ln, bias=1.0), R=[b_dtv], W=[b_dtv])
                    k.op(dve, lambda e: e.tensor_tensor(out=av[:], in0=dtv[:], in1=Abc[:], op=ALU.mult), R=[b_dtv, b_A], W=[b_av])
                    if upto == 3:
                        return done()
                    pC, bpC = PB[0]
                    k.op(pe, lambda e: e.matmul(pC[:, 0:8], lhsT=tri[:, :], rhs=av[:, dc:dc + 8], start=True, stop=True), R=[b_tri, b_av], W=[bpC])
                    k.op(pe, lambda e: e.matmul(pC[:, 8:16], lhsT=onesf[:, :], rhs=av[:, dc:dc + 8], start=True, stop=True), R=[b_ones, b_av], W=[bpC])
                    k.op(dve, lambda e: e.tensor_copy(out=cum[:], in_=pC[:, 0:8]), R=[bpC], W=[b_cum])
                    k.op(dve, lambda e: e.tensor_scalar(out=ncum[:], in0=pC[:, 0:8], scalar1=-1.0, scalar2=None, op0=ALU.mult), R=[bpC], W=[b_ncum])
                    k.op(dve, lambda e: e.tensor_tensor(out=dsv[:], in0=pC[:, 8:16], in1=cum[:], op=ALU.subtract), R=[bpC, b_cum], W=[b_dsv])
                    k.op(act, lambda e: e.activation(out=dsv[:], in_=dsv[:], func=AF.Exp), R=[b_dsv], W=[b_dsv])
                    k.op(act, lambda e: e.activation(out=Ev[:], in_=cum[:], func=AF.Exp), R=[b_cum], W=[b_Ev])
                    k.op(act, lambda e: e.activation(out=cdv[:], in_=pC[:, 8:16], func=AF.Exp), R=[bpC], W=[b_cdv])
                    xs3 = xs_tok[:, :].rearrange("p (h e) -> p h e", h=8)
                    k.op(dve, lambda e: e.tensor_tensor(out=xdt[:], in0=xs3, in1=dtv[:, dc:dc + 8].unsqueeze(2).to_broadcast([128, 8, 64]), op=ALU.mult), R=[b_xst, b_dtv], W=[b_xdt])
                    k.op(dve, lambda e: e.tensor_tensor(out=xdd[:], in0=xdt[:], in1=dsv[:, :].unsqueeze(2).to_broadcast([128, 8, 64]), op=ALU.mult), R=[b_xdt, b_dsv], W=[b_xdd])
                    if upto == 4:
                        return done()
                    pCB, bpCB = PB[1]
                    for g in range(2):
                        k.op(pe, lambda e: e.matmul(pCB[:, g * 128:(g + 1) * 128], lhsT=xbf[:, 4 + g, :], rhs=xbf[:, 6 + g, :], start=True, stop=True), R=[b_xbf], W=[bpCB])
                    k.op(act, lambda e: e.copy(out=cbs[:], in_=pCB[:, 0:256].rearrange("p (g l) -> p g l", g=2)), R=[bpCB], W=[b_cbs])
                    pY, bpY = PB[2]
                    pYo, bpYo = PB[3]
                    k.op(pool, lambda e: e.tensor_copy(out=abA[:], in_=av[:, dc:dc + 8].unsqueeze(2).to_broadcast([128, 8, 128])), R=[b_av], W=[b_abA])
                    for h in range(8):
                        pD, bpD = PB[4 + h // 4]
                        c0 = (h % 4) * 128
                        k.op(pe, lambda e: e.matmul(pD[:, c0:c0 + 128], lhsT=abA[:, h, :], rhs=tri[:, :], start=True, stop=False), R=[b_abA, b_tri], W=[bpD])
                        k.op(pe, lambda e: e.matmul(pD[:, c0:c0 + 128], lhsT=identf[:, :], rhs=nm[:, :], start=False, stop=True), R=[b_idf, b_nm], W=[bpD])
                    for h in range(8):
                        pD, bpD = PB[4 + h // 4]
                        c0 = (h % 4) * 128
                        k.op(act, lambda e: e.activation(out=decA[:, h, :], in_=pD[:, c0:c0 + 128], func=AF.Exp, bias=ncum[:, h:h + 1]), R=[bpD, b_ncum], W=[b_decA])
                    for g in range(2):
                        k.op(dve, lambda e: e.tensor_tensor(out=GA[:, g * 4:(g + 1) * 4, :], in0=decA[:, g * 4:(g + 1) * 4, :], in1=cbs[:, g:g + 1, :].to_broadcast([128, 4, 128]), op=ALU.mult), R=[b_decA, b_cbs], W=[b_GA])
                    for h in range(8):
                        k.op(pe, lambda e: e.matmul(pY[:, h * 64:(h + 1) * 64], lhsT=GA[:, h, :], rhs=xdt[:, h, :], start=True, stop=True), R=[b_GA, b_xdt], W=[bpY])
                    for g in range(2):
                        k.op(pe, lambda e: e.matmul(pYo[:, g * 256:(g + 1) * 256], lhsT=xbf[:, 6 + g, :], rhs=Hb[:, g * 4:(g + 1) * 4, :].rearrange("p h e -> p (h e)"), start=True, stop=True), R=[b_xbf, b_Hb], W=[bpYo])
                    k.op(dve, lambda e: e.tensor_tensor(out=ytmp[:, :].rearrange("p (h e) -> p h e", h=8), in0=pYo[:, :].rearrange("p (h e) -> p h e", h=8), in1=Ev[:, :].unsqueeze(2).to_broadcast([128, 8, 64]), op=ALU.mult), R=[bpYo, b_Ev], W=[b_ytmp])
                    k.op(dve, lambda e: e.tensor_tensor(out=ydir[:], in0=ytmp[:], in1=pY[:, :], op=ALU.add), R=[b_ytmp, bpY], W=[b_ydir])
                    if upto == 5:
                        return done()
                    pSt, bpSt = PB[0]
                    for g in range(2):
                        k.op(pe, lambda e: e.matmul(pSt[:, g * 256:(g + 1) * 256], lhsT=b_tok[:, g, :], rhs=xdd[:, g * 4:(g + 1) * 4, :].rearrange("p h e -> p (h e)"), start=True, stop=True), R=[b_btok, b_xdd], W=[bpSt])
                    k.op(dve, lambda e: e.tensor_tensor(out=Hf[:], in0=Hf[:], in1=cdv[:, :].unsqueeze(2).to_broadcast([128, 8, 64]), op=ALU.mult), R=[b_Hf, b_cdv], W=[b_Hf])
                    k.op(dve, lambda e: e.tensor_tensor(out=Hf[:], in0=Hf[:], in1=pSt[:, :].rearrange("p (h e) -> p h e", h=8), op=ALU.add), R=[b_Hf, bpSt], W=[b_Hf])
                    k.op(pool, lambda e: e.tensor_copy(out=Hb[:], in_=Hf[:]), R=[b_Hf], W=[b_Hb])
                    if direction == 0:
                        k.dma(sp, yf_d[t0:t0 + 128, :], ydir[:], R=[b_ydir], W=[b_yf])
                        continue
                    if upto == 6:
                        return done()
                    k.dma(sp, yfw[:], yf_d[t0:t0 + 128, :], R=[b_yf], W=[b_yfw])
                    k.dma(sp, zt[:], z_d[t0:t0 + 128, :], R=[b_z], W=[b_zt])
                    k.dma(sp, at_sb[:], at_d[:, :, t0:t0 + 128].rearrange("h e t -> e h t"), R=[b_at], W=[b_atsb])
                    xtile, bx = xt[ci % 2]
                    k.dma(sp, xtile[:], x_in[row0 + t0:row0 + t0 + 128, :], W=[bx])
                    k.op(dve, lambda e: e.tensor_tensor(out=ydir[:], in0=ydir[:], in1=yfw[:], op=ALU.add), R=[b_ydir, b_yfw], W=[b_ydir])
                    k.op(pool, lambda e: e.tensor_tensor(out=ytmp[:, :].rearrange("p (h e) -> p h e", h=8), in0=xs3, in1=dsk[:, :].unsqueeze(2).to_broadcast([128, 8, 64]), op=ALU.mult), R=[b_xst, b_dsk], W=[b_ytmp])
                    k.op(dve, lambda e: e.tensor_tensor(out=ydir[:], in0=ydir[:], in1=ytmp[:], op=ALU.add), R=[b_ydir, b_ytmp], W=[b_ydir])
                    k.op(act, lambda e: e.activation(out=zt[:], in_=zt[:], func=AF.Silu), R=[b_zt], W=[b_zt])
                    k.op(dve, lambda e: e.tensor_tensor(out=ydir[:], in0=ydir[:], in1=zt[:], op=ALU.mult), R=[b_ydir, b_zt], W=[b_ydir])
                    for g in range(2):
                        k.op(act, lambda e: e.activation(out=junk[:, g * 256:(g + 1) * 256], in_=ydir[:, g * 256:(g + 1) * 256], func=AF.Square, accum_out=sm[:, g:g + 1]), R=[b_ydir], W=[b_junk, b_sm])
                    k.op(dve, lambda e: e.tensor_scalar(out=sm[:, 2:4], in0=sm[:, 0:2], scalar1=1.0 / 256, scalar2=EPS, op0=ALU.mult, op1=ALU.add), R=[b_sm], W=[b_sm])
                    k.op(act, lambda e: e.activation(out=sm[:, 4:6], in_=sm[:, 2:4], func=AF.Sqrt), R=[b_sm], W=[b_sm])
                    k.op(dve, lambda e: e.reciprocal(out=sm[:, 6:8], in_=sm[:, 4:6]), R=[b_sm], W=[b_sm])
                    for g in range(2):
                        k.op(dve, lambda e: e.scalar_tensor_tensor(out=ssd_bf[:, g * 256:(g + 1) * 256], in0=ydir[:, g * 256:(g + 1) * 256], scalar=sm[:, 6 + g:7 + g], in1=gssd[:, g * 256:(g + 1) * 256], op0=ALU.mult, op1=ALU.mult), R=[b_ydir, b_sm, b_gssd], W=[b_ssdbf])
                    ph, bph = PH[cnt["ph"] % 2]
                    cnt["ph"] += 1
                    for cc in range(4):
                        k.op(pe, lambda e: e.transpose(out=ph[:, cc * 128:(cc + 1) * 128], in_=ssd_bf[:, cc * 128:(cc + 1) * 128], identity=ident[:]), R=[b_ssdbf, b_id], W=[bph])
                    k.op(act, lambda e: e.copy(out=ssdT[:], in_=ph[:, 0:512].rearrange("p (c t) -> p c t", c=4)), R=[bph], W=[b_ssdT])
                    for half in range(2):
                        pX, bpX = PB[4 + half]
                        hs = slice(half * 512, (half + 1) * 512)
                        for h in range(8):
                            k.op(pe, lambda e: e.matmul(pX[:, :], lhsT=at_sb[:, h, :], rhs=wo_att[:, h, hs], start=(h == 0), stop=False), R=[b_atsb, b_woa], W=[bpX])
                        for cc in range(4):
                            k.op(pe, lambda e: e.matmul(pX[:, :], lhsT=ssdT[:, cc, :], rhs=wo_ssd[:, cc, hs], start=False, stop=(cc == 3)), R=[b_ssdT, b_wos], W=[bpX])
                        k.op(dve, lambda e: e.tensor_tensor(out=x1t[:, hs], in0=xtile[:, hs], in1=pX[:, :], op=ALU.add), R=[bx, bpX], W=[b_x1t])
                    k.dma(sp, x1_out[row0 + t0:row0 + t0 + 128, :], x1t[:], R=[b_x1t], W=[b_x1])
                    rmsnorm_to_bf(x1t[:], b_x1t, gffn[:], b_gffn, hb[:], b_hb)
                    ph, bph = PH[cnt["ph"] % 2]
                    cnt["ph"] += 1
                    for cc in range(8):
                        k.op(pe, lambda e: e.transpose(out=ph[:, cc * 128:(cc + 1) * 128], in_=hb[:, cc * 128:(cc + 1) * 128], identity=ident[:]), R=[b_hb, b_id], W=[bph])
                    k.op(act, lambda e: e.copy(out=hnT[:], in_=ph[:, :].rearrange("p (c t) -> p c t", c=8)), R=[bph], W=[b_hnT])
                    pR, bpR = PB[1]
                    for kc in range(8):
                        k.op(pe, lambda e: e.matmul(pR[:, 0:16], lhsT=hnT[:, kc, :], rhs=wr_bf[:, kc, :], start=(kc == 0), stop=(kc == 7)), R=[b_hnT, b_wr], W=[bpR])
                    k.op(dve, lambda e: e.tensor_reduce(out=sm[:, 0:1], in_=pR[:, 0:16], axis=AX.X, op=ALU.max, negate=True), R=[bpR], W=[b_sm])
                    k.op(act, lambda e: e.activation(out=lg[:], in_=pR[:, 0:16], func=AF.Exp, bias=sm[:, 0:1], accum_out=sm[:, 1:2]), R=[bpR, b_sm], W=[b_lg, b_sm])
                    k.op(dve, lambda e: e.reciprocal(out=sm[:, 2:3], in_=sm[:, 1:2]), R=[b_sm], W=[b_sm])
                    k.op(dve, lambda e: e.tensor_scalar(out=afft[:], in0=lg[:], scalar1=sm[:, 2:3], scalar2=None, op0=ALU.mult), R=[b_lg, b_sm], W=[b_afft])
                    k.dma(sp, aff_out[row0 + t0:row0 + t0 + 128, :], afft[:], R=[b_afft], W=[b_aff])

    k.finish([b_x1, b_aff])
    return nc, k


def build_b(NT, TG, n_exp=16, FF=2816):
    nc, k = _build_b(NT, TG, n_exp, FF)
    k.close()
    return nc, k.n_ins


def _build_b(NT, TG, n_exp, FF):
    nc = bass.Bass("TRN2", target_bir_lowering=False)
    k = K(nc)
    k.lazy_pe = True
    sp, act, dve, pool, pe = k.sp, k.act, k.dve, k.pool, k.pe
    NTL = NT // 128
    NFC = FF // 128
    CAP = TG // 8
    TPP = TG // 128

    def din(name, shape):
        return nc.dram_tensor(name, list(shape), F32, kind="ExternalInput").ap()

    x1_in = din("x1", [NT, D])
    aff_all = din("aff_all", [2, TG, 16])
    aff_loc = din("aff_loc", [NT, 16])
    p_in = din("p", [NT, 256])
    w_gate = din("w_gate", [n_exp, D, FF])
    w_up = din("w_up", [n_exp, D, FF])
    w_down = din("w_down", [n_exp, FF, D])
    g_ffn = din("g_ffn", [1, D])
    g_pg = din("g_pg", [1, D])
    g_ple = din("g_ple", [1, D])
    g_fin = din("g_final", [1, D])
    w_pg = din("w_pg", [D, D])
    w_ple = din("w_ple", [256, D])
    y_out = nc.dram_tensor("y", [NT, D], F32, kind="ExternalOutput").ap()
    b_y = Buf("y")
    yacc_d = nc.dram_tensor("yacc_d", [NT, D], F32).ap()

    def T(name, shape, dt):
        return k.sb(name, shape, dt), Buf(name)

    PB = [(k.ps(f"pb{i}", [128, 512], F32), Buf(f"pb{i}", excl=True)) for i in range(6)]
    PH = [(k.ps(f"ph{i}", [128, 1024], BF16), Buf(f"ph{i}", excl=True)) for i in range(2)]

    onesf, b_ones = T("onesf", [128, 128], F32)
    identf, b_idf = T("identf", [128, 128], F32)
    ident, b_id = T("ident", [128, 128], BF16)
    k.op(pool, lambda e: e.memset(onesf[:], 1.0), W=[b_ones])
    k.op(pool, lambda e: e.memset(identf[:], 0.0), W=[b_idf])
    k.op(pool, lambda e: e.affine_select(out=identf[:], in_=identf[:], pattern=[[-1, 128]], compare_op=ALU.not_equal, fill=1.0, base=0, channel_multiplier=1), R=[b_idf], W=[b_idf])
    k.op(dve, lambda e: e.tensor_copy(out=ident[:], in_=identf[:]), R=[b_idf], W=[b_id])
    thr, b_thr = T("thr", [128, 32], F32)

    def rmsnorm(xtile, bx, gtile, bg, out, bout, n=D):
        k.op(act, lambda e: e.activation(out=junk[:, 0:n], in_=xtile, func=AF.Square, accum_out=st_s[:, 0:1]), R=[bx], W=[b_junk, b_sts])
        k.op(dve, lambda e: e.tensor_scalar(out=st_s[:, 1:2], in0=st_s[:, 0:1], scalar1=1.0 / n, scalar2=EPS, op0=ALU.mult, op1=ALU.add), R=[b_sts], W=[b_sts])
        k.op(act, lambda e: e.activation(out=st_s[:, 2:3], in_=st_s[:, 1:2], func=AF.Sqrt), R=[b_sts], W=[b_sts])
        k.op(dve, lambda e: e.reciprocal(out=st_s[:, 3:4], in_=st_s[:, 2:3]), R=[b_sts], W=[b_sts])
        k.op(dve, lambda e: e.scalar_tensor_tensor(out=out, in0=xtile, scalar=st_s[:, 3:4], in1=gtile, op0=ALU.mult, op1=ALU.mult), R=[bx, b_sts, bg], W=[bout])

    with k.scope():
        aff, b_affs = T("aff", [128, 2, TPP, 16], F32)
        cmp_, b_cmp = T("cmp", [128, 2, TPP, 16], BF16)
        cntt, b_cnt = T("cntt", [128, 32], F32)
        lo, b_lo = T("lo", [128, 32], F32)
        hi, b_hi = T("hi", [128, 32], F32)
        mid, b_mid = T("mid", [128, 32], F32)
        ge, b_ge = T("ge", [128, 32], F32)
        d1, b_d1 = T("d1", [128, 32], F32)
        for g in range(2):
            k.dma(sp, aff[:, g, :, :], aff_all[g].rearrange("(p t) e -> p t e", p=128), W=[b_affs])
        k.op(pool, lambda e: e.memset(lo[:], 0.0), W=[b_lo])
        k.op(pool, lambda e: e.memset(hi[:], 1.0), W=[b_hi])
        k.op(pool, lambda e: e.memset(mid[:], 0.5), W=[b_mid])
        for it in range(32):
            mb = mid[:, :].rearrange("p (g e) -> p g e", g=2).unsqueeze(2).to_broadcast([128, 2, TPP, 16])
            k.op(dve, lambda e: e.tensor_tensor(out=cmp_[:], in0=aff[:], in1=mb, op=ALU.is_ge), R=[b_affs, b_mid], W=[b_cmp])
            k.op(dve, lambda e: e.tensor_reduce(out=cntt[:, :].rearrange("p (g e) -> p g e", g=2), in_=cmp_[:].rearrange("p g t e -> p g e t"), axis=AX.X, op=ALU.add), R=[b_cmp], W=[b_cnt])
            pC, bpC = PB[it % 2]
            k.op(pe, lambda e: e.matmul(pC[:, 0:32], lhsT=onesf[:, :], rhs=cntt[:, :], start=True, stop=True), R=[b_ones, b_cnt], W=[bpC])
            k.op(dve, lambda e: e.tensor_scalar(out=ge[:], in0=pC[:, 0:32], scalar1=CAP - 0.5, scalar2=None, op0=ALU.is_ge), R=[bpC], W=[b_ge])
            k.op(dve, lambda e: e.tensor_tensor(out=d1[:], in0=mid[:], in1=lo[:], op=ALU.subtract), R=[b_mid, b_lo], W=[b_d1])
            k.op(dve, lambda e: e.tensor_tensor(out=d1[:], in0=d1[:], in1=ge[:], op=ALU.mult), R=[b_d1, b_ge], W=[b_d1])
            k.op(dve, lambda e: e.tensor_tensor(out=lo[:], in0=lo[:], in1=d1[:], op=ALU.add), R=[b_lo, b_d1], W=[b_lo])
            k.op(dve, lambda e: e.tensor_tensor(out=d1[:], in0=hi[:], in1=mid[:], op=ALU.subtract), R=[b_hi, b_mid], W=[b_d1])
            k.op(dve, lambda e: e.tensor_tensor(out=d1[:], in0=d1[:], in1=ge[:], op=ALU.mult), R=[b_d1, b_ge], W=[b_d1])
            k.op(dve, lambda e: e.tensor_tensor(out=hi[:], in0=mid[:], in1=d1[:], op=ALU.add), R=[b_mid, b_d1], W=[b_hi])
            k.op(dve, lambda e: e.tensor_tensor(out=mid[:], in0=lo[:], in1=hi[:], op=ALU.add), R=[b_lo, b_hi], W=[b_mid])
            k.op(dve, lambda e: e.tensor_scalar(out=mid[:], in0=mid[:], scalar1=0.5, scalar2=None, op0=ALU.mult), R=[b_mid], W=[b_mid])
        k.op(dve, lambda e: e.tensor_copy(out=thr[:], in_=lo[:]), R=[b_lo], W=[b_thr])

    TBT = 8
    RS = 256
    NBLK = NTL // TBT
    b_ytl = [Buf(f"yacct{i}") for i in range(NTL)]
    hn_d = nc.dram_tensor("hn_d", [NT, D], BF16).ap()
    b_hn_d = Buf("hn_d")
    gm, b_gm = T("gm2", [128, NTL, 16], F32)
    slotidx, b_slot = T("slotidx", [128, NTL, 16], F32)
    triU, b_triU = T("triU", [128, 128], F32)
    k.op(pool, lambda e: e.affine_select(out=triU[:], in_=onesf[:], pattern=[[1, 128]], compare_op=ALU.is_ge, fill=0.0, base=0, channel_multiplier=-1), R=[b_ones], W=[b_triU])
    iota_f, b_iota = T("iota_f", [128, RS], F32)
    k.op(pool, lambda e: e.iota(iota_f[:], pattern=[[1, RS]], base=0, channel_multiplier=0, allow_small_or_imprecise_dtypes=True), W=[b_iota])
    cnt = {"ph": 0}
    with k.scope():
        xt = [T(f"xts{i}", [128, D], F32) for i in range(2)]
        junk, b_junk = T("junk_s", [128, D], F32)
        st_s, b_sts = T("st_ss", [128, 8], F32)
        hbs = [T(f"hbs{i}", [128, D], BF16) for i in range(2)]
        gffn, b_gffn = T("gffn_s", [128, D], F32)
        k.dma(sp, gffn[:], g_ffn[0:1, :].to_broadcast([128, D]), W=[b_gffn])
        afl, b_afl = T("afl", [128, 16], F32)
        msk, b_msk = T("msk", [128, 16], F32)
        basev, b_base = T("basev", [128, 16], F32)
        slv, b_slv = T("slv", [128, 16], F32)
        for t in range(NTL):
            g = 0 if t < NTL // 2 else 1
            xtile, bx = xt[t % 2]
            k.dma(sp, xtile[:], x1_in[t * 128:(t + 1) * 128, :], W=[bx])
            k.dma(sp, yacc_d[t * 128:(t + 1) * 128, :], xtile[:], R=[bx], W=[b_ytl[t]])
            k.dma(sp, afl[:], aff_loc[t * 128:(t + 1) * 128, :], W=[b_afl])
            k.op(dve, lambda e: e.tensor_tensor(out=msk[:], in0=afl[:], in1=thr[:, g * 16:(g + 1) * 16], op=ALU.is_ge), R=[b_afl, b_thr], W=[b_msk])
            k.op(dve, lambda e: e.tensor_tensor(out=gm[:, t, :], in0=afl[:], in1=msk[:], op=ALU.mult), R=[b_afl, b_msk], W=[b_gm])
            if t % TBT == 0:
                k.op(pool, lambda e: e.memset(basev[:], 0.0), W=[b_base])
            pC, bpC = PB[t % 2]
            k.op(pe, lambda e: e.matmul(pC[:, 0:16], lhsT=triU[:, :], rhs=msk[:, :], start=True, stop=True), R=[b_triU, b_msk], W=[bpC])
            k.op(pe, lambda e: e.matmul(pC[:, 16:32], lhsT=onesf[:, :], rhs=msk[:, :], start=True, stop=True), R=[b_ones, b_msk], W=[bpC])
            k.op(dve, lambda e: e.tensor_tensor(out=slv[:], in0=pC[:, 0:16], in1=basev[:], op=ALU.add), R=[bpC, b_base], W=[b_slv])
            k.op(dve, lambda e: e.tensor_tensor(out=slv[:], in0=slv[:], in1=msk[:], op=ALU.mult), R=[b_slv, b_msk], W=[b_slv])
            k.op(dve, lambda e: e.tensor_scalar(out=slotidx[:, t, :], in0=slv[:], scalar1=-1.0, scalar2=None, op0=ALU.add), R=[b_slv], W=[b_slot])
            k.op(dve, lambda e: e.tensor_tensor(out=basev[:], in0=basev[:], in1=pC[:, 16:32], op=ALU.add), R=[b_base, bpC], W=[b_base])
            hb, b_hb = hbs[t % 2]
            rmsnorm(xtile[:], bx, gffn[:], b_gffn, hb[:], b_hb)
            k.dma(sp, hn_d[t * 128:(t + 1) * 128, :], hb[:], R=[b_hb], W=[b_hn_d])

    with k.scope():
        stg = [T(f"stgm{i}", [128, 1024], F32) for i in range(2)]
        wg, b_wg = T("wg", [128, 8, FF], BF16)
        wu, b_wu = T("wu", [128, 8, FF], BF16)
        wd, b_wd = T("wd", [128, NFC, D], BF16)
        hnt = [T(f"hnt{i}", [128, D], BF16) for i in range(2)]
        Sm, b_S0 = T("Sm", [128, TBT, RS], BF16)
        b_Sj = [Buf(f"S{j}") for j in range(TBT)]
        STm, b_ST = T("STm", [128, TBT, 2, 128], BF16)
        xsT, b_xsT = T("xsT", [128, 8, RS], BF16)
        actb, b_actb = T("actb", [128, NFC, RS], BF16)
        sg, b_sg = T("sg", [128, RS], F32)
        yeb, b_yeb = T("yeb", [128, 2, D], BF16)
        yos = [T(f"yo{i}", [128, D], F32) for i in range(2)]
        sc = {"stg": 0}

        def load_cast(dst_ap, src_ap, bdst, c):
            (st, bst) = stg[sc["stg"] % 2]
            sc["stg"] += 1
            sv = st[:, 0:src_ap.shape[1] * src_ap.shape[2]].rearrange("p (c n) -> p c n", c=src_ap.shape[1])
            k.dma(sp, sv, src_ap, W=[bst])
            eng = [dve, pool, act][c % 3]
            if eng is act:
                k.op(act, lambda e: e.copy(out=dst_ap, in_=sv), R=[bst], W=[bdst])
            else:
                k.op(eng, lambda e: e.tensor_copy(out=dst_ap, in_=sv), R=[bst], W=[bdst])

        for ex in range(n_exp):
            c = 0
            for (wsrc, wdst, bw) in ((w_gate, wg, b_wg), (w_up, wu, b_wu)):
                for n0 in range(0, FF, 128):
                    load_cast(wdst[:, :, n0:n0 + 128], wsrc[ex, :, n0:n0 + 128].rearrange("(c p) n -> p c n", p=128), bw, c)
                    c += 1
            for f0 in range(0, NFC):
                load_cast(wd[:, f0:f0 + 1, :], w_down[ex, f0 * 128:(f0 + 1) * 128, :].rearrange("(c p) n -> p c n", p=128), b_wd, c)
                c += 1
            for blk in range(NBLK):
                for j in range(TBT):
                    tl = blk * TBT + j
                    k.op(dve, lambda e: e.tensor_scalar(out=Sm[:, j, :], in0=iota_f[:], scalar1=slotidx[:, tl, ex:ex + 1], scalar2=None, op0=ALU.is_equal), R=[b_iota, b_slot], W=[b_Sj[j]])
                    ht, bht = hnt[j % 2]
                    k.dma(sp, ht[:], hn_d[tl * 128:(tl + 1) * 128, :], R=[b_hn_d], W=[bht])
                    for kc in range(8):
                        pg_, bpg_ = PB[kc // 2]
                        k.op(pe, lambda e: e.matmul(pg_[:, (kc % 2) * RS:(kc % 2 + 1) * RS], lhsT=ht[:, kc * 128:(kc + 1) * 128], rhs=Sm[:, j, :], start=(j == 0 and kc % 2 == 0), stop=(j == TBT - 1), skip_group_check=True), R=[bht, b_Sj[j]], W=[bpg_])
                for i in range(4):
                    pg_, bpg_ = PB[i]
                    if i % 2 == 0:
                        k.op(act, lambda e: e.copy(out=xsT[:, 2 * i:2 * i + 2, :], in_=pg_[:, :].rearrange("p (c s) -> p c s", c=2)), R=[bpg_], W=[b_xsT])
                    else:
                        k.op(dve, lambda e: e.tensor_copy(out=xsT[:, 2 * i:2 * i + 2, :], in_=pg_[:, :].rearrange("p (c s) -> p c s", c=2)), R=[bpg_], W=[b_xsT])
                for hh in range(2):
                    ph, bph = PH[hh]
                    for jj in range(4):
                        j = hh * 4 + jj
                        for st_ in range(2):
                            k.op(pe, lambda e: e.transpose(out=ph[:, (jj * 2 + st_) * 128:(jj * 2 + st_ + 1) * 128], in_=Sm[:, j, st_ * 128:(st_ + 1) * 128], identity=ident[:]), R=[b_Sj[j], b_id], W=[bph])
                    if hh == 0:
                        k.op(act, lambda e: e.copy(out=STm[:, 0:4, :, :], in_=ph[:, :].rearrange("p (j s t) -> p j s t", j=4, s=2)), R=[bph], W=[b_ST])
                    else:
                        k.op(dve, lambda e: e.tensor_copy(out=STm[:, 4:8, :, :], in_=ph[:, :].rearrange("p (j s t) -> p j s t", j=4, s=2)), R=[bph], W=[b_ST])
                for fc in range(NFC):
                    pGU, bpGU = PB[4 + fc % 2]
                    for kc in range(8):
                        k.op(pe, lambda e: e.matmul(pGU[:, 0:RS], lhsT=wg[:, kc, fc * 128:(fc + 1) * 128], rhs=xsT[:, kc, :], start=(kc == 0), stop=(kc == 7)), R=[b_wg, b_xsT], W=[bpGU])
                    for kc in range(8):
                        k.op(pe, lambda e: e.matmul(pGU[:, RS:2 * RS], lhsT=wu[:, kc, fc * 128:(fc + 1) * 128], rhs=xsT[:, kc, :], start=(kc == 0), stop=(kc == 7)), R=[b_wu, b_xsT], W=[bpGU])
                    k.op(act, lambda e: e.activation(out=sg[:], in_=pGU[:, 0:RS], func=AF.Silu), R=[bpGU], W=[b_sg])
                    k.op(dve, lambda e: e.tensor_tensor(out=actb[:, fc, :], in0=sg[:], in1=pGU[:, RS:2 * RS], op=ALU.mult), R=[b_sg, bpGU], W=[b_actb])
                for st_ in range(2):
                    for half in range(2):
                        pY, bpY = PB[st_ * 2 + half]
                        for fc in range(NFC):
                            k.op(pe, lambda e: e.matmul(pY[:, :], lhsT=actb[:, fc, st_ * 128:(st_ + 1) * 128], rhs=wd[:, fc, half * 512:(half + 1) * 512], start=(fc == 0), stop=(fc == NFC - 1)), R=[b_actb, b_wd], W=[bpY])
                        if half == 0:
                            k.op(act, lambda e: e.copy(out=yeb[:, st_, 0:512], in_=pY[:, :]), R=[bpY], W=[b_yeb])
                        else:
                            k.op(dve, lambda e: e.tensor_copy(out=yeb[:, st_, 512:1024], in_=pY[:, :]), R=[bpY], W=[b_yeb])
                for j in range(TBT):
                    tl = blk * TBT + j
                    trow = yacc_d[tl * 128:(tl + 1) * 128, :]
                    yo, b_yo = yos[j % 2]
                    for half in range(2):
                        pZ, bpZ = PB[(j % 2) * 2 + half]
                        for st_ in range(2):
                            k.op(pe, lambda e: e.matmul(pZ[:, :], lhsT=STm[:, j, st_, :], rhs=yeb[:, st_, half * 512:(half + 1) * 512], start=(st_ == 0), stop=(st_ == 1)), R=[b_ST, b_yeb], W=[bpZ])
                        if half == 0:
                            k.op(dve, lambda e: e.tensor_scalar(out=yo[:, 0:512], in0=pZ[:, :], scalar1=gm[:, tl, ex:ex + 1], scalar2=None, op0=ALU.mult), R=[bpZ, b_gm], W=[b_yo])
                        else:
                            k.op(act, lambda e: e.activation(out=yo[:, 512:1024], in_=pZ[:, :], func=AF.Copy, scale=gm[:, tl, ex:ex + 1]), R=[bpZ, b_gm], W=[b_yo])
                    k.dma(pool, trow, yo[:], R=[b_yo], W=[b_ytl[tl]], accum_op=ALU.add)

    with k.scope():
        stg = [T(f"stgp{i}", [128, 1024], F32) for i in range(2)]
        xt = [T(f"xtp{i}", [128, D], F32) for i in range(2)]
        junk, b_junk = T("junk_p", [128, D], F32)
        st_s, b_sts = T("st_sp", [128, 8], F32)
        hb, b_hb = T("hb_p", [128, D], BF16)
        hnT, b_hnT = T("hnT_p", [128, 8, 128], BF16)
        wpg, b_wpg = T("wpg", [128, 8, D], BF16)
        wple, b_wple = T("wple", [128, 2, D], BF16)
        gpg, b_gpg = T("gpg", [128, D], F32)
        gple, b_gple = T("gple", [128, D], F32)
        gfin, b_gfin = T("gfin", [128, D], F32)
        for (gt, bg, src) in ((gpg, b_gpg, g_pg), (gple, b_gple, g_ple), (gfin, b_gfin, g_fin)):
            k.dma(sp, gt[:], src[0:1, :].to_broadcast([128, D]), W=[bg])
        for c in range(8):
            st, bst = stg[c % 2]
            k.dma(sp, st[:, 0:D], w_pg[c * 128:(c + 1) * 128, :], W=[bst])
            k.op(dve, lambda e: e.tensor_copy(out=wpg[:, c, :], in_=st[:, 0:D]), R=[bst], W=[b_wpg])
        for c in range(2):
            st, bst = stg[c % 2]
            k.dma(sp, st[:, 0:D], w_ple[c * 128:(c + 1) * 128, :], W=[bst])
            k.op(dve, lambda e: e.tensor_copy(out=wple[:, c, :], in_=st[:, 0:D]), R=[bst], W=[b_wple])
        pt_, b_pt = T("pt", [128, 256], F32)
        pb_, b_pb = T("pbf", [128, 256], BF16)
        pT_, b_pT = T("pT", [128, 2, 128], BF16)
        er, b_er = T("er", [128, D], F32)
        ev, b_ev = T("ev", [128, D], F32)
        gs, b_gs = T("gs", [128, D], F32)
        yt, b_yt = T("yt", [128, D], F32)
        for t in range(NTL):
            xtile, bx = xt[t % 2]
            k.dma(sp, xtile[:], yacc_d[t * 128:(t + 1) * 128, :], R=[b_ytl[t]], W=[bx])
            k.dma(sp, pt_[:], p_in[t * 128:(t + 1) * 128, :], W=[b_pt])
            k.op(pool, lambda e: e.tensor_copy(out=pb_[:], in_=pt_[:]), R=[b_pt], W=[b_pb])
            ph, bph = PH[cnt["ph"] % 2]
            cnt["ph"] += 1
            for c in range(2):
                k.op(pe, lambda e: e.transpose(out=ph[:, c * 128:(c + 1) * 128], in_=pb_[:, c * 128:(c + 1) * 128], identity=ident[:]), R=[b_pb, b_id], W=[bph])
            k.op(act, lambda e: e.copy(out=pT_[:], in_=ph[:, 0:256].rearrange("p (c t) -> p c t", c=2)), R=[bph], W=[b_pT])
            for half in range(2):
                pE, bpE = PB[half]
                for c in range(2):
                    k.op(pe, lambda e: e.matmul(pE[:, :], lhsT=pT_[:, c, :], rhs=wple[:, c, half * 512:(half + 1) * 512], start=(c == 0), stop=(c == 1)), R=[b_pT, b_wple], W=[bpE])
                k.op(act, lambda e: e.copy(out=er[:, half * 512:(half + 1) * 512], in_=pE[:, :]), R=[bpE], W=[b_er])
            rmsnorm(er[:], b_er, gple[:], b_gple, ev[:], b_ev)
            rmsnorm(xtile[:], bx, gpg[:], b_gpg, hb[:], b_hb)
            ph, bph = PH[cnt["ph"] % 2]
            cnt["ph"] += 1
            for c in range(8):
                k.op(pe, lambda e: e.transpose(out=ph[:, c * 128:(c + 1) * 128], in_=hb[:, c * 128:(c + 1) * 128], identity=ident[:]), R=[b_hb, b_id], W=[bph])
            k.op(act, lambda e: e.copy(out=hnT[:], in_=ph[:, :].rearrange("p (c t) -> p c t", c=8)), R=[bph], W=[b_hnT])
            for half in range(2):
                pE, bpE = PB[2 + half]
                for c in range(8):
                    k.op(pe, lambda e: e.matmul(pE[:, :], lhsT=hnT[:, c, :], rhs=wpg[:, c, half * 512:(half + 1) * 512], start=(c == 0), stop=(c == 7)), R=[b_hnT, b_wpg], W=[bpE])
                k.op(act, lambda e: e.activation(out=gs[:, half * 512:(half + 1) * 512], in_=pE[:, :], func=AF.Sigmoid), R=[bpE], W=[b_gs])
            k.op(dve, lambda e: e.tensor_tensor(out=gs[:], in0=gs[:], in1=ev[:], op=ALU.mult), R=[b_gs, b_ev], W=[b_gs])
            k.op(dve, lambda e: e.tensor_tensor(out=gs[:], in0=gs[:], in1=xtile[:], op=ALU.add), R=[b_gs, bx], W=[b_gs])
            rmsnorm(gs[:], b_gs, gfin[:], b_gfin, yt[:], b_yt)
            k.dma(sp, y_out[t * 128:(t + 1) * 128, :], yt[:], R=[b_yt], W=[b_y])
    k.finish([b_y])
    return nc, k


_CACHE = {}


def kernel(x_prompt, x_sample, p_prompt, p_sample, rel_bias, g_mix, w_in, conv_w, conv_b, dt_bias, a_log, d_skip, g_ssd,
           w_out, g_ffn, w_router, w_gate, w_up, w_down, g_pg, w_pg, w_ple, g_ple, g_final):
    f = lambda a: np.ascontiguousarray(np.asarray(a, dtype=np.float32))
    xp, xs_ = f(x_prompt), f(x_sample)
    B, S1, _ = xp.shape
    B2, S2, _ = xs_.shape
    pb, sb_ = B // NCORES, B2 // NCORES
    seqs = []
    r = 0
    for i in range(pb):
        seqs.append((r, S1))
        r += S1
    for i in range(sb_):
        seqs.append((r, S2))
        r += S2
    NT = r
    nca, _ = build_a(seqs)
    common = dict(w_in=f(w_in[0]), w_out=f(w_out[0]), w_router=f(w_router[0]), g_mix=f(g_mix[0])[None], g_ffn=f(g_ffn[0])[None],
                  conv_w=f(conv_w[0]), conv_b=f(conv_b[0])[None], dt_bias=f(dt_bias[0]).reshape(1, 16), a_log=f(a_log[0]).reshape(1, 16),
                  d_skip=f(d_skip[0])[None], g_ssd=f(g_ssd[0])[None], rel_bias=f(rel_bias), oh=_onehot_tables())
    xcore = []
    for c in range(NCORES):
        xcore.append(np.concatenate([xp[c * pb:(c + 1) * pb].reshape(-1, D), xs_[c * sb_:(c + 1) * sb_].reshape(-1, D)], 0))
    ra = run_bass_kernel_spmd(nca, [dict(common, x=xcore[c]) for c in range(NCORES)], core_ids=list(range(NCORES)))
    x1 = [ra.results[c]["x1"] for c in range(NCORES)]
    aff = [ra.results[c]["aff"] for c in range(NCORES)]
    n1 = pb * S1
    aff_all = np.stack([np.concatenate([a[:n1] for a in aff], 0), np.concatenate([a[n1:] for a in aff], 0)], 0)
    TG = aff_all.shape[1]
    assert n1 == NT - n1 and TG == NCORES * n1
    ncb, _ = build_b(NT, TG)
    pp, ps_ = f(p_prompt[0]), f(p_sample[0])
    commonb = dict(aff_all=np.ascontiguousarray(aff_all), w_gate=f(w_gate[0]), w_up=f(w_up[0]), w_down=f(w_down[0]), g_ffn=f(g_ffn[0])[None],
                   g_pg=f(g_pg[0])[None], g_ple=f(g_ple[0])[None], g_final=f(g_final)[None], w_pg=f(w_pg[0]), w_ple=f(w_ple[0]))
    inb = []
    for c in range(NCORES):
        pc = np.concatenate([pp[c * pb:(c + 1) * pb].reshape(-1, 256), ps_[c * sb_:(c + 1) * sb_].reshape(-1, 256)], 0)
        inb.append(dict(commonb, x1=x1[c], aff_loc=aff[c], p=pc))
    rb = run_bass_kernel_spmd(ncb, inb, core_ids=list(range(NCORES)))
    ys = [rb.results[c]["y"] for c in range(NCORES)]
    y_p = np.concatenate([y[:n1] for y in ys], 0).reshape(B, S1, D)
    y_s = np.concatenate([y[n1:] for y in ys], 0).reshape(B2, S2, D)
    return y_p, y_s
```
